# Optimizing a Trainium2 kernel written in Bass

```python
import math
import jax, jax.numpy as jnp
from jax import lax
import numpy as np

D_MODEL = 1024
BATCH = 8
SEQ = 4096
DEPTH = 2

GRID_W = 64
CTX_LEN = 256
EPS = 1e-6
HEAD_DIM = 64
A_HEADS = 8
A_KV = 2
B_HEADS = 8
B_KV = 2
WINDOW = 128
Q_BLOCK = 128
ROPE_THETA = 10000.0
C_HEADS = 4
C_DK = 128
C_DV = 128
CONV_W = 5
CHUNK = 64
D_HEADS = 4
D_DK = 128
D_DV = 128
RET_DECAY_BASE = 5.0
PEER_HEADS = 8
N_KEYS = 128
N_EXPERTS = N_KEYS * N_KEYS
PEER_QDIM = 256
PEER_HALF = PEER_QDIM // 2
PEER_TOPK = 16
PEER_BLOCK = 128
ATT_SPLITS = (A_HEADS * HEAD_DIM, A_KV * HEAD_DIM, A_KV * HEAD_DIM, B_HEADS * HEAD_DIM, B_KV * HEAD_DIM, B_KV * HEAD_DIM)
ATT_IN = sum(ATT_SPLITS)
REC_SPLITS = (C_HEADS * (2 * C_DK + C_DV), C_HEADS * C_DV, 4 * C_HEADS, D_HEADS * D_DK, D_HEADS * D_DK, D_HEADS * D_DV, 2 * D_HEADS * D_DV)
REC_IN = sum(REC_SPLITS)
MIX_WIDTH = A_HEADS * HEAD_DIM + B_HEADS * HEAD_DIM

kernel_name = "hybrid_dit_gqa_swa_gdn_retnet_peer"

f32 = jnp.float32


def _split_cols(p, sizes):
    return jnp.split(p, [int(s) for s in np.cumsum(sizes)[:-1]], axis=-1)


def _rmsnorm(x, g):
    x32 = x.astype(f32)
    y = x32 * lax.rsqrt(jnp.mean(x32 * x32, axis=-1, keepdims=True) + EPS)
    return (y * g.astype(f32)).astype(x.dtype)


def _l2norm(x):
    x32 = x.astype(f32)
    return (x32 * lax.rsqrt(jnp.sum(x32 * x32, axis=-1, keepdims=True) + EPS)).astype(x.dtype)


def _group_norm(x, g):
    x32 = x.astype(f32)
    mu = jnp.mean(x32, axis=-1, keepdims=True)
    var = jnp.mean(jnp.square(x32 - mu), axis=-1, keepdims=True)
    y = (x32 - mu) * lax.rsqrt(var + EPS)
    return (y * g.astype(f32).reshape(x.shape[-2:])).astype(x.dtype)


def _modulate(h, shift, scale):
    return h * (1 + scale) + shift


def _rope_2d(x, row, col):
    d = x.shape[-1]
    half, quarter = d // 2, d // 4
    freqs = ROPE_THETA ** (-jnp.arange(quarter, dtype=f32) / quarter)

    def rot(xp, pos):
        ang = pos.astype(f32)[:, None] * freqs
        cos, sin = jnp.cos(ang)[None, :, None, :], jnp.sin(ang)[None, :, None, :]
        x1, x2 = xp[..., :quarter].astype(f32), xp[..., quarter:].astype(f32)
        return jnp.concatenate([x1 * cos - x2 * sin, x2 * cos + x1 * sin], axis=-1)

    return jnp.concatenate([rot(x[..., :half], row), rot(x[..., half:], col)], axis=-1).astype(x.dtype)


def _att_project(h, w_in, q_g, k_g):
    Bn, L, _ = h.shape
    qa, ka, va, qb, kb, vb = _split_cols(h @ w_in, ATT_SPLITS)
    qa = _rmsnorm(qa.reshape(Bn, L, A_HEADS, HEAD_DIM), q_g)
    ka = _rmsnorm(ka.reshape(Bn, L, A_KV, HEAD_DIM), k_g)
    va = va.reshape(Bn, L, A_KV, HEAD_DIM)
    qb = qb.reshape(Bn, L, B_HEADS, HEAD_DIM)
    kb = kb.reshape(Bn, L, B_KV, HEAD_DIM)
    vb = vb.reshape(Bn, L, B_KV, HEAD_DIM)
    return qa, ka, va, qb, kb, vb


def _group(q, n_kv):
    Bn, L, H, d = q.shape
    return (q * (d ** -0.5)).reshape(Bn, L, n_kv, H // n_kv, d)


def _gqa_attend(q, keys, vals, masks, sink):
    scores = []
    for k, m in zip(keys, masks):
        s = jnp.einsum("bqkgd,bskd->bkgqs", q, k).astype(f32)
        scores.append(s if m is None else jnp.where(m, s, -jnp.inf))
    sizes = [k.shape[1] for k in keys]
    if sink is not None:
        scores.append(jnp.broadcast_to(sink.astype(f32)[None, :, :, None, None], scores[0].shape[:-1] + (1,)))
    p = jax.nn.softmax(jnp.concatenate(scores, axis=-1), axis=-1).astype(vals[0].dtype)
    parts = _split_cols(p, sizes + [p.shape[-1] - sum(sizes)])
    out = 0
    for pi, v in zip(parts, vals):
        out = out + jnp.einsum("bkgqs,bskd->bqkgd", pi, v)
    return out


def _att_latent(qa, ka, va, qb, kb, vb, ka_c, va_c, kb_c, vb_c, sink):
    Bn, L = qa.shape[:2]
    nb = L // Q_BLOCK
    band = Q_BLOCK + 2 * WINDOW
    kb_pad = jnp.pad(kb, ((0, 0), (WINDOW, WINDOW), (0, 0), (0, 0)))
    vb_pad = jnp.pad(vb, ((0, 0), (WINDOW, WINDOW), (0, 0), (0, 0)))
    q_off = jnp.arange(Q_BLOCK)
    k_off = jnp.arange(band) - WINDOW

    def to_blocks(q):
        return jnp.moveaxis(q.reshape(Bn, nb, Q_BLOCK, *q.shape[2:]), 1, 0)

    def one_block(args):
        j, qa_j, qb_j = args
        start = j * Q_BLOCK
        oa = _gqa_attend(qa_j, [ka, ka_c], [va, va_c], [None, None], None)
        kb_j = lax.dynamic_slice_in_dim(kb_pad, start, band, axis=1)
        vb_j = lax.dynamic_slice_in_dim(vb_pad, start, band, axis=1)
        qpos, kpos = start + q_off, start + k_off
        mask = (jnp.abs(qpos[:, None] - kpos[None, :]) <= WINDOW) & (kpos >= 0)[None, :] & (kpos < L)[None, :]
        ob = _gqa_attend(qb_j, [kb_j, kb_c], [vb_j, vb_c], [mask, None], sink)
        return oa, ob

    oa, ob = lax.map(one_block, (jnp.arange(nb), to_blocks(qa), to_blocks(qb)))

    def from_blocks(o):
        return jnp.moveaxis(o, 0, 1).reshape(Bn, L, -1)

    return jnp.concatenate([from_blocks(oa), from_blocks(ob)], axis=-1)


def _att_mixer(h_lat, h_ctx, w_in, q_g, k_g, sink, w_out, row, col, with_ctx_out):
    Bn, Lc = h_ctx.shape[:2]
    qa, ka, va, qb, kb, vb = _att_project(h_lat, w_in, q_g, k_g)
    qa, ka, qb, kb = (_rope_2d(t, row, col) for t in (qa, ka, qb, kb))
    qa_c, ka_c, va_c, qb_c, kb_c, vb_c = _att_project(h_ctx, w_in, q_g, k_g)
    sink_g = sink.reshape(B_KV, B_HEADS // B_KV)
    y_lat = _att_latent(_group(qa, A_KV), ka, va, _group(qb, B_KV), kb, vb, ka_c, va_c, kb_c, vb_c, sink_g) @ w_out
    y_ctx = None
    if with_ctx_out:
        oa = _gqa_attend(_group(qa_c, A_KV), [ka_c], [va_c], [None], None)
        ob = _gqa_attend(_group(qb_c, B_KV), [kb_c], [vb_c], [None], sink_g)
        y_ctx = jnp.concatenate([oa.reshape(Bn, Lc, -1), ob.reshape(Bn, Lc, -1)], axis=-1) @ w_out
    return y_lat, y_ctx


def _short_conv(x, w):
    ch = x.shape[-1]
    return lax.conv_general_dilated(x, w[:, None, :], window_strides=(1,), padding=[(CONV_W // 2, CONV_W // 2)],
                                    dimension_numbers=("NWC", "WIO", "NWC"), feature_group_count=ch)


def _to_chunks(t):
    Bn, L, H = t.shape[:3]
    t = t.astype(f32).reshape(Bn, L // CHUNK, CHUNK, H, *t.shape[3:])
    return jnp.moveaxis(jnp.moveaxis(t, 3, 2), 1, 0)


def _from_chunks(o):
    n, Bn, H, C, d = o.shape
    return jnp.moveaxis(jnp.moveaxis(o, 0, 1), 2, 3).reshape(Bn, n * C, H, d)


def _gated_delta_chunked(q, k, v, beta, log_alpha, s0):
    dv = v.shape[-1]
    qc, kc, vc, bc, ac = (_to_chunks(t) for t in (q, k, v, beta, log_alpha))
    g = jnp.cumsum(ac, axis=-1)
    tri = jnp.tril(jnp.ones((CHUNK, CHUNK), bool))
    tri_strict = jnp.tril(jnp.ones((CHUNK, CHUNK), bool), -1)
    diff = g[..., :, None] - g[..., None, :]
    dec = jnp.exp(jnp.where(tri, diff, -jnp.inf))
    dec_strict = jnp.exp(jnp.where(tri_strict, diff, -jnp.inf))
    kk = jnp.einsum("nbhcd,nbhsd->nbhcs", kc, kc)
    a_mat = jnp.eye(CHUNK, dtype=f32) + bc[..., None] * kk * dec_strict
    rhs = jnp.concatenate([bc[..., None] * vc, (bc * jnp.exp(g))[..., None] * kc], axis=-1)
    sol = lax.linalg.triangular_solve(a_mat, rhs, left_side=True, lower=True)
    u, wk = sol[..., :dv], sol[..., dv:]
    qk = jnp.einsum("nbhcd,nbhsd->nbhcs", qc, kc) * dec
    g_last = g[..., -1:]
    k_end = kc * jnp.exp(g_last - g)[..., None]

    def step(s, xs):
        qj, uj, wkj, gj, qkj, kej, glj = xs
        w = uj - jnp.einsum("bhcd,bhde->bhce", wkj, s)
        o = jnp.exp(gj)[..., None] * jnp.einsum("bhcd,bhde->bhce", qj, s) + jnp.einsum("bhcs,bhse->bhce", qkj, w)
        s = jnp.exp(glj)[..., None] * s + jnp.einsum("bhcd,bhce->bhde", kej, w)
        return s, o

    s, o = lax.scan(step, s0.astype(f32), (qc, u, wk, g, qk, k_end, g_last))
    return _from_chunks(o).astype(v.dtype), s


def _retention_chunked(q, k, v, log_gamma, s0):
    qc, kc, vc = _to_chunks(q), _to_chunks(k), _to_chunks(v)
    lg = log_gamma.astype(f32)
    pos = jnp.arange(CHUNK, dtype=f32)
    tri = jnp.tril(jnp.ones((CHUNK, CHUNK), bool))
    dmat = jnp.exp(jnp.where(tri, lg[:, None, None] * (pos[:, None] - pos[None, :]), -jnp.inf))
    inner = jnp.einsum("nbhcs,nbhse->nbhce", jnp.einsum("nbhcd,nbhsd->nbhcs", qc, kc) * dmat, vc)
    q_dec = jnp.exp(lg[:, None] * (pos + 1.0))
    k_dec = jnp.exp(lg[:, None] * (CHUNK - 1.0 - pos))
    chunk_dec = jnp.exp(lg * CHUNK)
    kd = kc * k_dec[..., None]

    def step(s, xs):
        qj, kj, vj = xs
        o = jnp.einsum("bhcd,bhde->bhce", qj, s) * q_dec[..., None]
        s = chunk_dec[:, None, None] * s + jnp.einsum("bhcd,bhce->bhde", kj, vj)
        return s, o

    s, cross = lax.scan(step, s0.astype(f32), (qc, kd, vc))
    return _from_chunks(inner + cross).astype(v.dtype), s


def _directional(scan_fn, args, s0, reverse):
    if reverse:
        args = tuple(jnp.flip(a, axis=1) for a in args)
    o, s = scan_fn(*args, s0)
    return (jnp.flip(o, axis=1) if reverse else o), s


def _rec_features(h, w_in, conv_w, a_log, dt_bias, row, col):
    Bn, L, _ = h.shape
    qkv, z, gates, qd, kd, vd, gd = _split_cols(h @ w_in, REC_SPLITS)
    qkv = jax.nn.silu(_short_conv(qkv, conv_w))
    qc, kc, vc = _split_cols(qkv, (C_HEADS * C_DK, C_HEADS * C_DK, C_HEADS * C_DV))
    qc = _l2norm(qc.reshape(Bn, L, C_HEADS, C_DK)) * (C_DK ** -0.5)
    kc = _l2norm(kc.reshape(Bn, L, C_HEADS, C_DK))
    vc = vc.reshape(Bn, L, C_HEADS, C_DV)
    gates = gates.reshape(Bn, L, 2, 2, C_HEADS).astype(f32)
    log_alpha = -jnp.exp(a_log.astype(f32)) * jax.nn.softplus(gates[:, :, 0] + dt_bias.astype(f32))
    beta = jax.nn.sigmoid(gates[:, :, 1])
    qd = qd.reshape(Bn, L, D_HEADS, D_DK)
    kd = kd.reshape(Bn, L, D_HEADS, D_DK)
    if row is not None:
        qd, kd = _rope_2d(qd, row, col), _rope_2d(kd, row, col)
    kd = kd * (D_DK ** -0.5)
    vd = vd.reshape(Bn, L, D_HEADS, D_DV)
    gd = gd.reshape(Bn, L, 2, D_HEADS * D_DV)
    return qc, kc, vc, z, beta, log_alpha, qd, kd, vd, gd


def _rec_merge(delta_dirs, z, ret_dirs, gd, out_g, gn_g, w_out):
    Bn, L = z.shape[:2]
    yc = _rmsnorm(delta_dirs[0] + delta_dirs[1], out_g).reshape(Bn, L, -1) * jax.nn.silu(z)
    yd = 0
    for d in range(2):
        yd = yd + _group_norm(ret_dirs[d], gn_g).reshape(Bn, L, -1) * jax.nn.silu(gd[:, :, d])
    return jnp.concatenate([yc, yd], axis=-1) @ w_out


def _rec_mixer(h_lat, h_ctx, w_in, conv_w, a_log, dt_bias, out_g, gn_g, w_out, row, col, with_ctx_out):
    Bn = h_lat.shape[0]
    qc, kc, vc, z, beta, la, qd, kd, vd, gd = _rec_features(h_lat, w_in, conv_w, a_log, dt_bias, row, col)
    qc_c, kc_c, vc_c, z_c, beta_c, la_c, qd_c, kd_c, vd_c, gd_c = _rec_features(h_ctx, w_in, conv_w, a_log, dt_bias, None, None)
    log_gamma = jnp.log1p(-jnp.exp2(-(RET_DECAY_BASE + jnp.arange(D_HEADS, dtype=f32))))
    retention = lambda q, k, v, s0: _retention_chunked(q, k, v, log_gamma, s0)
    zero_c = jnp.zeros((Bn, C_HEADS, C_DK, C_DV), f32)
    zero_d = jnp.zeros((Bn, D_HEADS, D_DK, D_DV), f32)
    delta_lat, delta_ctx, ret_lat, ret_ctx = [], [], [], []
    for d, rev in ((0, False), (1, True)):
        o, s = _directional(_gated_delta_chunked, (qc_c, kc_c, vc_c, beta_c[:, :, d], la_c[:, :, d]), zero_c, rev)
        delta_ctx.append(o)
        delta_lat.append(_directional(_gated_delta_chunked, (qc, kc, vc, beta[:, :, d], la[:, :, d]), s, rev)[0])
        o, s = _directional(retention, (qd_c, kd_c, vd_c), zero_d, rev)
        ret_ctx.append(o)
        ret_lat.append(_directional(retention, (qd, kd, vd), s, rev)[0])
    y_lat = _rec_merge(delta_lat, z, ret_lat, gd, out_g, gn_g, w_out)
    y_ctx = _rec_merge(delta_ctx, z_c, ret_ctx, gd_c, out_g, gn_g, w_out) if with_ctx_out else None
    return y_lat, y_ctx


def _peer(h, w_q, sub_keys, u, v):
    shp = h.shape
    tokens = h.reshape(-1, PEER_BLOCK, shp[-1])

    def block(xb):
        T = xb.shape[0]
        q = (xb @ w_q).reshape(T, PEER_HEADS, 2, PEER_HALF)
        s = jnp.einsum("thpd,pkd->thpk", q, sub_keys).astype(f32)
        sv, si = lax.top_k(s, PEER_TOPK)
        cand_s = (sv[:, :, 0, :, None] + sv[:, :, 1, None, :]).reshape(T, PEER_HEADS, -1)
        cand_i = (si[:, :, 0, :, None] * N_KEYS + si[:, :, 1, None, :]).reshape(T, PEER_HEADS, -1)
        top_s, pos = lax.top_k(cand_s, PEER_TOPK)
        idx = jnp.take_along_axis(cand_i, pos, axis=-1)
        gate = jax.nn.softmax(top_s, axis=-1)
        act = jax.nn.gelu(jnp.einsum("td,thkd->thk", xb, u[idx]).astype(f32), approximate=False)
        return jnp.einsum("thk,thkd->td", (gate * act).astype(xb.dtype), v[idx])

    return lax.map(block, tokens).reshape(shp)


def setup_inputs(seed: int = 0) -> dict:
    key = jax.random.key(seed)
    k = jax.random.split(key, 26)
    D = D_MODEL
    n_even, n_odd = (DEPTH + 1) // 2, DEPTH // 2

    def nrm(i, shape, std):
        return jax.random.normal(k[i], shape, f32) * std

    dt = jnp.exp(jax.random.uniform(k[16], (n_odd, 2, C_HEADS), f32, math.log(1e-3), math.log(1e-1)))
    return {
        "x": nrm(0, (BATCH, SEQ, D), 1.0),
        "c": nrm(1, (BATCH, D), 1.0),
        "ctx": nrm(2, (BATCH, CTX_LEN, D), 1.0),
        "c_ctx": nrm(3, (D,), 1.0),
        "mod_w": nrm(4, (DEPTH, D, 6 * D), 0.5 * D ** -0.5),
        "mod_b": nrm(5, (DEPTH, 6 * D), 0.02),
        "norm1_g": 1.0 + nrm(6, (DEPTH, D), 0.02),
        "norm2_g": 1.0 + nrm(7, (DEPTH, D), 0.02),
        "att_w_in": nrm(8, (n_even, D, ATT_IN), D ** -0.5),
        "att_q_norm": 1.0 + nrm(9, (n_even, HEAD_DIM), 0.02),
        "att_k_norm": 1.0 + nrm(10, (n_even, HEAD_DIM), 0.02),
        "att_sink": nrm(11, (n_even, B_HEADS), 0.5),
        "att_w_out": nrm(12, (n_even, MIX_WIDTH, D), MIX_WIDTH ** -0.5),
        "rec_w_in": nrm(13, (n_odd, D, REC_IN), D ** -0.5),
        "rec_conv_w": nrm(14, (n_odd, CONV_W, C_HEADS * (2 * C_DK + C_DV)), CONV_W ** -0.5),
        "rec_a_log": jnp.log(jax.random.uniform(k[15], (n_odd, 2, C_HEADS), f32, 1.0, 16.0)),
        "rec_dt_bias": dt + jnp.log(-jnp.expm1(-dt)),
        "rec_out_norm": 1.0 + nrm(17, (n_odd, C_DV), 0.02),
        "rec_gn_g": 1.0 + nrm(18, (n_odd, D_HEADS * D_DV), 0.02),
        "rec_w_out": nrm(19, (n_odd, MIX_WIDTH, D), MIX_WIDTH ** -0.5),
        "peer_w_q": nrm(20, (DEPTH, D, PEER_HEADS * PEER_QDIM), D ** -0.5),
        "peer_sub_keys": nrm(21, (DEPTH, 2, N_KEYS, PEER_HALF), PEER_HALF ** -0.5),
        "peer_u": nrm(22, (DEPTH, N_EXPERTS, D), D ** -0.5),
        "peer_v": nrm(23, (DEPTH, N_EXPERTS, D), 0.5),
        "final_norm_g": 1.0 + nrm(24, (D,), 0.02),
    }


def reference(x, c, ctx, c_ctx, mod_w, mod_b, norm1_g, norm2_g, att_w_in, att_q_norm, att_k_norm, att_sink, att_w_out,
              rec_w_in, rec_conv_w, rec_a_log, rec_dt_bias, rec_out_norm, rec_gn_g, rec_w_out,
              peer_w_q, peer_sub_keys, peer_u, peer_v, final_norm_g):
    rows = x.shape[1] // GRID_W
    row = jnp.repeat(jnp.arange(rows, dtype=jnp.int32), GRID_W)
    col = jnp.tile(jnp.arange(GRID_W, dtype=jnp.int32), rows)
    silu_c, silu_cc = jax.nn.silu(c), jax.nn.silu(c_ctx)
    h, hc = x, ctx
    for layer in range(DEPTH):
        last = layer == DEPTH - 1
        mod = jnp.split((silu_c @ mod_w[layer] + mod_b[layer])[:, None, :], 6, axis=-1)
        mod_c = jnp.split((silu_cc @ mod_w[layer] + mod_b[layer])[None, None, :], 6, axis=-1)
        a = _modulate(_rmsnorm(h, norm1_g[layer]), mod[0], mod[1])
        ac = _modulate(_rmsnorm(hc, norm1_g[layer]), mod_c[0], mod_c[1])
        i = layer // 2
        if layer % 2 == 0:
            y, yc = _att_mixer(a, ac, att_w_in[i], att_q_norm[i], att_k_norm[i], att_sink[i], att_w_out[i],
                               row, col, not last)
        else:
            y, yc = _rec_mixer(a, ac, rec_w_in[i], rec_conv_w[i], rec_a_log[i], rec_dt_bias[i], rec_out_norm[i],
                               rec_gn_g[i], rec_w_out[i], row, col, not last)
        h = h + mod[2] * y
        h = h + mod[5] * _peer(_modulate(_rmsnorm(h, norm2_g[layer]), mod[3], mod[4]),
                               peer_w_q[layer], peer_sub_keys[layer], peer_u[layer], peer_v[layer])
        if not last:
            hc = hc + mod_c[2] * yc
            hc = hc + mod_c[5] * _peer(_modulate(_rmsnorm(hc, norm2_g[layer]), mod_c[3], mod_c[4]),
                                       peer_w_q[layer], peer_sub_keys[layer], peer_u[layer], peer_v[layer])
    return _rmsnorm(h, final_norm_g)
```

```python
import numpy as np
from contextlib import ExitStack
import concourse.bass as bass
import concourse.mybir as mybir
from concourse.bass_utils import run_bass_kernel_spmd

F32 = mybir.dt.float32
BF16 = mybir.dt.bfloat16
AF = mybir.ActivationFunctionType
ALU = mybir.AluOpType
AX = mybir.AxisListType

EPOCH = 30000
NSLOT = 8
D = 1024
L = 4096
LC = 256
NT = 34
NTL = 32
EPS = 1e-6


class _Eng:
    def __init__(self, name, obj):
        self.name = name
        self.obj = obj
        self.sems = []
        self.count = 0
        self.waited = {}
        self.slots = []
        self.slot_i = 0
        self.ninstr = 0


class _Res:
    __slots__ = ("lw", "rd")

    def __init__(self):
        self.lw = None
        self.rd = {}


class Ctx:
    def __init__(self, nc):
        self.nc = nc
        self.es = ExitStack()
        self.semh = {}
        self.nsem = 0
        self.engs = {}
        for name, obj in (("pe", nc.tensor), ("dve", nc.vector), ("act", nc.scalar),
                          ("pool", nc.gpsimd), ("sp", nc.sync)):
            self.engs[name] = _Eng(name, obj)
        self.res = {}
        self.scopes = []

    def new_sem(self, name):
        self.nsem += 1
        h = self.es.enter_context(self.nc.semaphore("%s_%d" % (name, self.nsem)))
        key = "s%d_%s" % (self.nsem, name)
        self.semh[key] = h
        return key

    def push(self):
        self.scopes.append(ExitStack())

    def pop(self):
        self.barrier()
        self.scopes.pop().close()

    def _stk(self):
        return self.scopes[-1] if self.scopes else self.es

    def sb(self, name, shape, dt=F32):
        self.uid = getattr(self, "uid", 0) + 1
        return self._stk().enter_context(self.nc.sbuf_tensor("%s_s%d" % (name, self.uid), list(shape), dt))

    def ps(self, name, shape, dt=F32):
        self.uid = getattr(self, "uid", 0) + 1
        return self._stk().enter_context(self.nc.psum_tensor("%s_p%d" % (name, self.uid), list(shape), dt))

    def dram(self, name, shape, dt=F32):
        return self.nc.dram_tensor(name, list(shape), dt, kind="Internal").ap()

    def close(self):
        self.es.close()

    def _wait(self, E, tok):
        if tok is None:
            return
        sem, val = tok
        if E.waited.get(sem, 0) >= val:
            return
        E.obj.wait_ge(self.semh[sem], val)
        E.waited[sem] = val

    def _R(self, key):
        r = self.res.get(key)
        if r is None:
            r = self.res[key] = _Res()
        return r

    def op(self, e, fn, reads=(), writes=(), dma=False):
        E = self.engs[e]
        own = set(E.sems)
        need = []
        for k in reads:
            r = self._R(k)
            if r.lw is not None:
                need.append(r.lw)
        for k in writes:
            r = self._R(k)
            if r.lw is not None:
                need.append(r.lw)
            for s, v in r.rd.items():
                need.append((s, v))
        for tok in need:
            if e == "pe" and tok[0] in own:
                continue
            self._wait(E, tok)
        if dma:
            if not E.slots:
                for i in range(NSLOT):
                    E.slots.append([self.new_sem("%s_dma%d" % (e, i)), 0])
            sl = E.slots[E.slot_i % NSLOT]
            E.slot_i += 1
            if sl[1] > 0:
                self._wait(E, (sl[0], sl[1]))
            if sl[1] + 16 > EPOCH:
                sl[0] = self.new_sem("%s_dmaX" % e)
                sl[1] = 0
            ins = fn(E.obj)
            sl[1] += 16
            ins.then_inc(self.semh[sl[0]], 16)
            tok = (sl[0], sl[1])
        else:
            if not E.sems or E.count >= EPOCH:
                E.sems.append(self.new_sem(e))
                E.count = 0
            ins = fn(E.obj)
            E.count += 1
            ins.then_inc(self.semh[E.sems[-1]], 1)
            tok = (E.sems[-1], E.count)
        E.ninstr += 1
        for k in writes:
            r = self._R(k)
            r.lw = tok
            r.rd = {}
        for k in reads:
            if k in writes:
                continue
            r = self._R(k)
            r.rd[tok[0]] = max(r.rd.get(tok[0], 0), tok[1])
        return tok

    def barrier(self):
        toks = []
        for E in self.engs.values():
            if E.sems and E.count > 0:
                toks.append((E.sems[-1], E.count))
            for sl in E.slots:
                if sl[1] > 0:
                    toks.append((sl[0], sl[1]))
        for E in self.engs.values():
            own = set(E.sems)
            for t in toks:
                if t[0] in own:
                    continue
                self._wait(E, t)

    def finish(self, keys, e="sp"):
        E = self.engs[e]
        for k in keys:
            self._wait(E, self._R(k).lw)

    def dma(self, out, in_, reads, writes, e="sp", **kw):
        return self.op(e, lambda o: o.dma_start(out=out, in_=in_, **kw), reads, writes, dma=True)

    def mm(self, out, lhsT, rhs, start, stop, reads, writes):
        return self.op("pe", lambda o: o.matmul(out, lhsT, rhs, start=start, stop=stop), reads, writes)

    def tr(self, out, in_, ident, reads, writes):
        return self.op("pe", lambda o: o.transpose(out, in_, ident), reads, writes)

    def act(self, out, in_, func, reads, writes, **kw):
        return self.op("act", lambda o: o.activation(out=out, in_=in_, func=func, **kw), reads, writes)

    def tt(self, e, out, in0, in1, op, reads, writes):
        return self.op(e, lambda o: o.tensor_tensor(out=out, in0=in0, in1=in1, op=op), reads, writes)

    def ts(self, e, out, in0, s1, s2, op0, op1, reads, writes):
        if op1 is None:
            return self.op(e, lambda o: o.tensor_scalar(out=out, in0=in0, scalar1=s1, scalar2=None, op0=op0), reads, writes)
        return self.op(e, lambda o: o.tensor_scalar(out=out, in0=in0, scalar1=s1, scalar2=s2, op0=op0, op1=op1), reads, writes)

    def smul(self, e, out, in0, sc, reads, writes):
        if e == "act":
            return self.op(e, lambda o: o.activation(out=out, in_=in0, func=AF.Copy, scale=sc), reads, writes)
        return self.op(e, lambda o: o.tensor_scalar(out=out, in0=in0, scalar1=sc, scalar2=None, op0=ALU.mult), reads, writes)

    def cp(self, e, out, in_, reads, writes):
        if e == "act":
            return self.op(e, lambda o: o.copy(out=out, in_=in_), reads, writes)
        return self.op(e, lambda o: o.tensor_copy(out=out, in_=in_), reads, writes)


class Prog:
    pass


def _interleave(gens):
    alive = list(gens)
    while alive:
        for g in list(alive):
            try:
                next(g)
            except StopIteration:
                alive.remove(g)


def hrows(K, t):
    return K.H[t * 128:(t + 1) * 128, :]


def src_rows(K, layer, t):
    if layer == 0:
        if t < NTL:
            return K.x[t * 128:(t + 1) * 128, :], "x"
        return K.ctx[(t - NTL) * 128:(t - NTL + 1) * 128, :], "ctx"
    return hrows(K, t), "H%d" % t


def emit_modulation(c, K, layer):
    c.push()
    wbuf = [c.sb("modw%d" % i, [128, 3072]) for i in range(4)]
    pm = [c.ps("pm%d" % i, [128, 512]) for i in range(6)]
    pcol = c.ps("pcol", [128, 64])
    pg = c.ps("pgate", [128, 512])
    modb = c.sb("modb", [2, 6144])
    K.modrow = c.sb("modrow", [2, 6144])
    c.dma(modb[0:1, :], K.mod_b[layer:layer + 1, :], [], ["modb"])
    c.dma(modb[1:2, :], K.mod_b[layer:layer + 1, :], [], ["modb"])
    n = 0
    for half in range(2):
        for ch in range(8):
            wb = wbuf[n % 4]
            wk = "modw%d" % (n % 4)
            n += 1
            c.dma(wb[:], K.mod_w[layer, ch * 128:(ch + 1) * 128, half * 3072:(half + 1) * 3072], [], [wk],
                  e=("sp", "pool", "act")[n % 3])
            for j in range(6):
                c.mm(pm[j][0:2, :], K.scT[:, ch, :], wb[:, j * 512:(j + 1) * 512], ch == 0, ch == 7,
                     [wk, "scT"], ["pm%d" % j])
        for j in range(6):
            col = half * 3072 + j * 512
            c.tt("dve", K.modrow[0:2, col:col + 512], pm[j][0:2, :], modb[0:2, col:col + 512], ALU.add,
                 ["pm%d" % j, "modb"], ["modrow"])
    for si, seg in enumerate((0, 1, 3, 4)):
        for ch in range(8):
            idx = si * 8 + ch
            c.mm(pcol[:, idx * 2:idx * 2 + 2], K.modrow[0:2, seg * 1024 + ch * 128: seg * 1024 + (ch + 1) * 128],
                 K.identf[0:2, 0:2], True, True, ["modrow", "identf"], ["pcol"])
    colv = c.sb("colv", [128, 64])
    c.cp("dve", colv[:], pcol[:], ["pcol"], ["colv"])
    cv = colv[:].rearrange("p (s c m) -> p s c m", s=4, c=8, m=2)
    for m in range(2):
        for which, (ssc, ssh, g) in enumerate(((1, 0, K.g1col), (3, 2, K.g2col))):
            sc = K.scol[m][which]
            c.ts("dve", sc[:], cv[:, ssc, :, m], 1.0, None, ALU.add, None, ["colv"], ["scol%d%d" % (m, which)])
            c.tt("dve", sc[:], sc[:], g[:, layer, :], ALU.mult, ["scol%d%d" % (m, which), "gcol"], ["scol%d%d" % (m, which)])
            c.cp("dve", K.shcol[m][which][:], cv[:, ssh, :, m], ["colv"], ["shcol%d%d" % (m, which)])
    for m in range(2):
        for which, seg in enumerate((2, 5)):
            for half in range(2):
                c.mm(pg[:, :], K.sel[0:2, m, :], K.modrow[0:2, seg * 1024 + half * 512: seg * 1024 + (half + 1) * 512],
                     True, True, ["modrow", "sel"], ["pgate"])
                c.cp("act", K.gbc[m][which][:, half * 512:(half + 1) * 512], pg[:, :], ["pgate"], ["gbc%d%d" % (m, which)])
    c.pop()


def emit_norm_T(c, K, W, src, srckey, m, which, aT_out, aTkey, keep_h=None):
    W.n += 1
    i = W.n % 2
    hb, hk = W.hbuf[i], "hbuf%d" % i
    c.dma(hb[:], src, [srckey], [hk], e="sp" if W.n % 2 else "pool")
    c.tt("dve", W.junk[:], hb[:], hb[:], ALU.mult, [hk], ["junk"])
    c.op("dve", lambda o: o.tensor_reduce(out=W.ss[:, 0:1], in_=W.junk[:], axis=AX.X, op=ALU.add), ["junk"], ["ss"])
    c.ts("dve", W.ss[:, 1:2], W.ss[:, 0:1], 1.0 / D, EPS, ALU.mult, ALU.add, ["ss"], ["ss1"])
    c.act(W.ss[:, 3:4], W.ss[:, 1:2], AF.Sqrt, ["ss1"], ["ss3"])
    c.op("dve", lambda o: o.reciprocal(out=W.ss[:, 2:3], in_=W.ss[:, 3:4]), ["ss3"], ["ss2"])
    c.ts("dve", W.hn[:], hb[:], W.ss[:, 2:3], None, ALU.mult, None, [hk, "ss2"], ["hn"])
    for ch in range(8):
        c.tr(W.ptr[:, ch * 128:(ch + 1) * 128], W.hn[:, ch * 128:(ch + 1) * 128], K.identb[:], ["hn", "identb"], ["ptr"])
    pv = W.ptr[:, 0:1024].rearrange("p (c t) -> p c t", c=8)
    sc = K.scol[m][which][:].unsqueeze(2).to_broadcast([128, 8, 128])
    sh = K.shcol[m][which][:].unsqueeze(2).to_broadcast([128, 8, 128])
    c.tt("dve", W.tmpf[:].rearrange("p (c t) -> p c t", c=8), pv, sc, ALU.mult, ["ptr", "scol%d%d" % (m, which)], ["tmpf"])
    c.tt("dve", aT_out, W.tmpf[:].rearrange("p (c t) -> p c t", c=8), sh, ALU.add, ["tmpf", "shcol%d%d" % (m, which)], [aTkey])


def alloc_normwork(c, ptr):
    W = Prog()
    W.n = 0
    W.hbuf = [c.sb("hbuf%d" % i, [128, 1024]) for i in range(2)]
    W.junk = c.sb("junk", [128, 1024])
    W.tmpf = c.sb("tmpf", [128, 1024])
    W.ss = c.sb("ss", [128, 4])
    W.hn = c.sb("hn", [128, 1024], BF16)
    W.ptr = ptr
    return W


def emit_residual(c, K, W, layer_src, py, pykeys, m, which, t, first_layer_src=None):
    src, srckey = layer_src
    W.n += 1
    i = W.n % 2
    hb, hk = W.hbuf[i], "hbuf%d" % i
    c.dma(hb[:], src, [srckey], [hk], e="sp" if W.n % 2 else "pool")
    for hb_ in range(2):
        c.tt("dve", W.tmpf[:, hb_ * 512:(hb_ + 1) * 512], py[:, hb_ * 512:(hb_ + 1) * 512], K.gbc[m][which][:, hb_ * 512:(hb_ + 1) * 512],
             ALU.mult, list(pykeys) + ["gbc%d%d" % (m, which)], ["tmpf"])
    c.tt("dve", hb[:], hb[:], W.tmpf[:], ALU.add, [hk, "tmpf"], [hk])
    c.dma(hrows(K, t), hb[:], [hk], ["H%d" % t], e="sp")


def emit_attention(c, K):
    layer = 0
    c.push()
    kTa = [c.sb("kTa%d" % i, [128, NT * 128], BF16) for i in range(2)]
    kTb = [c.sb("kTb%d" % i, [128, NT * 128], BF16) for i in range(2)]
    Va = c.sb("Va", [128, NT, 2, 128], BF16)
    Vb = c.sb("Vb", [128, NT, 2, 128], BF16)
    for i in range(2):
        c.op("pool", lambda o: o.memset(kTa[i][:], 0.0), [], ["kTa"])
        c.op("pool", lambda o: o.memset(kTb[i][:], 0.0), [], ["kTb"])
    c.op("pool", lambda o: o.memset(Va[:], 0.0), [], ["Va"])
    c.op("pool", lambda o: o.memset(Vb[:], 0.0), [], ["Vb"])
    c.op("pool", lambda o: o.memset(Va[:, :, :, 64:65], 1.0), ["Va"], ["Va"])
    c.op("pool", lambda o: o.memset(Vb[:, :, :, 64:65], 1.0), ["Vb"], ["Vb"])
    K.qg = c.sb("qgs", [128, 640])
    K.sc10 = c.sb("sc10s", [128, 640])
    K.sink = c.sb("sinks", [128, 8])
    mk = c.sb("masksf", [128, 2, 128])
    mkb = c.sb("masksb", [128, 2, 128], BF16)
    K.maskLO = mkb[:, 0, :]
    K.maskUP = mkb[:, 1, :]
    c.dma(K.qg[:], K.qg_in, [], ["qg"])
    c.dma(K.sc10[:], K.sc10_in, [], ["sc10"])
    c.dma(K.sink[64:65, :], K.sink_in, [], ["sink"])
    c.dma(mk[:], K.masks_in, [], ["masksf"])
    c.cp("dve", mkb[:], mk[:], ["masksf"], ["masks"])
    c.push()
    winb = c.sb("winb", [128, 8, 1536], BF16)
    wst = [c.sb("wst%d" % i, [128, 1536]) for i in range(2)]
    for ch in range(8):
        c.dma(wst[ch % 2][:], K.att_w_in[ch * 128:(ch + 1) * 128, :], [], ["wst%d" % (ch % 2)], e="sp" if ch % 2 else "pool")
        c.cp("dve" if ch % 2 else "pool", winb[:, ch, :], wst[ch % 2][:], ["wst%d" % (ch % 2)], ["winb"])
    ptr = c.ps("ptr", [128, 1024], BF16)
    W = alloc_normwork(c, ptr)
    pp = [c.ps("pproj%d" % i, [128, 512]) for i in range(3)]
    ptq = c.ps("ptq", [128, 1280], BF16)
    aT = [c.sb("aT%d" % i, [128, 8, 128], BF16) for i in range(2)]
    pj = c.sb("pj", [128, 1536])
    G10 = c.sb("G10", [128, 640])
    c.tt("dve", G10[:], K.qg[:], K.sc10[:], ALU.mult, ["qg", "sc10"], ["G10"])
    sq = c.sb("sq", [128, 640])
    st10 = c.sb("st10", [128, 32])
    X1 = c.sb("X1", [128, 640])
    X2 = c.sb("X2", [128, 640])
    R1 = c.sb("R1", [128, 640])
    R2 = c.sb("R2", [128, 640])
    cosf = [c.sb("cosf%d" % i, [128, 64]) for i in range(2)]
    sinf = [c.sb("sinf%d" % i, [128, 64]) for i in range(2)]
    ST = c.sb("ST", [128, 1280], BF16)
    qst = [c.sb("qst%d" % i, [128, 10, 128], BF16) for i in range(2)]
    for t in range(NT):
        m = 0 if t < NTL else 1
        src, sk = src_rows(K, layer, t)
        a, ak = aT[t % 2], "aT%d" % (t % 2)
        emit_norm_T(c, K, W, src, sk, m, 0, a[:], ak)
        for nb in range(3):
            for ch in range(8):
                c.mm(pp[nb][:, :], a[:, ch, :], winb[:, ch, nb * 512:(nb + 1) * 512], ch == 0, ch == 7, [ak, "winb"], ["pproj%d" % nb])
        for nb in range(3):
            c.cp("act", pj[:, nb * 512:(nb + 1) * 512], pp[nb][:, :], ["pproj%d" % nb], ["pj"])
        c.cp("act", Va[:, t, :, 0:64], pj[:, 640:768].rearrange("p (k d) -> p k d", k=2), ["pj"], ["Va"])
        c.cp("act", Vb[:, t, :, 0:64], pj[:, 1408:1536].rearrange("p (k d) -> p k d", k=2), ["pj"], ["Vb"])
        c.tt("dve", sq[:], pj[:, 0:640], pj[:, 0:640], ALU.mult, ["pj"], ["sq"])
        c.op("dve", lambda o: o.tensor_reduce(out=st10[:, 0:10], in_=sq[:].rearrange("p (h d) -> p h d", h=10), axis=AX.X, op=ALU.add), ["sq"], ["st10a"])
        c.ts("dve", st10[:, 10:20], st10[:, 0:10], 1.0 / 64, EPS, ALU.mult, ALU.add, ["st10a"], ["st10b"])
        c.act(st10[:, 10:20], st10[:, 10:20], AF.Sqrt, ["st10b"], ["st10b"])
        c.op("dve", lambda o: o.reciprocal(out=st10[:, 20:30], in_=st10[:, 10:20]), ["st10b"], ["st10c"])
        c.tt("dve", X1[:].rearrange("p (h d) -> p h d", h=10), pj[:, 0:640].rearrange("p (h d) -> p h d", h=10),
             st10[:, 20:30].unsqueeze(2).to_broadcast([128, 10, 64]), ALU.mult, ["pj", "st10c"], ["X1"])
        c.tt("dve", X1[:], X1[:], G10[:], ALU.mult, ["X1", "G10"], ["X1"])
        c.tt("dve", X2[:], pj[:, 768:1408], K.sc10[:], ALU.mult, ["pj", "sc10"], ["X2"])
        if t < NTL:
            cf, sf = cosf[t % 2], sinf[t % 2]
            c.dma(cf[:], K.cosf[t * 128:(t + 1) * 128, :], [], ["cosf%d" % (t % 2)])
            c.dma(sf[:], K.sinf[t * 128:(t + 1) * 128, :], [], ["sinf%d" % (t % 2)])
        for bi, (X, xk) in enumerate(((X1, "X1"), (X2, "X2"))):
            if t < NTL:
                ck, sk2 = "cosf%d" % (t % 2), "sinf%d" % (t % 2)
                e1 = "dve"
                c.tt(e1, R1[:].rearrange("p (h d) -> p h d", h=10), X[:].rearrange("p (h d) -> p h d", h=10),
                     cf[:].unsqueeze(1).to_broadcast([128, 10, 64]), ALU.mult, [xk, ck], ["R1"])
                X5 = X[:].rearrange("p (h g s f) -> p h g s f", h=10, g=2, s=2, f=16)
                R5 = R2[:].rearrange("p (h g s f) -> p h g s f", h=10, g=2, s=2, f=16)
                S4 = sf[:].rearrange("p (g s f) -> p g s f", g=2, s=2, f=16)
                for s in range(2):
                    c.tt(e1, R5[:, :, :, s, :], X5[:, :, :, 1 - s, :], S4[:, :, s, :].unsqueeze(1).to_broadcast([128, 10, 2, 16]),
                         ALU.mult, [xk, sk2], ["R2"])
                srcq0, srcq1, rk = R1, R2, ["R1", "R2"]
            else:
                srcq0, srcq1, rk = X, None, [xk]
            b0 = bi * 5 * 128
            qout = ST[:, b0:b0 + 512].rearrange("p (pp half d) -> p half pp d", pp=4, half=2, d=64)
            kout = ST[:, b0 + 512:b0 + 640]
            qi0 = srcq0[:, 0:512].rearrange("p (half pp d) -> p half pp d", half=2, pp=4, d=64)
            if srcq1 is not None:
                qi1 = srcq1[:, 0:512].rearrange("p (half pp d) -> p half pp d", half=2, pp=4, d=64)
                c.tt("dve", qout, qi0, qi1, ALU.add, rk, ["ST"])
                c.tt("dve", kout, srcq0[:, 512:640], srcq1[:, 512:640], ALU.add, rk, ["ST"])
            else:
                c.cp("dve", qout, qi0, rk, ["ST"])
                c.cp("act", kout, srcq0[:, 512:640], rk, ["ST"])
        for blk in range(10):
            c.tr(ptq[:, blk * 128:(blk + 1) * 128], ST[:, blk * 128:(blk + 1) * 128], K.identb[:], ["ST", "identb"], ["ptq"])
        qs, qk_ = qst[t % 2], "qst%d" % (t % 2)
        c.cp("act", qs[:].rearrange("p b t -> p (b t)"), ptq[:, :], ["ptq"], [qk_])
        for hf in range(2):
            c.cp("act", kTa[hf][hf * 64:(hf + 1) * 64, t * 128:(t + 1) * 128], qs[hf * 64:(hf + 1) * 64, 4, :], [qk_], ["kTa"])
            c.cp("act", kTb[hf][hf * 64:(hf + 1) * 64, t * 128:(t + 1) * 128], qs[hf * 64:(hf + 1) * 64, 9, :], [qk_], ["kTb"])
        c.dma(K.QTA[:, :, t * 128:(t + 1) * 128], qs[:, 0:4, :], [qk_], ["QTA"], e="sp")
        c.dma(K.QTB[:, :, t * 128:(t + 1) * 128], qs[:, 5:9, :], [qk_], ["QTB"], e="sp")
    c.pop()
    c.push()
    woutb = c.sb("woutb", [64, 16, 1024], BF16)
    c.push()
    wst2 = [c.sb("wst2_%d" % i, [64, 4, 1024]) for i in range(2)]
    wo = K.att_w_out.rearrange("(hh d) n -> d hh n", d=64)
    for g in range(4):
        c.dma(wst2[g % 2][:], wo[:, g * 4:(g + 1) * 4, :], [], ["wst2_%d" % (g % 2)], e="sp" if g % 2 else "pool")
        c.cp("dve" if g % 2 else "pool", woutb[:, g * 4:(g + 1) * 4, :], wst2[g % 2][:], ["wst2_%d" % (g % 2)], ["woutb"])
    c.pop()
    pS = [c.ps("pS%d" % i, [128, 512]) for i in range(2)]
    pO = [c.ps("pO%d" % i, [128, 512]) for i in range(2)]
    pB = c.ps("pB", [128, 512])
    pY = c.ps("pY", [128, 1024])
    PT = [c.sb("PT%d" % i, [128, 512], BF16) for i in range(3)]
    OTs = c.sb("OTs", [128, 512])
    rr = c.sb("rr", [128, 512])
    mixT = c.sb("mixT", [64, 16, 512], BF16)
    qa_t = [c.sb("qa_t%d" % i, [128, 4, 512], BF16) for i in range(2)]
    qb_t = [c.sb("qb_t%d" % i, [128, 4, 512], BF16) for i in range(2)]
    esink = c.sb("esink", [128, 8])
    c.act(esink[64:65, :], K.sink[64:65, :], AF.Exp, ["sink"], ["esink"])
    W = Prog()
    W.n = 0
    W.hbuf = [c.sb("hbuf%d" % i, [128, 1024]) for i in range(2)]
    W.tmpf = c.sb("tmpf", [128, 1024])
    cnt = Prog()
    cnt.s = 0
    cnt.o = 0

    def finalize(po, pok, NQ, mix_out, sink_ap=None):
        if sink_ap is not None:
            c.tt("dve", rr[64:65, 0:NQ].rearrange("p (h q) -> p h q", h=4), po[64:65, 0:NQ].rearrange("p (h q) -> p h q", h=4),
                 sink_ap, ALU.add, [pok, "esink"], ["rr"])
            c.op("dve", lambda o: o.reciprocal(out=rr[64:65, 0:NQ], in_=rr[64:65, 0:NQ]), ["rr"], ["rr"])
        else:
            c.op("dve", lambda o: o.reciprocal(out=rr[64:65, 0:NQ], in_=po[64:65, 0:NQ]), [pok], ["rr"])
        c.mm(pB[0:64, 0:NQ], K.onesf[64:65, 0:64], rr[64:65, 0:NQ], True, True, ["rr", "onesf"], ["pB"])
        c.cp("act", OTs[0:64, 0:NQ], po[0:64, 0:NQ], [pok], ["OTs"])
        c.tt("dve", mix_out, OTs[0:64, 0:NQ] if len(mix_out.shape) == 2 else OTs[0:64, 0:NQ].rearrange("p (h q) -> p h q", h=4),
             pB[0:64, 0:NQ] if len(mix_out.shape) == 2 else pB[0:64, 0:NQ].rearrange("p (h q) -> p h q", h=4),
             ALU.mult, ["OTs", "pB"], ["mixT"])

    qtiles = [(i * 512, 512) for i in range(8)] + [(L, 256)]
    for qi, (q0, NQ) in enumerate(qtiles):
        is_ctx = q0 >= L
        qa, qak = qa_t[qi % 2], "qa_t%d" % (qi % 2)
        qb, qbk = qb_t[qi % 2], "qb_t%d" % (qi % 2)
        c.dma(qa[:, :, 0:NQ], K.QTA[:, :, q0:q0 + NQ], ["QTA"], [qak], e="sp")
        c.dma(qb[:, :, 0:NQ], K.QTB[:, :, q0:q0 + NQ], ["QTB"], [qbk], e="pool")
        ktiles = [32, 33] if is_ctx else list(range(NT))
        for half in range(2):
            ps_ = slice(half * 64, (half + 1) * 64)
            for pq in range(4):
                h = half * 4 + pq
                po, pok = pO[cnt.o % 2], "pO%d" % (cnt.o % 2)
                cnt.o += 1
                def S_a(ki):
                    kt = ktiles[ki]
                    n_ = cnt.s + ki
                    c.mm(pS[n_ % 2][:, 0:NQ], kTa[half][:, kt * 128:(kt + 1) * 128], qa[:, pq, 0:NQ], True, True, ["kTa", qak], ["pS%d" % (n_ % 2)])

                S_a(0)
                for ki, kt in enumerate(ktiles):
                    n_ = cnt.s + ki
                    s_, sk_ = pS[n_ % 2], "pS%d" % (n_ % 2)
                    p_, pk_ = PT[n_ % 3], "PT%d" % (n_ % 3)
                    if ki + 1 < len(ktiles):
                        S_a(ki + 1)
                    c.act(p_[:, 0:NQ], s_[:, 0:NQ], AF.Exp, [sk_], [pk_])
                    c.mm(po[:, 0:NQ], Va[:, kt, half, :], p_[:, 0:NQ], ki == 0, ki == len(ktiles) - 1, ["Va", pk_], [pok])
                cnt.s += len(ktiles)
                finalize(po, pok, NQ, mixT[:, h, 0:NQ])
        for jb in range(NQ // 128):
            j = q0 // 128 + jb
            if is_ctx:
                kl = [(32, None), (33, None)]
            else:
                kl = [(32, None), (33, None)]
                if j - 1 >= 0:
                    kl.append((j - 1, "LO"))
                kl.append((j, None))
                if j + 1 < NTL:
                    kl.append((j + 1, "UP"))
            for half in range(2):
                ps_ = slice(half * 64, (half + 1) * 64)
                po, pok = pO[cnt.o % 2], "pO%d" % (cnt.o % 2)
                cnt.o += 1
                def S_b(ki):
                    kt = kl[ki][0]
                    n_ = cnt.s + ki
                    c.mm(pS[n_ % 2][:, :].rearrange("p (h q) -> p h q", h=4), kTb[half][:, kt * 128:(kt + 1) * 128], qb[:, :, jb * 128:(jb + 1) * 128],
                         True, True, ["kTb", qbk], ["pS%d" % (n_ % 2)])

                S_b(0)
                for ki, (kt, msk) in enumerate(kl):
                    n_ = cnt.s + ki
                    s_, sk_ = pS[n_ % 2], "pS%d" % (n_ % 2)
                    p_, pk_ = PT[n_ % 3], "PT%d" % (n_ % 3)
                    if ki + 1 < len(kl):
                        S_b(ki + 1)
                    c.act(p_[:, :], s_[:, :], AF.Exp, [sk_], [pk_])
                    if msk is not None:
                        mt = K.maskLO if msk == "LO" else K.maskUP
                        c.tt("pool", p_[:, :].rearrange("p (h q) -> p h q", h=4), p_[:, :].rearrange("p (h q) -> p h q", h=4),
                             mt[:].unsqueeze(1).to_broadcast([128, 4, 128]), ALU.mult, [pk_, "masks"], [pk_])
                    c.mm(po[:, :], Vb[:, kt, half, :], p_[:, :], ki == 0, ki == len(kl) - 1, ["Vb", pk_], [pok])
                cnt.s += len(kl)
                finalize(po, pok, 512, mixT[:, 8 + half * 4: 8 + half * 4 + 4, jb * 128:(jb + 1) * 128],
                         sink_ap=esink[64:65, half * 4:half * 4 + 4].unsqueeze(2).to_broadcast([1, 4, 128]))
        for tt_ in range(NQ // 128):
            t = q0 // 128 + tt_
            m = 1 if is_ctx else 0
            for nb in range(2):
                for hh in range(16):
                    c.mm(pY[:, nb * 512:(nb + 1) * 512], mixT[:, hh, tt_ * 128:(tt_ + 1) * 128], woutb[:, hh, nb * 512:(nb + 1) * 512],
                         hh == 0, hh == 15, ["mixT", "woutb"], ["pY"])
            emit_residual(c, K, W, src_rows(K, layer, t), pY[:, :], ["pY"], m, 0, t)
    c.pop()
    c.pop()


def emit_peer_prep(c, K, layer):
    c.push()
    ust = [c.sb("ust%d" % i, [128, 1024]) for i in range(2)]
    vst = [c.sb("vst%d" % i, [128, 1024]) for i in range(2)]
    ub = [c.sb("ub%d" % i, [128, 1024], BF16) for i in range(2)]
    vb = [c.sb("vb%d" % i, [128, 1024], BF16) for i in range(2)]
    uT = [c.sb("uTs%d" % i, [128, 1024], BF16) for i in range(2)]
    pu = [c.ps("pu%d" % i, [128, 1024], BF16) for i in range(2)]
    for i in range(128):
        k = i % 2
        c.dma(ust[k][:], K.peer_u[layer, i * 128:(i + 1) * 128, :], [], ["ust%d" % k], e="sp")
        c.dma(vst[k][:], K.peer_v[layer, i * 128:(i + 1) * 128, :], [], ["vst%d" % k], e="pool")
        c.cp("dve", ub[k][:], ust[k][:], ["ust%d" % k], ["ub%d" % k])
        c.cp("pool", vb[k][:], vst[k][:], ["vst%d" % k], ["vb%d" % k])
        for ch in range(8):
            c.tr(pu[k][:, ch * 128:(ch + 1) * 128], ub[k][:, ch * 128:(ch + 1) * 128], K.identb[:], ["ub%d" % k, "identb"], ["pu%d" % k])
        c.cp("act", uT[k][:], pu[k][:], ["pu%d" % k], ["uTs%d" % k])
        c.dma(K.UT[i], uT[k][:], ["uTs%d" % k], ["UT"], e="sp")
        c.dma(K.VB[i], vb[k][:], ["vb%d" % k], ["VB"], e="pool")
    c.pop()


def emit_peer(c, K, layer, ntiles):
    emit_peer_prep(c, K, layer)
    c.push()
    GT = 2
    wqb = c.sb("wqb", [128, 8, 2048], BF16)
    wst = [c.sb("wqst%d" % i, [128, 2048]) for i in range(2)]
    for ch in range(8):
        c.dma(wst[ch % 2][:], K.peer_w_q[layer, ch * 128:(ch + 1) * 128, :], [], ["wqst%d" % (ch % 2)], e="sp" if ch % 2 else "pool")
        c.cp("dve" if ch % 2 else "pool", wqb[:, ch, :], wst[ch % 2][:], ["wqst%d" % (ch % 2)], ["wqb"])
    skf = c.sb("skf", [128, 2, 128])
    skb = c.sb("skb", [128, 2, 128], BF16)
    skT = c.sb("skT", [128, 2, 128], BF16)
    Bk = [c.ps("bank%d" % i, [128, 512]) for i in range(8)]
    Bkb = [b[:].bitcast(BF16) for b in Bk]
    for p in range(2):
        c.dma(skf[:, p, :], K.peer_sk[layer, p], [], ["skf"])
    c.cp("dve", skb[:], skf[:], ["skf"], ["skb"])
    for p in range(2):
        c.tr(Bkb[7][:, p * 128:(p + 1) * 128], skb[:, p, :], K.identb[:], ["skb", "identb"], ["bank7"])
    c.cp("dve", skT[:].rearrange("p a k -> p (a k)"), Bkb[7][:, 0:256], ["bank7"], ["skT"])

    W = alloc_normwork(c, Bkb[5])
    W.ptrkey = "bank5"
    xTg = c.sb("xTg", [128, 8, GT * 128], BF16)
    qb16 = c.sb("qb16", [128, 1024], BF16)
    qT = c.sb("qT", [128, 8, 128], BF16)
    S = [c.sb("S%d" % i, [128, 16, 128]) for i in range(GT)]
    Bb = [c.sb("Bb%d" % i, [128, 8, 128]) for i in range(GT)]
    TH = [c.sb("TH%d" % i, [128, 8, 128]) for i in range(GT)]
    SV = c.sb("SV", [128, 16, 16])
    wk = c.sb("wk", [128, 256])
    cand = c.sb("cand", [128, 8, 256])
    CV = c.sb("CV", [128, 8, 16])
    ez = c.sb("ez", [128, 8, 16])
    st8 = c.sb("st8", [128, 40])
    NB = 3
    tmpr = [c.sb("tmpr%d" % i, [128, 8, 128], BF16) for i in range(NB)]
    msk = [c.sb("msk%d" % i, [128, 8, 128], BF16) for i in range(NB)]
    tm2 = [c.sb("tm2_%d" % i, [128, 8, 128], BF16) for i in range(NB)]
    E1 = [c.sb("E1_%d" % i, [128, 8, 128], BF16) for i in range(GT)]
    E2Z = [c.sb("E2Z_%d" % i, [128, 8, 128], BF16) for i in range(GT)]
    uTi = [c.sb("uTi%d" % i, [128, 8, 128], BF16) for i in range(3)]
    vi = [c.sb("vi%d" % i, [128, 1024], BF16) for i in range(3)]
    gl = [c.sb("gl%d" % i, [128, GT * 128], BF16) for i in range(2)]
    wT = [c.sb("wT%d" % i, [128, GT * 128], BF16) for i in range(2)]
    ring = Prog()
    ring.n = 0
    ring.u = 0
    ngroups = ntiles // GT
    for g in range(ngroups):
        tiles = [g * GT + i for i in range(GT)]
        for tl, t in enumerate(tiles):
            m = 0 if t < NTL else 1
            emit_norm_T_peer(c, K, W, hrows(K, t), "H%d" % t, m, xTg[:, :, tl * 128:(tl + 1) * 128], "xTg")
            for hh in range(2):
                for nb in range(2):
                    for ch in range(8):
                        c.mm(Bk[nb][:, :], xTg[:, ch, tl * 128:(tl + 1) * 128], wqb[:, ch, hh * 1024 + nb * 512: hh * 1024 + (nb + 1) * 512],
                             ch == 0, ch == 7, ["xTg", "wqb"], ["bank%d" % nb])
                for nb in range(2):
                    c.cp("act", qb16[:, nb * 512:(nb + 1) * 512], Bk[nb][:, :], ["bank%d" % nb], ["qb16"])
                for b in range(8):
                    c.tr(Bkb[2][:, b * 128:(b + 1) * 128], qb16[:, b * 128:(b + 1) * 128], K.identb[:], ["qb16", "identb"], ["bank2"])
                c.cp("dve", qT[:].rearrange("p b t -> p (b t)"), Bkb[2][:, :], ["bank2"], ["qT"])
                for b in range(8):
                    bank = 3 + b // 4
                    c.mm(Bk[bank][:, (b % 4) * 128:(b % 4 + 1) * 128], qT[:, b, :], skT[:, b % 2, :], True, True, ["qT", "skT"], ["bank%d" % bank])
                for bb in range(2):
                    c.cp("act", S[tl][:, hh * 8 + bb * 4: hh * 8 + (bb + 1) * 4, :].rearrange("p b k -> p (b k)"), Bk[3 + bb][:, :],
                         ["bank%d" % (3 + bb)], ["S%d" % tl])
            Sk = "S%d" % tl
            for hp in range(16):
                c.op("dve", lambda o: o.max(out=SV[:, hp, 0:8], in_=S[tl][:, hp, :]), [Sk], ["SV"])
                c.op("dve", lambda o: o.match_replace(out=wk[:, 0:128], in_to_replace=SV[:, hp, 0:8], in_values=S[tl][:, hp, :], imm_value=-1e30),
                     [Sk, "SV"], ["wk"])
                c.op("dve", lambda o: o.max(out=SV[:, hp, 8:16], in_=wk[:, 0:128]), ["wk"], ["SV"])
            SV8 = SV[:].rearrange("p (h two) a -> p h two a", h=8, two=2)
            c.tt("dve", cand[:].rearrange("p h (a b) -> p h a b", a=16), SV8[:, :, 0, :].unsqueeze(3).to_broadcast([128, 8, 16, 16]),
                 SV8[:, :, 1, :].unsqueeze(2).to_broadcast([128, 8, 16, 16]), ALU.add, ["SV"], ["cand"])
            for h in range(8):
                c.op("dve", lambda o: o.max(out=CV[:, h, 0:8], in_=cand[:, h, :]), ["cand"], ["CV"])
                c.op("dve", lambda o: o.match_replace(out=wk[:, :], in_to_replace=CV[:, h, 0:8], in_values=cand[:, h, :], imm_value=-1e30),
                     ["cand", "CV"], ["wk"])
                c.op("dve", lambda o: o.max(out=CV[:, h, 8:16], in_=wk[:, :]), ["wk"], ["CV"])
            c.tt("dve", ez[:], CV[:], CV[:, :, 0:1].to_broadcast([128, 8, 16]), ALU.subtract, ["CV"], ["ez"])
            c.act(ez[:].rearrange("p h a -> p (h a)"), ez[:].rearrange("p h a -> p (h a)"), AF.Exp, ["ez"], ["ez"])
            c.op("dve", lambda o: o.tensor_reduce(out=st8[:, 0:8], in_=ez[:], axis=AX.X, op=ALU.add), ["ez"], ["st8a"])
            c.act(st8[:, 8:16], st8[:, 0:8], AF.Ln, ["st8a"], ["st8b"])
            c.tt("dve", st8[:, 16:24], st8[:, 8:16], SV8[:, :, 1, 0], ALU.add, ["st8b", "SV"], ["st8c"])
            c.ts("dve", st8[:, 24:32], CV[:, :, 15], -1e-5, None, ALU.add, None, ["CV"], ["st8d"])
            S8 = S[tl][:].rearrange("p (h two) k -> p h two k", h=8, two=2)
            Bt, Btk = Bb[tl], "Bb%d" % tl
            c.tt("dve", Bt[:], S8[:, :, 0, :], SV8[:, :, 0, 0:1].to_broadcast([128, 8, 128]), ALU.subtract, [Sk, "SV"], [Btk])
            c.act(E1[tl][:].rearrange("p h k -> p (h k)"), Bt[:].rearrange("p h k -> p (h k)"), AF.Exp, [Btk], ["E1_%d" % tl])
            c.tt("dve", Bt[:], S8[:, :, 1, :], st8[:, 16:24].unsqueeze(2).to_broadcast([128, 8, 128]), ALU.subtract, [Sk, "st8c"], [Btk])
            c.act(E2Z[tl][:].rearrange("p h k -> p (h k)"), Bt[:].rearrange("p h k -> p (h k)"), AF.Exp, [Btk], ["E2Z_%d" % tl])
            c.tt("dve", TH[tl][:], st8[:, 24:32].unsqueeze(2).to_broadcast([128, 8, 128]), S8[:, :, 0, :], ALU.subtract, [Sk, "st8d"], ["TH%d" % tl])
        st_ = {}

        def issue_pre(i):
            u_, uk = uTi[i % 3], "uTi%d" % (i % 3)
            v_, vk = vi[i % 3], "vi%d" % (i % 3)
            c.dma(u_[:].rearrange("p c j -> p (c j)"), K.UT[i], ["UT"], [uk], e="sp")
            c.dma(v_[:], K.VB[i], ["VB"], [vk], e="sp")
            pre, prek = Bk[4 + i % 2], "bank%d" % (4 + i % 2)
            for ch in range(8):
                c.mm(pre[:, 0:GT * 128], u_[:, ch, :], xTg[:, ch, :], ch == 0, ch == 7, [uk, "xTg"], [prek])

        def issue_gelu(i):
            pre, prek = Bk[4 + i % 2], "bank%d" % (4 + i % 2)
            c.act(gl[i % 2][:], pre[:, 0:GT * 128], AF.Gelu, [prek], ["gl%d" % (i % 2)])

        def issue_w(i):
            gtb, gtk = Bk[6 + i % 2], "bank%d" % (6 + i % 2)
            c.tt("dve", wT[i % 2][:], gtb[:, 0:GT * 128], gl[i % 2][:], ALU.mult, [gtk, "gl%d" % (i % 2)], ["wT%d" % (i % 2)])

        def issue_y(i):
            v_, vk = vi[i % 3], "vi%d" % (i % 3)
            for tl in range(GT):
                for nb in range(2):
                    c.mm(Bk[tl * 2 + nb][:, :], wT[i % 2][:, tl * 128:(tl + 1) * 128], v_[:, nb * 512:(nb + 1) * 512], i == 0, i == 127,
                         ["wT%d" % (i % 2), vk], ["bank%d" % (tl * 2 + nb)])

        issue_pre(0)
        issue_gelu(0)
        for i in range(128):
            if i + 1 < 128:
                issue_pre(i + 1)
            gtb, gtk = Bk[6 + i % 2], "bank%d" % (6 + i % 2)
            for tl in range(GT):
                S8 = S[tl][:].rearrange("p (h two) k -> p h two k", h=8, two=2)
                k = ring.n % NB
                ring.n += 1
                c.tt("dve", tmpr[k][:], S8[:, :, 1, :], TH[tl][:, :, i:i + 1].to_broadcast([128, 8, 128]), ALU.is_ge,
                     ["S%d" % tl, "TH%d" % tl], ["tmpr%d" % k])
                c.tt("dve", tm2[k][:].rearrange("p h k -> p (h k)"), tmpr[k][:].rearrange("p h k -> p (h k)"), E2Z[tl][:].rearrange("p h k -> p (h k)"),
                     ALU.mult, ["tmpr%d" % k, "E2Z_%d" % tl], ["tm2_%d" % k])
                c.tt("pool", msk[k][:], tm2[k][:], E1[tl][:, :, i:i + 1].to_broadcast([128, 8, 128]), ALU.mult, ["tm2_%d" % k, "E1_%d" % tl], ["msk%d" % k])
                for h in range(8):
                    c.mm(gtb[:, tl * 128:(tl + 1) * 128], msk[k][:, h, :], K.identb[:], h == 0, h == 7, ["msk%d" % k, "identb"], [gtk])
            if i >= 1:
                issue_w(i - 1)
                issue_y(i - 1)
            if i + 1 < 128:
                issue_gelu(i + 1)
        issue_w(127)
        issue_y(127)
        for tl, t in enumerate(tiles):
            m = 0 if t < NTL else 1
            emit_residual_banks(c, K, W, t, Bk[tl * 2], Bk[tl * 2 + 1], ["bank%d" % (tl * 2), "bank%d" % (tl * 2 + 1)], m)
    c.pop()


def emit_norm_T_peer(c, K, W, src, srckey, m, aT_out, aTkey):
    W.n += 1
    i = W.n % 2
    hb, hk = W.hbuf[i], "hbuf%d" % i
    c.dma(hb[:], src, [srckey], [hk], e="sp" if W.n % 2 else "pool")
    c.tt("pool", W.junk[:], hb[:], hb[:], ALU.mult, [hk], ["junk"])
    c.op("dve", lambda o: o.tensor_reduce(out=W.ss[:, 0:1], in_=W.junk[:], axis=AX.X, op=ALU.add), ["junk"], ["ss"])
    c.ts("dve", W.ss[:, 1:2], W.ss[:, 0:1], 1.0 / D, EPS, ALU.mult, ALU.add, ["ss"], ["ss1"])
    c.act(W.ss[:, 3:4], W.ss[:, 1:2], AF.Sqrt, ["ss1"], ["ss3"])
    c.op("dve", lambda o: o.reciprocal(out=W.ss[:, 2:3], in_=W.ss[:, 3:4]), ["ss3"], ["ss2"])
    c.ts("dve", W.hn[:], hb[:], W.ss[:, 2:3], None, ALU.mult, None, [hk, "ss2"], ["hn"])
    for ch in range(8):
        c.tr(W.ptr[:, ch * 128:(ch + 1) * 128], W.hn[:, ch * 128:(ch + 1) * 128], K.identb[:], ["hn", "identb"], [W.ptrkey])
    pv = W.ptr[:, 0:1024].rearrange("p (c t) -> p c t", c=8)
    sc = K.scol[m][1][:].unsqueeze(2).to_broadcast([128, 8, 128])
    sh = K.shcol[m][1][:].unsqueeze(2).to_broadcast([128, 8, 128])
    c.tt("dve", W.tmpf[:].rearrange("p (c t) -> p c t", c=8), pv, sc, ALU.mult, [W.ptrkey, "scol%d1" % m], ["tmpf"])
    c.tt("pool", aT_out, W.tmpf[:].rearrange("p (c t) -> p c t", c=8), sh, ALU.add, ["tmpf", "shcol%d1" % m], [aTkey])


def emit_residual_banks(c, K, W, t, b0, b1, keys, m):
    W.n += 1
    i = W.n % 2
    hb, hk = W.hbuf[i], "hbuf%d" % i
    c.dma(hb[:], hrows(K, t), ["H%d" % t], [hk], e="sp" if W.n % 2 else "pool")
    g = K.gbc[m][1]
    c.tt("dve", W.tmpf[:, 0:512], b0[:, :], g[:, 0:512], ALU.mult, [keys[0], "gbc%d1" % m], ["tmpf"])
    c.tt("dve", W.tmpf[:, 512:1024], b1[:, :], g[:, 512:1024], ALU.mult, [keys[1], "gbc%d1" % m], ["tmpf"])
    c.tt("pool", hb[:], hb[:], W.tmpf[:], ALU.add, [hk, "tmpf"], [hk])
    c.dma(hrows(K, t), hb[:], [hk], ["H%d" % t], e="sp")


def emit_final(c, K):
    c.push()
    hb = [c.sb("fh%d" % i, [128, 1024]) for i in range(2)]
    junk = c.sb("fjunk", [128, 1024])
    ss = c.sb("fss", [128, 4])
    K.fg = c.sb("fgs", [128, 1024])
    c.dma(K.fg[:], K.fg_in, [], ["fg"])
    for t in range(NTL):
        h, hk = hb[t % 2], "fh%d" % (t % 2)
        c.dma(h[:], hrows(K, t), ["H%d" % t], [hk], e="sp" if t % 2 else "pool")
        c.tt("pool", junk[:], h[:], h[:], ALU.mult, [hk], ["fjunk"])
        c.op("dve", lambda o: o.tensor_reduce(out=ss[:, 0:1], in_=junk[:], axis=AX.X, op=ALU.add), ["fjunk"], ["fss"])
        c.ts("dve", ss[:, 1:2], ss[:, 0:1], 1.0 / D, EPS, ALU.mult, ALU.add, ["fss"], ["fss1"])
        c.act(ss[:, 3:4], ss[:, 1:2], AF.Sqrt, ["fss1"], ["fss3"])
        c.op("dve", lambda o: o.reciprocal(out=ss[:, 2:3], in_=ss[:, 3:4]), ["fss3"], ["fss2"])
        c.op("dve", lambda o: o.scalar_tensor_tensor(out=h[:], in0=h[:], scalar=ss[:, 2:3], in1=K.fg[:], op0=ALU.mult, op1=ALU.mult),
             [hk, "fss2", "fg"], [hk])
        c.dma(K.out[t * 128:(t + 1) * 128, :], h[:], [hk], ["out"], e="sp")
    c.pop()


def build_program(stages=("mod0", "att", "peer0", "mod1", "rec", "peer1", "final"), dump_h=False):
    nc = bass.Bass("TRN2", target_bir_lowering=False)
    K = Prog()

    def IN(name, shape, dt=F32):
        return nc.dram_tensor(name, list(shape), dt, kind="ExternalInput").ap()

    K.x = IN("x", [L, D])
    K.ctx = IN("ctx", [LC, D])
    K.ccT = IN("ccT", [128, 8, 2])
    K.mod_w = IN("mod_w", [2, D, 6 * D])
    K.mod_b = IN("mod_b", [2, 6 * D])
    K.gcols = IN("gcols", [128, 2, 2, 8])
    K.att_w_in = IN("att_w_in", [D, 1536])
    K.att_w_out = IN("att_w_out", [D, D])
    K.qg_in = IN("qg", [128, 640])
    K.sc10_in = IN("sc10", [128, 640])
    K.sink_in = IN("sink", [1, 8])
    K.cosf = IN("cosf", [L, 64])
    K.sinf = IN("sinf", [L, 64])
    K.masks_in = IN("masks", [128, 2, 128])
    K.ident_in = IN("ident", [128, 128])
    K.sel_in = IN("sel", [2, 2, 128])
    K.peer_w_q = IN("peer_w_q", [2, D, 2048])
    K.peer_sk = IN("peer_sk", [2, 2, 128, 128])
    K.peer_u = IN("peer_u", [2, 16384, D])
    K.peer_v = IN("peer_v", [2, 16384, D])
    K.fg_in = IN("fg", [128, D])
    K.iotaj_in = IN("iotaj", [128, 128])
    K.iotaa_in = IN("iotaa", [128, 2048])
    K.rec_w_in = IN("rec_w_in", [D, 4624])
    K.rec_w_out = IN("rec_w_out", [D, D])
    K.recc_in = IN("recc", [128, 12, 128])
    K.dmt_in = IN("dmt", [128, 2, 4, 128])
    K.decs_in = IN("decs", [128, 2, 2, 4])
    K.outg_in = IN("outg", [128, 512])
    K.gng_in = IN("gng", [128, 512])
    K.convw_in = IN("convw", [128, 12, 5])
    K.alog_in = IN("alog", [128, 8])
    K.dtb_in = IN("dtb", [128, 8])
    K.cos2 = IN("cos2", [L, 128])
    K.sin2 = IN("sin2", [L, 128])
    K.out = nc.dram_tensor("out", [L, D], F32, kind="ExternalOutput").ap()

    c = Ctx(nc)
    K.H = c.dram("H", [NT * 128, D]) if not dump_h else nc.dram_tensor("H", [NT * 128, D], F32, kind="ExternalOutput").ap()
    K.QTA = c.dram("QTA", [128, 4, NT * 128], BF16)
    K.QTB = c.dram("QTB", [128, 4, NT * 128], BF16)
    K.UT = c.dram("UT", [128, 128, 1024], BF16)
    K.VB = c.dram("VB", [128, 128, 1024], BF16)
    K.AT = c.dram("AT", [128, 8, NT * 128], BF16)
    K.TK = c.dram("TK", [NT, 128, 3088])
    K.FT2 = c.dram("FT2", [NT, 128, 8, 128])
    K.FT = c.dram("FT", [12, 128, NT * 128])
    K.TM = c.dram("TM", [12, NT, 128, 128])
    for nm in ("PU", "PNW", "PQG", "PQK", "PKE", "RQK", "RQQ", "RKD", "OG", "OR"):
        setattr(K, nm, c.dram(nm, [2, NT, 128, 512]))
    K.PEG = c.dram("PEG", [2, NT, 128, 4])

    K.identf = c.sb("identf", [128, 128])
    K.identb = c.sb("identb", [128, 128], BF16)
    K.onesf = c.sb("onesf", [128, 64])
    K.sel = c.sb("sel", [2, 2, 128])
    K.scT = c.sb("scT", [128, 8, 2])
    gc = c.sb("gcol", [128, 2, 2, 8])
    K.g1col = gc[:, 0]
    K.g2col = gc[:, 1]
    K.scol = [[c.sb("scol%d%d" % (m, w), [128, 8]) for w in range(2)] for m in range(2)]
    K.shcol = [[c.sb("shcol%d%d" % (m, w), [128, 8]) for w in range(2)] for m in range(2)]
    K.gbc = [[c.sb("gbc%d%d" % (m, w), [128, 1024]) for w in range(2)] for m in range(2)]

    c.dma(K.identf[:], K.ident_in, [], ["identf"])
    c.cp("dve", K.identb[:], K.identf[:], ["identf"], ["identb"])
    c.op("pool", lambda o: o.memset(K.onesf[:], 1.0), [], ["onesf"])
    c.dma(K.sel[:], K.sel_in, [], ["sel"])
    c.dma(K.scT[:], K.ccT, [], ["scT"])
    c.act(K.scT[:].rearrange("p c m -> p (c m)"), K.scT[:].rearrange("p c m -> p (c m)"), AF.Silu, ["scT"], ["scT"])
    c.dma(gc[:], K.gcols, [], ["gcol"])

    if "loadH" in stages:
        Hin = IN("Hin", [NT * 128, D])
        for t in range(NT):
            c.dma(hrows(K, t), Hin[t * 128:(t + 1) * 128, :], [], ["H%d" % t], e="sp" if t % 2 else "pool")
    if "mod0" in stages:
        emit_modulation(c, K, 0)
    if "att" in stages:
        emit_attention(c, K)
    if "peer0" in stages:
        emit_peer4(c, K, 0, NT)
    if "mod1" in stages:
        emit_modulation(c, K, 1)
    if "rec" in stages:
        emit_rec(c, K)
    if "peer1" in stages:
        emit_peer4(c, K, 1, NTL)
    if "final" in stages:
        emit_final(c, K)
    c.barrier()
    keys = ["out"] + ["H%d" % t for t in range(NT)]
    for e in ("sp", "pool", "act"):
        c.finish(keys, e=e)
    c.close()
    K.ctxobj = c
    return nc, K


def rope_tables():
    quarter = 16
    freqs = (10000.0 ** (-np.arange(quarter, dtype=np.float32) / quarter)).astype(np.float32)
    t = np.arange(L)
    row = (t // 64).astype(np.float32)
    col = (t % 64).astype(np.float32)
    ar = row[:, None] * freqs[None, :]
    ac = col[:, None] * freqs[None, :]
    cosf = np.concatenate([np.cos(ar), np.cos(ar), np.cos(ac), np.cos(ac)], axis=1).astype(np.float32)
    sinf = np.concatenate([-np.sin(ar), np.sin(ar), -np.sin(ac), np.sin(ac)], axis=1).astype(np.float32)
    return cosf, sinf


def make_in_maps(inp, cores):
    f = lambda a: np.ascontiguousarray(np.asarray(a, dtype=np.float32))
    cosf, sinf = rope_tables()
    kq = np.arange(128)
    masks = np.stack([(kq[:, None] >= kq[None, :]), (kq[:, None] <= kq[None, :])], axis=1).astype(np.float32)
    sel = np.zeros((2, 2, 128), np.float32)
    sel[0, 0, :] = 1.0
    sel[1, 1, :] = 1.0
    sc10 = np.ones((128, 640), np.float32)
    sc10[:, :512] = 0.125
    g1 = f(inp["norm1_g"]).reshape(2, 8, 128)
    g2 = f(inp["norm2_g"]).reshape(2, 8, 128)
    gcols = np.ascontiguousarray(np.stack([g1, g2], axis=0).transpose(3, 0, 1, 2))
    qg = np.concatenate([np.tile(f(inp["att_q_norm"])[0], 8), np.tile(f(inp["att_k_norm"])[0], 2)])
    qg = np.ascontiguousarray(np.broadcast_to(qg[None, :], (128, 640)))
    fg = np.ascontiguousarray(np.broadcast_to(f(inp["final_norm_g"])[None, :], (128, D)))
    shared = {
        "mod_w": f(inp["mod_w"]), "mod_b": f(inp["mod_b"]), "gcols": gcols,
        "att_w_in": f(inp["att_w_in"])[0], "att_w_out": f(inp["att_w_out"])[0],
        "qg": qg, "sc10": sc10, "sink": f(inp["att_sink"]).reshape(1, 8),
        "cosf": cosf, "sinf": sinf, "masks": masks, "ident": np.eye(128, dtype=np.float32), "sel": sel,
        "peer_w_q": f(inp["peer_w_q"]), "peer_sk": f(inp["peer_sub_keys"]),
        "peer_u": f(inp["peer_u"]), "peer_v": f(inp["peer_v"]), "fg": fg,
    }
    recc, dmt, decs = rec_consts()
    cos2, sin2 = rope_tables2()
    rep = lambda v, n: np.ascontiguousarray(np.broadcast_to(np.asarray(v, np.float32).reshape(1, -1), (128, n)))
    shared.update({
        "rec_w_in": f(inp["rec_w_in"])[0], "rec_w_out": f(inp["rec_w_out"])[0],
        "recc": recc, "dmt": dmt, "decs": decs,
        "outg": rep(np.tile(f(inp["rec_out_norm"])[0], 4), 512), "gng": rep(f(inp["rec_gn_g"])[0], 512),
        "convw": np.ascontiguousarray(f(inp["rec_conv_w"])[0].reshape(5, 12, 128).transpose(2, 1, 0)),
        "alog": rep(f(inp["rec_a_log"])[0].reshape(8), 8), "dtb": rep(f(inp["rec_dt_bias"])[0].reshape(8), 8),
        "cos2": cos2, "sin2": sin2,
        "iotaj": np.ascontiguousarray(np.broadcast_to(np.arange(128, dtype=np.float32)[None, :], (128, 128))),
        "iotaa": np.ascontiguousarray(np.broadcast_to(np.tile(np.arange(16, dtype=np.float32), 128)[None, :], (128, 2048))),
    })
    maps = []
    for b in cores:
        cc = np.stack([f(inp["c"])[b], f(inp["c_ctx"])], axis=1)
        ccT = np.ascontiguousarray(cc.reshape(8, 128, 2).transpose(1, 0, 2))
        d = dict(shared)
        d["x"] = f(inp["x"])[b]
        d["ctx"] = f(inp["ctx"])[b]
        d["ccT"] = ccT
        maps.append(d)
    return maps


def kernel(**inputs):
    nc, K = build_program()
    maps = make_in_maps(inputs, list(range(8)))
    res = run_bass_kernel_spmd(nc, maps, core_ids=list(range(8)))
    out = np.stack([np.asarray(r["out"], dtype=np.float32) for r in res.results], axis=0)
    return out


RC_U, RC_NU, RC_NEG, RC_NEGT, RC_MD, RC_MO, RC_ONES, RC_NONES = 0, 2, 4, 6, 8, 9, 10, 11
GAMMAS = [1.0 - 2.0 ** (-(5.0 + h)) for h in range(4)]


def rec_consts():
    idx = np.arange(128)
    out = np.zeros((128, 12, 128), np.float32)
    for d in range(2):
        after = (idx[:, None] > idx[None, :]) if d == 0 else (idx[:, None] < idx[None, :])
        U = (after.T | np.eye(128, dtype=bool)).astype(np.float32)
        out[:, RC_U + d] = U
        out[:, RC_NU + d] = -U
        NEG = np.where(after, 0.0, -30000.0).astype(np.float32)
        out[:, RC_NEG + d] = NEG
        out[:, RC_NEGT + d] = NEG.T
    blk = (idx[:, None] // 64) == (idx[None, :] // 64)
    out[:, RC_MD] = blk.astype(np.float32)
    out[:, RC_MO] = (~blk).astype(np.float32)
    out[:, RC_ONES] = 1.0
    out[:, RC_NONES] = -1.0
    dmt = np.zeros((128, 2, 4, 128), np.float32)
    decs = np.zeros((128, 2, 2, 4), np.float32)
    for d in range(2):
        after = (idx[:, None] > idx[None, :]) if d == 0 else (idx[:, None] < idx[None, :])
        incl = after | np.eye(128, dtype=bool)
        dist = np.abs(idx[:, None] - idx[None, :]).astype(np.float64)
        pos = idx if d == 0 else 127 - idx
        for h in range(4):
            g = GAMMAS[h]
            m_is = np.where(incl, g ** dist, 0.0)
            dmt[:, d, h, :] = m_is.T
            decs[:, d, 0, h] = g ** (pos + 1.0)
            decs[:, d, 1, h] = g ** (127.0 - pos)
    return out, dmt, decs


def rope_tables2():
    quarter = 32
    freqs = (10000.0 ** (-np.arange(quarter, dtype=np.float32) / quarter)).astype(np.float32)
    t = np.arange(L)
    row = (t // 64).astype(np.float32)
    col = (t % 64).astype(np.float32)
    ar = row[:, None] * freqs[None, :]
    ac = col[:, None] * freqs[None, :]
    cos2 = np.concatenate([np.cos(ar), np.cos(ar), np.cos(ac), np.cos(ac)], axis=1).astype(np.float32)
    sin2 = np.concatenate([-np.sin(ar), np.sin(ar), -np.sin(ac), np.sin(ac)], axis=1).astype(np.float32)
    return cos2, sin2


def emit_rec_features(c, K):
    layer = 1
    c.push()
    c.push()
    wrb = c.sb("wrb", [128, 8, 3088], BF16)
    wst = [c.sb("wrst%d" % i, [128, 1544]) for i in range(2)]
    for ch in range(8):
        for hf in range(2):
            c.dma(wst[hf][:], K.rec_w_in[ch * 128:(ch + 1) * 128, 1536 + hf * 1544:1536 + (hf + 1) * 1544], [], ["wrst%d" % hf], e="sp" if hf else "pool")
            c.cp("dve" if hf else "pool", wrb[:, ch, hf * 1544:(hf + 1) * 1544], wst[hf][:], ["wrst%d" % hf], ["wrb"])
    aTt = [c.sb("aTt%d" % i, [128, 8, 128], BF16) for i in range(2)]
    bk = [c.ps("rb%d" % i, [128, 512]) for i in range(7)]
    ptr = c.ps("ptr", [128, 1024], BF16)
    W = alloc_normwork(c, ptr)
    TKb = [c.sb("TKb%d" % i, [128, 3088]) for i in range(2)]
    Xb = c.sb("Xb", [128, 1024])
    r1 = c.sb("rr1", [128, 1024])
    r2 = c.sb("rr2", [128, 1024])
    cos2 = [c.sb("cos2_%d" % i, [128, 128]) for i in range(2)]
    sin2 = [c.sb("sin2_%d" % i, [128, 128]) for i in range(2)]
    gv = c.sb("gv", [128, 32])
    negA = c.sb("negA", [128, 8])
    ftb = [c.sb("ftb%d" % i, [128, 8, 128]) for i in range(2)]
    c.act(negA[:], K.alog[:], AF.Exp, ["alog"], ["negA"])
    c.ts("dve", negA[:], negA[:], -1.0, None, ALU.mult, None, ["negA"], ["negA"])
    segs = [(0, 512, 0), (512, 16, 1), (528, 512, 2), (1040, 512, 3), (1552, 512, 4), (2064, 512, 5), (2576, 512, 6)]
    for t in range(NT):
        m = 0 if t < NTL else 1
        aT_, atk = aTt[t % 2], "aTt%d" % (t % 2)
        emit_norm_T(c, K, W, hrows(K, t), "H%d" % t, m, 0, aT_[:], atk)
        c.dma(K.AT[:, :, t * 128:(t + 1) * 128], aT_[:], [atk], ["AT"], e="pool")
        for (c0, n, b) in segs:
            for ch in range(8):
                c.mm(bk[b][:, 0:n], aT_[:, ch, :], wrb[:, ch, c0:c0 + n], ch == 0, ch == 7, [atk, "wrb"], ["rb%d" % b])
        T_, tk = TKb[t % 2], "TKb%d" % (t % 2)
        c.act(T_[:, 0:512], bk[0][:, :], AF.Silu, ["rb0"], [tk])
        c.act(T_[:, 512:1024], bk[5][:, :], AF.Silu, ["rb5"], [tk])
        c.act(T_[:, 1024:1536], bk[6][:, :], AF.Silu, ["rb6"], [tk])
        c.cp("dve", T_[:, 2560:3072], bk[4][:, :], ["rb4"], [tk])
        c.tt("dve", gv[:, 0:8], bk[1][:, 0:8], K.dtb[:], ALU.add, ["rb1", "dtb"], ["gv0"])
        c.act(gv[:, 8:16], gv[:, 0:8], AF.Exp, ["gv0"], ["gv1"])
        c.act(gv[:, 16:24], gv[:, 8:16], AF.Ln, ["gv1", "cst"], ["gv2"], bias=K.cst[:, 0:1], scale=1.0)
        c.tt("dve", T_[:, 3072:3080], gv[:, 16:24], negA[:], ALU.mult, ["gv2", "negA"], [tk])
        c.act(T_[:, 3080:3088], bk[1][:, 8:16], AF.Sigmoid, ["rb1"], [tk])
        dst = Xb if t < NTL else T_
        dk_ = "Xb" if t < NTL else tk
        off = 0 if t < NTL else 1536
        c.cp("act", dst[:, off:off + 512], bk[2][:, :], ["rb2"], [dk_])
        c.op("act", lambda o: o.mul(out=dst[:, off + 512:off + 1024], in_=bk[3][:, :], mul=128.0 ** -0.5), ["rb3"], [dk_])
        if t < NTL:
            cf, sf = cos2[t % 2], sin2[t % 2]
            c.dma(cf[:], K.cos2[t * 128:(t + 1) * 128, :], [], ["cos2_%d" % (t % 2)])
            c.dma(sf[:], K.sin2[t * 128:(t + 1) * 128, :], [], ["sin2_%d" % (t % 2)])
            c.tt("dve", r1[:].rearrange("p (h d) -> p h d", h=8), Xb[:].rearrange("p (h d) -> p h d", h=8),
                 cf[:].unsqueeze(1).to_broadcast([128, 8, 128]), ALU.mult, ["Xb", "cos2_%d" % (t % 2)], ["rr1"])
            X5 = Xb[:].rearrange("p (h g s f) -> p h g s f", h=8, g=2, s=2, f=32)
            R5 = r2[:].rearrange("p (h g s f) -> p h g s f", h=8, g=2, s=2, f=32)
            S4 = sf[:].rearrange("p (g s f) -> p g s f", g=2, s=2, f=32)
            for s in range(2):
                c.tt("dve", R5[:, :, :, s, :], X5[:, :, :, 1 - s, :], S4[:, :, s, :].unsqueeze(1).to_broadcast([128, 8, 2, 32]),
                     ALU.mult, ["Xb", "sin2_%d" % (t % 2)], ["rr2"])
            c.tt("dve", T_[:, 1536:2560], r1[:], r2[:], ALU.add, ["rr1", "rr2"], [tk])
        for j in range(8):
            c.tr(bk[2 + j // 4][:, (j % 4) * 128:(j % 4 + 1) * 128], T_[:, 1536 + j * 128:1536 + (j + 1) * 128], K.identf[:],
                 [tk, "identf"], ["rb%d" % (2 + j // 4)])
        fb, fk = ftb[t % 2], "ftb%d" % (t % 2)
        c.cp("act", fb[:, 0:4, :].rearrange("p a t -> p (a t)"), bk[2][:, :], ["rb2"], [fk])
        c.cp("act", fb[:, 4:8, :].rearrange("p a t -> p (a t)"), bk[3][:, :], ["rb3"], [fk])
        c.dma(K.FT2[t], fb[:], [fk], ["FT2_%d" % t], e="sp")
        c.dma(K.TK[t], T_[:], [tk], ["TK%d" % t], e="sp")
    c.pop()
    c.push()
    preL = c.sb("preL", [128, L + 4])
    preC = c.sb("preC", [128, LC + 4])
    xa = c.sb("xa", [128, NT * 128])
    sq = c.sb("sqr", [128, NT * 128])
    tmc = c.sb("tmc", [128, NT, 128])
    wch = [c.sb("wch%d" % i, [128, 8, 128], BF16) for i in range(2)]
    wcs = [c.sb("wcs%d" % i, [128, 8, 128]) for i in range(2)]
    pb = [c.ps("cpb%d" % i, [128, 512]) for i in range(2)]
    pn = [c.ps("cpn%d" % i, [128, 512]) for i in range(2)]
    pT = [c.ps("cpt%d" % i, [128, 512]) for i in range(2)]
    rs = [c.sb("rsb%d" % i, [128, 512]) for i in range(2)]
    aTg = [c.sb("aTg%d" % i, [128, 8, 512], BF16) for i in range(2)]
    c.op("pool", lambda o: o.memset(preL[:], 0.0), [], ["preL"])
    c.op("pool", lambda o: o.memset(preC[:], 0.0), [], ["preC"])
    groups = [(i * 512, 512) for i in range(8)] + [(L, 256)]
    wsrc = K.rec_w_in.rearrange("(c p) n -> p c n", p=128)
    for cc in range(12):
        wc_, wck = wch[cc % 2], "wch%d" % (cc % 2)
        c.dma(wcs[cc % 2][:], wsrc[:, :, cc * 128:(cc + 1) * 128], [], ["wcs%d" % (cc % 2)], e="pool")
        c.cp("act", wc_[:], wcs[cc % 2][:], ["wcs%d" % (cc % 2)], [wck])
        for gi, (t0, N) in enumerate(groups):
            p_, pk = pb[gi % 2], "cpb%d" % (gi % 2)
            ag, agk = aTg[gi % 2], "aTg%d" % (gi % 2)
            c.dma(ag[:, :, 0:N], K.AT[:, :, t0:t0 + N], ["AT"], [agk], e="sp")
            for ch in range(8):
                c.mm(p_[:, 0:N], wc_[:, ch, :], ag[:, ch, 0:N], ch == 0, ch == 7, [wck, agk], [pk])
            if t0 < L:
                c.cp("act", preL[:, 2 + t0:2 + t0 + N], p_[:, 0:N], [pk], ["preL"])
            else:
                c.cp("act", preC[:, 2:2 + N], p_[:, 0:N], [pk], ["preC"])
        for (pre, prk, o0, n) in ((preL, "preL", 0, L), (preC, "preC", L, LC)):
            e1 = "dve" if o0 == 0 else "pool"
            c.ts(e1, xa[:, o0:o0 + n], pre[:, 0:n], K.convw[:, cc, 0:1], None, ALU.mult, None, [prk, "convw"], ["xa"])
            for k in range(1, 5):
                c.op("dve", lambda o: o.scalar_tensor_tensor(out=xa[:, o0:o0 + n], in0=pre[:, k:k + n], scalar=K.convw[:, cc, k:k + 1],
                                                         in1=xa[:, o0:o0 + n], op0=ALU.mult, op1=ALU.add), [prk, "convw", "xa"], ["xa"])
        c.act(xa[:], xa[:], AF.Silu, ["xa"], ["xa"])
        if cc < 8:
            c.tt("dve", sq[:], xa[:], xa[:], ALU.mult, ["xa"], ["sqr"])
            scale = (128.0 ** -0.5) if cc < 4 else 1.0
            for gi, (t0, N) in enumerate(groups):
                p_, pk = pn[gi % 2], "cpn%d" % (gi % 2)
                r_, rk = rs[gi % 2], "rsb%d" % (gi % 2)
                c.mm(p_[:, 0:N], K.recc[:, RC_ONES, :], sq[:, t0:t0 + N], True, True, ["sqr", "recc"], [pk])
                c.act(r_[:, 0:N], p_[:, 0:N], AF.Sqrt, [pk, "cst"], [rk], bias=K.cst[:, 1:2], scale=1.0)
                c.op("dve", lambda o: o.reciprocal(out=r_[:, 0:N], in_=r_[:, 0:N]), [rk], [rk])
                c.op("dve", lambda o: o.scalar_tensor_tensor(out=xa[:, t0:t0 + N], in0=xa[:, t0:t0 + N], scalar=scale, in1=r_[:, 0:N],
                                                           op0=ALU.mult, op1=ALU.mult), ["xa", rk], ["xa"])
        c.dma(K.FT[cc], xa[:], ["xa"], ["FT"], e="sp")
        for t in range(NT):
            p_, pk = pT[(t // 4) % 2], "cpt%d" % ((t // 4) % 2)
            c.tr(p_[:, (t % 4) * 128:(t % 4 + 1) * 128], xa[:, t * 128:(t + 1) * 128], K.identf[:], ["xa", "identf"], [pk])
            if t % 4 == 3 or t == NT - 1:
                tb = (t // 4) * 4
                nn = t - tb + 1
                c.cp("act" if (t // 4) % 2 else "dve", tmc[:, tb:tb + nn, :].rearrange("p a ch -> p (a ch)"), p_[:, 0:nn * 128], [pk], ["tmc"])
        c.dma(K.TM[cc].rearrange("a t ch -> t a ch"), tmc[:], ["tmc"], ["TM"], e="sp")
    c.pop()
    c.pop()


def emit_rec_pre(c, K):
    c.push()
    RC = K.recc
    qkv = c.sb("qkv", [128, 12, 128])
    gts = c.sb("gts", [128, 16])
    qT = c.sb("qTf", [128, 4, 128])
    kT = c.sb("kTf", [128, 4, 128])
    rt = c.sb("rtk", [128, 12, 128])
    ft2 = c.sb("ft2", [128, 8, 128])
    Mdb = RC[:, RC_MD, :].unsqueeze(1).to_broadcast([128, 4, 128])
    Mob = RC[:, RC_MO, :].unsqueeze(1).to_broadcast([128, 4, 128])
    Ib = K.identf[:].unsqueeze(1).to_broadcast([128, 4, 128])
    f2 = lambda ap: ap.rearrange("p a b -> p (a b)")
    Bs = []
    for d in range(2):
        B = Prog()
        B.sx = "_%d" % d
        B.gb = [c.ps("gbk%d_%d" % (i, d), [128, 512]) for i in range(2)]
        B.xb = [c.ps("xbk%d_%d" % (i, d), [128, 512]) for i in range(2)]
        B.g = 0
        B.sv = c.sb("sv%d" % d, [128, 32])
        for nm in ("kb", "kbT", "B0", "B1", "DT", "Dm", "DTI", "LT", "LoT", "Lm", "qkd", "nwk", "qg", "qgT", "kend", "qkm", "qdq", "qdqT", "kdd"):
            setattr(B, nm, c.sb("%s%d" % (nm, d), [128, 4, 128]))
        for nm in ("xbuf", "x1", "ybuf"):
            setattr(B, nm, c.sb("%s%d" % (nm, d), [128, 4, 256]))
        B.Tn = [c.sb("Tn%d_%d" % (i, d), [128, 4, 128]) for i in range(6)]
        B.Pn = [c.sb("Pn%d_%d" % (i, d), [128, 4, 128]) for i in range(2)]
        Bs.append(B)

    def nextbank(B):
        b = B.g % 2
        B.g += 1
        return B.gb[b], "gbk%d%s" % (b, B.sx)

    def neumann(buf, bk_):
        B = neumann.B
        sx = B.sx
        for n in range(5, -1, -1):
            for h in range(4):
                c.mm(B.xb[h // 2][:, (h % 2) * 256:(h % 2 + 1) * 256], B.Tn[n][:, h, :], buf[:, h, :], True, True, ["Tn%d" % n + sx, bk_], ["xbk%d" % (h // 2) + sx])
            for hb in range(2):
                c.tt("dve", f2(buf[:, hb * 2:hb * 2 + 2, :]), f2(buf[:, hb * 2:hb * 2 + 2, :]), B.xb[hb][:, :],
                     ALU.add if n > 0 else ALU.subtract, [bk_, "xbk%d" % hb + sx], [bk_])
            yield

    def unit(t, d, B):
        sx = B.sx
        neumann.B = B
        a = gts[:, d * 4:(d + 1) * 4]
        beta = gts[:, 8 + d * 4:8 + (d + 1) * 4]
        U = RC[:, RC_U + d, :]
        yield
        p_, pk = nextbank(B)
        c.mm(p_[:, 0:4], U, a, True, True, ["recc", "gts"], [pk])
        c.mm(p_[:, 4:8], RC[:, RC_ONES, :], a, True, True, ["recc", "gts"], [pk])
        c.cp("dve", B.sv[:, 0:8], p_[:, 0:8], [pk], [("sv" + sx)])
        c.act(B.sv[:, 8:16], B.sv[:, 0:8], AF.Exp, [("sv" + sx)], [("sv_e" + sx)])
        c.tt("dve", B.sv[:, 16:20], B.sv[:, 4:8], B.sv[:, 0:4], ALU.subtract, [("sv" + sx)], [("sv_d" + sx)])
        c.act(B.sv[:, 20:24], B.sv[:, 16:20], AF.Exp, [("sv_d" + sx)], [("sv_k" + sx)])
        c.tt("dve", B.sv[:, 24:28], beta, B.sv[:, 8:12], ALU.mult, ["gts", ("sv_e" + sx)], [("sv_b" + sx)])
        c.dma(K.PEG[d, t], B.sv[:, 12:16], [("sv_e" + sx)], ["PEG%d_%d" % (d, t)], e="pool")
        for h in range(4):
            c.smul("dve" if h % 2 else "act", B.kb[:, h, :], qkv[:, 4 + h, :], beta[:, h:h + 1], ["qkv", "gts"], [("kbt" + sx)])
        yield
        p_, pk = nextbank(B)
        for h in range(4):
            c.tr(p_[:, h * 128:(h + 1) * 128], B.kb[:, h, :], K.identf[:], [("kbt" + sx), "identf"], [pk])
        c.cp("act", f2(B.kbT[:]), p_[:, :], [pk], [("kbT" + sx)])
        for h in range(4):
            c.smul("act", B.B0[:, h, :], RC[:, RC_ONES, :], a[:, h:h + 1], ["recc", "gts"], [("B0" + sx)])
            c.smul("act" if h % 2 else "dve", B.B1[:, h, :], U, a[:, h:h + 1], ["recc", "gts"], [("B1" + sx)])
        yield
        p_, pk = nextbank(B)
        for h in range(4):
            o_ = p_[:, h * 128:(h + 1) * 128]
            c.mm(o_, B.B0[:, h, :], U, True, False, [("B0" + sx), "recc"], [pk])
            c.mm(o_, B.B1[:, h, :], RC[:, RC_NONES, :], False, False, [("B1" + sx), "recc"], [pk])
            c.mm(o_, K.identf[:], RC[:, RC_NEGT + d, :], False, True, ["identf", "recc"], [pk])
        c.act(f2(B.DT[:]), p_[:, :], AF.Exp, [pk], [("DT" + sx)])
        yield
        p_, pk = nextbank(B)
        for h in range(4):
            o_ = p_[:, h * 128:(h + 1) * 128]
            c.mm(o_, B.B1[:, h, :], RC[:, RC_ONES, :], True, False, [("B1" + sx), "recc"], [pk])
            c.mm(o_, B.B0[:, h, :], RC[:, RC_NU + d, :], False, False, [("B0" + sx), "recc"], [pk])
            c.mm(o_, K.identf[:], RC[:, RC_NEG + d, :], False, True, ["identf", "recc"], [pk])
        c.act(f2(B.Dm[:]), p_[:, :], AF.Exp, [pk], [("Dm" + sx)])
        yield
        p_, pk = nextbank(B)
        for h in range(4):
            c.mm(p_[:, h * 128:(h + 1) * 128], kT[:, h, :], B.kbT[:, h, :], True, True, ["kTf", ("kbT" + sx)], [pk])
        c.tt("dve", f2(B.LT[:]), p_[:, :], f2(B.DT[:]), ALU.mult, [pk, ("DT" + sx)], [("LT" + sx)])
        c.tt("dve", B.Tn[0][:], B.LT[:], Mdb, ALU.mult, [("LT" + sx), "recc"], ["Tn0" + sx])
        c.tt("dve", B.LoT[:], B.LT[:], Mob, ALU.mult, [("LT" + sx), "recc"], [("LoT" + sx)])
        yield
        p_, pk = nextbank(B)
        for h in range(4):
            c.mm(p_[:, h * 128:(h + 1) * 128], B.kbT[:, h, :], kT[:, h, :], True, True, ["kTf", ("kbT" + sx)], [pk])
        c.tt("dve", f2(B.Lm[:]), p_[:, :], f2(B.Dm[:]), ALU.mult, [pk, ("Dm" + sx)], [("Lm" + sx)])
        c.tt("dve", B.Pn[0][:], B.Lm[:], Mdb, ALU.mult, [("Lm" + sx), "recc"], ["Pn0" + sx])
        yield
        p_, pk = nextbank(B)
        for h in range(4):
            c.mm(p_[:, h * 128:(h + 1) * 128], kT[:, h, :], qT[:, h, :], True, True, ["kTf", "qTf"], [pk])
        c.tt("dve", B.DTI[:], B.DT[:], Ib, ALU.add, [("DT" + sx), "identf"], [("DTI" + sx)])
        c.tt("dve", f2(B.qkd[:]), p_[:, :], f2(B.DTI[:]), ALU.mult, [pk, ("DTI" + sx)], [("qkd" + sx)])
        c.dma(K.PQK[d, t], f2(B.qkd[:]), [("qkd" + sx)], ["PQK%d_%d" % (d, t)], e="sp")
        for n in range(5):
            if n < 4:
                p_, pk = nextbank(B)
                for h in range(4):
                    c.mm(p_[:, h * 128:(h + 1) * 128], B.Tn[n][:, h, :], B.Pn[n % 2][:, h, :], True, True, [("Tn%d" % n + sx), ("Pn%d" % (n % 2) + sx)], [pk])
                c.cp("act", f2(B.Pn[(n + 1) % 2][:]), p_[:, :], [pk], [("Pn%d" % ((n + 1) % 2) + sx)])
            p_, pk = nextbank(B)
            for h in range(4):
                c.mm(p_[:, h * 128:(h + 1) * 128], B.Pn[n % 2][:, h, :], B.Tn[n][:, h, :], True, True, [("Tn%d" % n + sx), ("Pn%d" % (n % 2) + sx)], [pk])
            c.cp("dve", f2(B.Tn[n + 1][:]), p_[:, :], [pk], [("Tn%d" % (n + 1) + sx)])
            yield
        for h in range(4):
            c.smul("act", B.xbuf[:, h, 0:128], qkv[:, 8 + h, :], beta[:, h:h + 1], ["qkv", "gts"], [("xbuf" + sx)])
            c.ts("dve", B.xbuf[:, h, 128:256], qkv[:, 4 + h, :], B.sv[:, 24 + h:25 + h], None, ALU.mult, None, ["qkv", ("sv_b" + sx)], [("xbuf" + sx)])
        neumann.B = B
        yield from neumann(B.xbuf, "xbuf" + sx)
        c.cp("act", B.x1[:], B.xbuf[:], [("xbuf" + sx)], [("x1" + sx)])
        for h in range(4):
            c.mm(B.xb[h // 2][:, (h % 2) * 256:(h % 2 + 1) * 256], B.LoT[:, h, :], B.x1[:, h, :], True, True, [("LoT" + sx), ("x1" + sx)], [("xbk%d" % (h // 2) + sx)])
        for hb in range(2):
            c.cp("act", f2(B.ybuf[:, hb * 2:hb * 2 + 2, :]), B.xb[hb][:, :], [("xbk%d" % hb + sx)], [("ybuf" + sx)])
        neumann.B = B
        yield from neumann(B.ybuf, "ybuf" + sx)
        c.tt("dve", B.x1[:], B.x1[:], B.ybuf[:], ALU.subtract, [("x1" + sx), ("ybuf" + sx)], [("x1" + sx)])
        c.dma(K.PU[d, t].rearrange("p (h v) -> p h v", h=4), B.x1[:, :, 0:128], [("x1" + sx)], ["PU%d_%d" % (d, t)], e="sp")
        yield
        p_, pk = nextbank(B)
        for h in range(4):
            c.tr(p_[:, h * 128:(h + 1) * 128], B.x1[:, h, 128:256], K.identf[:], [("x1" + sx), "identf"], [pk])
        c.op("act", lambda o: o.mul(out=f2(B.nwk[:]), in_=p_[:, :], mul=-1.0), [pk], [("nwk" + sx)])
        c.dma(K.PNW[d, t], f2(B.nwk[:]), [("nwk" + sx)], ["PNW%d_%d" % (d, t)], e="pool")
        for h in range(4):
            c.smul("dve" if h % 2 else "act", B.qg[:, h, :], qkv[:, h, :], B.sv[:, 8 + h:9 + h], ["qkv", ("sv_e" + sx)], [("qgt" + sx)])
            c.smul("act" if h % 2 else "dve", B.kend[:, h, :], qkv[:, 4 + h, :], B.sv[:, 20 + h:21 + h], ["qkv", ("sv_k" + sx)], [("kend" + sx)])
        yield
        p_, pk = nextbank(B)
        for h in range(4):
            c.tr(p_[:, h * 128:(h + 1) * 128], B.qg[:, h, :], K.identf[:], [("qgt" + sx), "identf"], [pk])
        c.cp("act", f2(B.qgT[:]), p_[:, :], [pk], [("qgT" + sx)])
        c.dma(K.PQG[d, t], f2(B.qgT[:]), [("qgT" + sx)], ["PQG%d_%d" % (d, t)], e="sp")
        c.dma(K.PKE[d, t], f2(B.kend[:]), [("kend" + sx)], ["PKE%d_%d" % (d, t)], e="pool")
        yield
        p_, pk = nextbank(B)
        for h in range(4):
            c.mm(p_[:, h * 128:(h + 1) * 128], ft2[:, 4 + h, :], ft2[:, h, :], True, True, ["ft2"], [pk])
        c.tt("dve", f2(B.qkm[:]), p_[:, :], f2(K.dmt[:, d]), ALU.mult, [pk, "dmt"], [("qkm" + sx)])
        c.dma(K.RQK[d, t], f2(B.qkm[:]), [("qkm" + sx)], ["RQK%d_%d" % (d, t)], e="sp")
        for h in range(4):
            c.smul("act", B.qdq[:, h, :], rt[:, h, :], K.decs[:, d, 0, h:h + 1], ["rtk", "decs"], [("qdq" + sx)])
            c.ts("dve", B.kdd[:, h, :], rt[:, 4 + h, :], K.decs[:, d, 1, h:h + 1], None, ALU.mult, None, ["rtk", "decs"], [("kdd" + sx)])
        yield
        p_, pk = nextbank(B)
        for h in range(4):
            c.tr(p_[:, h * 128:(h + 1) * 128], B.qdq[:, h, :], K.identf[:], [("qdq" + sx), "identf"], [pk])
        c.cp("act", f2(B.qdqT[:]), p_[:, :], [pk], [("qdqT" + sx)])
        c.dma(K.RQQ[d, t], f2(B.qdqT[:]), [("qdqT" + sx)], ["RQQ%d_%d" % (d, t)], e="pool")
        c.dma(K.RKD[d, t], f2(B.kdd[:]), [("kdd" + sx)], ["RKD%d_%d" % (d, t)], e="sp")


    for t in range(NT):
        c.dma(qkv[:], K.TM[:, t].rearrange("a p ch -> p a ch"), ["TM"], ["qkv"], e="sp")
        c.dma(gts[:], K.TK[t][:, 3072:3088], ["TK%d" % t], ["gts"], e="pool")
        c.dma(qT[:], K.FT[0:4, :, t * 128:(t + 1) * 128].rearrange("a p t -> p a t"), ["FT"], ["qTf"], e="sp")
        c.dma(kT[:], K.FT[4:8, :, t * 128:(t + 1) * 128].rearrange("a p t -> p a t"), ["FT"], ["kTf"], e="pool")
        c.dma(rt[:], K.TK[t][:, 1536:3072].rearrange("p (a ch) -> p a ch", a=12), ["TK%d" % t], ["rtk"], e="sp")
        c.dma(ft2[:], K.FT2[t], ["FT2_%d" % t], ["ft2"], e="pool")

        _interleave([unit(t, 0, Bs[0]), unit(t, 1, Bs[1])])
    c.pop()


def emit_rec_scan(c, K):
    c.push()
    Sg = [[c.sb("Sg%d%d" % (d, h), [128, 128]) for h in range(4)] for d in range(2)]
    Sr = [[c.sb("Sr%d%d" % (d, h), [128, 128]) for h in range(4)] for d in range(2)]
    for d in range(2):
        for h in range(4):
            c.op("pool", lambda o: o.memset(Sg[d][h][:], 0.0), [], ["Sg%d%d" % (d, h)])
            c.op("pool", lambda o: o.memset(Sr[d][h][:], 0.0), [], ["Sr%d%d" % (d, h)])
    names = ("u", "nw", "qg", "qk", "ke", "rqk", "rqq", "rkd", "vd")
    bufs = [[{n: c.sb("sc_%s%d%d" % (n, d, i), [128, 4, 128]) for n in names} for i in range(2)] for d in range(2)]
    egl = [[c.sb("sc_egl%d%d" % (d, i), [128, 4]) for i in range(2)] for d in range(2)]
    wb = [c.sb("sc_w%d" % d, [128, 4, 128]) for d in range(2)]
    og = [c.sb("sc_og%d" % d, [128, 4, 128]) for d in range(2)]
    orr = [c.sb("sc_or%d" % d, [128, 4, 128]) for d in range(2)]
    pbk = [[c.ps("scp%d%d" % (d, i), [128, 512]) for i in range(4)] for d in range(2)]
    f2 = lambda ap: ap.rearrange("p a b -> p (a b)")
    order = [[32, 33] + list(range(32)), [33, 32] + list(range(31, -1, -1))]
    cdec = [g ** 128.0 for g in GAMMAS]
    for step in range(NT):
        for d in range(2):
            t = order[d][step]
            i = step % 2
            B = bufs[d][i]
            kk = lambda n: "sc_%s%d%d" % (n, d, i)
            srcs = {"u": (K.PU, "PU"), "nw": (K.PNW, "PNW"), "qg": (K.PQG, "PQG"), "qk": (K.PQK, "PQK"), "ke": (K.PKE, "PKE"),
                    "rqk": (K.RQK, "RQK"), "rqq": (K.RQQ, "RQQ"), "rkd": (K.RKD, "RKD")}
            for j, (n, (src, sn)) in enumerate(srcs.items()):
                c.dma(f2(B[n][:]), src[d, t], ["%s%d_%d" % (sn, d, t)], [kk(n)], e="sp" if j % 2 else "pool")
            c.dma(f2(B["vd"][:]), K.TK[t][:, 2560:3072], ["TK%d" % t], [kk("vd")], e="sp")
            c.dma(egl[d][i][:], K.PEG[d, t], ["PEG%d_%d" % (d, t)], ["sc_egl%d%d" % (d, i)], e="pool")
            pw, po, pS, pr = pbk[d]
            pk = ["scp%d%d" % (d, j) for j in range(4)]
            latent = t < NTL
            for h in range(4):
                sgk = "Sg%d%d" % (d, h)
                c.mm(pw[:, h * 128:(h + 1) * 128], B["nw"][:, h, :], Sg[d][h][:], True, True, [kk("nw"), sgk], [pk[0]])
            c.tt("dve", f2(wb[d][:]), f2(B["u"][:]), pw[:, :], ALU.add, [kk("u"), pk[0]], ["sc_w%d" % d])
            for h in range(4):
                sgk = "Sg%d%d" % (d, h)
                if latent:
                    c.mm(po[:, h * 128:(h + 1) * 128], B["qg"][:, h, :], Sg[d][h][:], True, False, [kk("qg"), sgk], [pk[1]])
                    c.mm(po[:, h * 128:(h + 1) * 128], B["qk"][:, h, :], wb[d][:, h, :], False, True, [kk("qk"), "sc_w%d" % d], [pk[1]])
                c.mm(pS[:, h * 128:(h + 1) * 128], B["ke"][:, h, :], wb[d][:, h, :], True, True, [kk("ke"), "sc_w%d" % d], [pk[2]])
            for h in range(4):
                sgk = "Sg%d%d" % (d, h)
                c.op("dve", lambda o: o.scalar_tensor_tensor(out=Sg[d][h][:], in0=Sg[d][h][:], scalar=egl[d][i][:, h:h + 1], in1=pS[:, h * 128:(h + 1) * 128],
                                                           op0=ALU.mult, op1=ALU.add), [sgk, "sc_egl%d%d" % (d, i), pk[2]], [sgk])
            if latent:
                c.cp("act", f2(og[d][:]), po[:, :], [pk[1]], ["sc_og%d" % d])
                c.dma(K.OG[d, t], f2(og[d][:]), ["sc_og%d" % d], ["OG%d_%d" % (d, t)], e="sp")
            for h in range(4):
                srk = "Sr%d%d" % (d, h)
                if latent:
                    c.mm(pr[:, h * 128:(h + 1) * 128], B["rqq"][:, h, :], Sr[d][h][:], True, False, [kk("rqq"), srk], [pk[3]])
                    c.mm(pr[:, h * 128:(h + 1) * 128], B["rqk"][:, h, :], B["vd"][:, h, :], False, True, [kk("rqk"), kk("vd")], [pk[3]])
                c.mm(pw[:, h * 128:(h + 1) * 128], B["rkd"][:, h, :], B["vd"][:, h, :], True, True, [kk("rkd"), kk("vd")], [pk[0]])
            for h in range(4):
                srk = "Sr%d%d" % (d, h)
                c.op("pool" if False else "dve", lambda o: o.scalar_tensor_tensor(out=Sr[d][h][:], in0=Sr[d][h][:], scalar=float(cdec[h]), in1=pw[:, h * 128:(h + 1) * 128],
                                                           op0=ALU.mult, op1=ALU.add), [srk, pk[0]], [srk])
            if latent:
                c.cp("act", f2(orr[d][:]), pr[:, :], [pk[3]], ["sc_or%d" % d])
                c.dma(K.OR[d, t], f2(orr[d][:]), ["sc_or%d" % d], ["OR%d_%d" % (d, t)], e="pool")
    c.pop()


def emit_rec_merge(c, K):
    layer = 1
    c.push()
    woutb = c.sb("rwoutb", [128, 8, 1024], BF16)
    wst = [c.sb("rwst%d" % i, [128, 1024]) for i in range(2)]
    for ch in range(8):
        c.dma(wst[ch % 2][:], K.rec_w_out[ch * 128:(ch + 1) * 128, :], [], ["rwst%d" % (ch % 2)], e="sp" if ch % 2 else "pool")
        c.cp("dve" if ch % 2 else "pool", woutb[:, ch, :], wst[ch % 2][:], ["rwst%d" % (ch % 2)], ["rwoutb"])
    ogb = [[c.sb("m_og%d%d" % (d, i), [128, 4, 128]) for i in range(2)] for d in range(2)]
    orb = [[c.sb("m_or%d%d" % (d, i), [128, 4, 128]) for i in range(2)] for d in range(2)]
    zg = [c.sb("m_zg%d" % i, [128, 1536]) for i in range(2)]
    ds = c.sb("m_ds", [128, 4, 128])
    sq = c.sb("m_sq", [128, 4, 128])
    xc = c.sb("m_xc", [128, 4, 128])
    st = c.sb("m_st", [128, 32])
    mixf = c.sb("m_mixf", [128, 1024])
    mixb = c.sb("m_mixb", [128, 1024], BF16)
    mixT = c.sb("m_mixT", [128, 8, 128], BF16)
    ptr = c.ps("m_ptr", [128, 1024], BF16)
    pY = c.ps("m_pY", [128, 1024])
    W = Prog()
    W.n = 0
    W.hbuf = [c.sb("hbuf%d" % i, [128, 1024]) for i in range(2)]
    W.tmpf = c.sb("tmpf", [128, 1024])
    f2 = lambda ap: ap.rearrange("p a b -> p (a b)")
    bc4 = lambda ap: ap.unsqueeze(2).to_broadcast([128, 4, 128])
    for t in range(NTL):
        i = t % 2
        for d in range(2):
            c.dma(f2(ogb[d][i][:]), K.OG[d, t], ["OG%d_%d" % (d, t)], ["m_og%d%d" % (d, i)], e="sp")
            c.dma(f2(orb[d][i][:]), K.OR[d, t], ["OR%d_%d" % (d, t)], ["m_or%d%d" % (d, i)], e="pool")
        c.dma(zg[i][:], K.TK[t][:, 0:1536], ["TK%d" % t], ["m_zg%d" % i], e="sp")
        c.tt("dve", ds[:], ogb[0][i][:], ogb[1][i][:], ALU.add, ["m_og0%d" % i, "m_og1%d" % i], ["m_ds"])
        c.tt("dve", sq[:], ds[:], ds[:], ALU.mult, ["m_ds"], ["m_sq"])
        c.op("dve", lambda o: o.tensor_reduce(out=st[:, 0:4], in_=sq[:], axis=AX.X, op=ALU.add), ["m_sq"], ["m_st0"])
        c.ts("dve", st[:, 4:8], st[:, 0:4], 1.0 / 128, EPS, ALU.mult, ALU.add, ["m_st0"], ["m_st1"])
        c.act(st[:, 4:8], st[:, 4:8], AF.Sqrt, ["m_st1"], ["m_st1"])
        c.op("dve", lambda o: o.reciprocal(out=st[:, 8:12], in_=st[:, 4:8]), ["m_st1"], ["m_st2"])
        c.tt("dve", ds[:], ds[:], bc4(st[:, 8:12]), ALU.mult, ["m_ds", "m_st2"], ["m_ds"])
        c.tt("dve", f2(ds[:]), f2(ds[:]), K.outg[:], ALU.mult, ["m_ds", "outg"], ["m_ds"])
        c.tt("dve", mixf[:, 0:512], f2(ds[:]), zg[i][:, 0:512], ALU.mult, ["m_ds", "m_zg%d" % i], ["m_mixf"])
        for d in range(2):
            x = orb[d][i]
            xk = "m_or%d%d" % (d, i)
            c.op("dve", lambda o: o.tensor_reduce(out=st[:, 12:16], in_=x[:], axis=AX.X, op=ALU.add), [xk], ["m_st3"])
            c.ts("dve", st[:, 16:20], st[:, 12:16], 1.0 / 128, None, ALU.mult, None, ["m_st3"], ["m_st4"])
            c.tt("dve", xc[:], x[:], bc4(st[:, 16:20]), ALU.subtract, [xk, "m_st4"], ["m_xc"])
            c.tt("dve", sq[:], xc[:], xc[:], ALU.mult, ["m_xc"], ["m_sq"])
            c.op("dve", lambda o: o.tensor_reduce(out=st[:, 20:24], in_=sq[:], axis=AX.X, op=ALU.add), ["m_sq"], ["m_st5"])
            c.ts("dve", st[:, 24:28], st[:, 20:24], 1.0 / 128, EPS, ALU.mult, ALU.add, ["m_st5"], ["m_st6"])
            c.act(st[:, 24:28], st[:, 24:28], AF.Sqrt, ["m_st6"], ["m_st6"])
            c.op("dve", lambda o: o.reciprocal(out=st[:, 28:32], in_=st[:, 24:28]), ["m_st6"], ["m_st7"])
            c.tt("dve", xc[:], xc[:], bc4(st[:, 28:32]), ALU.mult, ["m_xc", "m_st7"], ["m_xc"])
            c.tt("dve", f2(xc[:]), f2(xc[:]), K.gng[:], ALU.mult, ["m_xc", "gng"], ["m_xc"])
            if d == 0:
                c.tt("dve", mixf[:, 512:1024], f2(xc[:]), zg[i][:, 512:1024], ALU.mult, ["m_xc", "m_zg%d" % i], ["m_mixf"])
            else:
                c.tt("dve", f2(xc[:]), f2(xc[:]), zg[i][:, 1024:1536], ALU.mult, ["m_xc", "m_zg%d" % i], ["m_xc"])
                c.tt("dve", mixf[:, 512:1024], mixf[:, 512:1024], f2(xc[:]), ALU.add, ["m_xc", "m_mixf"], ["m_mixf"])
        c.cp("act", mixb[:], mixf[:], ["m_mixf"], ["m_mixb"])
        for ch in range(8):
            c.tr(ptr[:, ch * 128:(ch + 1) * 128], mixb[:, ch * 128:(ch + 1) * 128], K.identb[:], ["m_mixb", "identb"], ["m_ptr"])
        c.cp("act", f2(mixT[:]), ptr[:, :], ["m_ptr"], ["m_mixT"])
        for nb in range(2):
            for ch in range(8):
                c.mm(pY[:, nb * 512:(nb + 1) * 512], mixT[:, ch, :], woutb[:, ch, nb * 512:(nb + 1) * 512], ch == 0, ch == 7, ["m_mixT", "rwoutb"], ["m_pY"])
        emit_residual(c, K, W, (hrows(K, t), "H%d" % t), pY[:, :], ["m_pY"], 0, 0, t)
    c.pop()


def emit_rec(c, K):
    c.push()
    K.recc = c.sb("recc", [128, 12, 128])
    K.dmt = c.sb("dmt", [128, 2, 4, 128])
    K.decs = c.sb("decs", [128, 2, 2, 4])
    K.outg = c.sb("outg", [128, 512])
    K.gng = c.sb("gng", [128, 512])
    K.convw = c.sb("convw", [128, 12, 5])
    K.alog = c.sb("alog", [128, 8])
    K.dtb = c.sb("dtb", [128, 8])
    K.cst = c.sb("cst", [128, 2])
    c.dma(K.recc[:], K.recc_in, [], ["recc"])
    c.dma(K.dmt[:], K.dmt_in, [], ["dmt"])
    c.dma(K.decs[:], K.decs_in, [], ["decs"])
    c.dma(K.outg[:], K.outg_in, [], ["outg"])
    c.dma(K.gng[:], K.gng_in, [], ["gng"])
    c.dma(K.convw[:], K.convw_in, [], ["convw"])
    c.dma(K.alog[:], K.alog_in, [], ["alog"])
    c.dma(K.dtb[:], K.dtb_in, [], ["dtb"])
    c.op("pool", lambda o: o.memset(K.cst[:, 0:1], 1.0), [], ["cst"])
    c.op("pool", lambda o: o.memset(K.cst[:, 1:2], EPS), [], ["cst"])
    emit_rec_features(c, K)
    emit_rec_pre(c, K)
    emit_rec_scan(c, K)
    emit_rec_merge(c, K)
    c.pop()


U32 = mybir.dt.uint32


def emit_peer2(c, K, layer, ntiles):
    emit_peer_prep(c, K, layer)
    c.push()
    GT = 2
    wqb = c.sb("wqb", [128, 8, 2048], BF16)
    c.push()
    wst = [c.sb("wqst%d" % i, [128, 2048]) for i in range(2)]
    for ch in range(8):
        c.dma(wst[ch % 2][:], K.peer_w_q[layer, ch * 128:(ch + 1) * 128, :], [], ["wqst%d" % (ch % 2)], e="sp" if ch % 2 else "pool")
        c.cp("dve" if ch % 2 else "pool", wqb[:, ch, :], wst[ch % 2][:], ["wqst%d" % (ch % 2)], ["wqb"])
    c.pop()
    skf = c.sb("skf", [128, 2, 128])
    skb = c.sb("skb", [128, 2, 128], BF16)
    skT = c.sb("skT", [128, 2, 128], BF16)
    Bk = [c.ps("bank%d" % i, [128, 512]) for i in range(8)]
    Bkb = [b[:].bitcast(BF16) for b in Bk]
    for p in range(2):
        c.dma(skf[:, p, :], K.peer_sk[layer, p], [], ["skf"])
    c.cp("dve", skb[:], skf[:], ["skf"], ["skb"])
    for p in range(2):
        c.tr(Bkb[7][:, p * 128:(p + 1) * 128], skb[:, p, :], K.identb[:], ["skb", "identb"], ["bank7"])
    c.cp("dve", skT[:].rearrange("p a k -> p (a k)"), Bkb[7][:, 0:256], ["bank7"], ["skT"])
    iotaj = c.sb("iotaj", [128, 128])
    iota_a = c.sb("iota_a", [128, 8, 16, 16])
    c.dma(iotaj[:], K.iotaj_in, [], ["iotaj"])
    c.dma(iota_a[:].rearrange("p h r a -> p (h r a)"), K.iotaa_in, [], ["iota_a"])

    W = alloc_normwork(c, Bkb[5])
    W.ptrkey = "bank5"
    xTg = c.sb("xTg", [128, 8, GT * 128], BF16)
    qb16 = c.sb("qb16", [128, 1024], BF16)
    qT = c.sb("qT", [128, 8, 128], BF16)
    S = c.sb("S", [128, 16, 128])
    SV = c.sb("SV", [128, 16, 16])
    SIu = c.sb("SIu", [128, 16, 16], U32)
    SIf = c.sb("SIf", [128, 16, 16])
    wk = c.sb("wk", [128, 256])
    cand = c.sb("cand", [128, 8, 256])
    CV = c.sb("CV", [128, 8, 16])
    CPu = c.sb("CPu", [128, 8, 16], U32)
    CPf = c.sb("CPf", [128, 8, 16])
    ab = c.sb("abf", [128, 2, 8, 16])
    eq = c.sb("eqg", [128, 8, 16, 16])
    IJG = c.sb("IJG", [128, 3, 128])
    IJGT = [c.sb("IJGT%d" % i, [128, 3, 128]) for i in range(2)]
    ez = c.sb("ez", [128, 8, 16])
    st8 = c.sb("st8", [128, 16])
    NB = 4
    At = [c.sb("At%d" % i, [128, 128], BF16) for i in range(NB)]
    Bt = [c.sb("Bt%d" % i, [128, 128], BF16) for i in range(NB)]
    GG = c.sb("GG", [128, GT * 128, 128], BF16)
    uTi = [c.sb("uTi%d" % i, [128, 8, 128], BF16) for i in range(3)]
    vi = [c.sb("vi%d" % i, [128, 1024], BF16) for i in range(3)]
    gl = [c.sb("gl%d" % i, [128, GT * 128], BF16) for i in range(2)]
    wT = [c.sb("wT%d" % i, [128, GT * 128], BF16) for i in range(2)]
    ring = Prog()
    ring.n = 0
    ring.e = 0
    ngroups = ntiles // GT
    for g in range(ngroups):
        tiles = [g * GT + i for i in range(GT)]
        for tl, t in enumerate(tiles):
            m = 0 if t < NTL else 1
            emit_norm_T_peer(c, K, W, hrows(K, t), "H%d" % t, m, xTg[:, :, tl * 128:(tl + 1) * 128], "xTg")
            for hh in range(2):
                for nb in range(2):
                    for ch in range(8):
                        c.mm(Bk[nb][:, :], xTg[:, ch, tl * 128:(tl + 1) * 128], wqb[:, ch, hh * 1024 + nb * 512: hh * 1024 + (nb + 1) * 512],
                             ch == 0, ch == 7, ["xTg", "wqb"], ["bank%d" % nb])
                for nb in range(2):
                    c.cp("act", qb16[:, nb * 512:(nb + 1) * 512], Bk[nb][:, :], ["bank%d" % nb], ["qb16"])
                for b in range(8):
                    c.tr(Bkb[2][:, b * 128:(b + 1) * 128], qb16[:, b * 128:(b + 1) * 128], K.identb[:], ["qb16", "identb"], ["bank2"])
                c.cp("act", qT[:].rearrange("p b t -> p (b t)"), Bkb[2][:, :], ["bank2"], ["qT"])
                for b in range(8):
                    bank = 3 + b // 4
                    c.mm(Bk[bank][:, (b % 4) * 128:(b % 4 + 1) * 128], qT[:, b, :], skT[:, b % 2, :], True, True, ["qT", "skT"], ["bank%d" % bank])
                for bb in range(2):
                    c.cp("act", S[:, hh * 8 + bb * 4: hh * 8 + (bb + 1) * 4, :].rearrange("p b k -> p (b k)"), Bk[3 + bb][:, :],
                         ["bank%d" % (3 + bb)], ["S"])
            for hp in range(16):
                c.op("dve", lambda o: o.max(out=SV[:, hp, 0:8], in_=S[:, hp, :]), ["S"], ["SV"])
                c.op("dve", lambda o: o.max_index(out=SIu[:, hp, 0:8], in_max=SV[:, hp, 0:8], in_values=S[:, hp, :]), ["S", "SV"], ["SIu"])
                c.op("dve", lambda o: o.match_replace(out=wk[:, 0:128], in_to_replace=SV[:, hp, 0:8], in_values=S[:, hp, :], imm_value=-1e30),
                     ["S", "SV"], ["wk"])
                c.op("dve", lambda o: o.max(out=SV[:, hp, 8:16], in_=wk[:, 0:128]), ["wk"], ["SV"])
                c.op("dve", lambda o: o.max_index(out=SIu[:, hp, 8:16], in_max=SV[:, hp, 8:16], in_values=wk[:, 0:128]), ["wk", "SV"], ["SIu"])
            c.cp("dve", SIf[:], SIu[:], ["SIu"], ["SIf"])
            SV8 = SV[:].rearrange("p (h two) a -> p h two a", h=8, two=2)
            SI8 = SIf[:].rearrange("p (h two) a -> p h two a", h=8, two=2)
            c.tt("dve", cand[:].rearrange("p h (a b) -> p h a b", a=16), SV8[:, :, 0, :].unsqueeze(3).to_broadcast([128, 8, 16, 16]),
                 SV8[:, :, 1, :].unsqueeze(2).to_broadcast([128, 8, 16, 16]), ALU.add, ["SV"], ["cand"])
            for h in range(8):
                c.op("dve", lambda o: o.max(out=CV[:, h, 0:8], in_=cand[:, h, :]), ["cand"], ["CV"])
                c.op("dve", lambda o: o.max_index(out=CPu[:, h, 0:8], in_max=CV[:, h, 0:8], in_values=cand[:, h, :]), ["cand", "CV"], ["CPu"])
                c.op("dve", lambda o: o.match_replace(out=wk[:, :], in_to_replace=CV[:, h, 0:8], in_values=cand[:, h, :], imm_value=-1e30),
                     ["cand", "CV"], ["wk"])
                c.op("dve", lambda o: o.max(out=CV[:, h, 8:16], in_=wk[:, :]), ["wk"], ["CV"])
                c.op("dve", lambda o: o.max_index(out=CPu[:, h, 8:16], in_max=CV[:, h, 8:16], in_values=wk[:, :]), ["wk", "CV"], ["CPu"])
            c.cp("dve", CPf[:], CPu[:], ["CPu"], ["CPf"])
            c.ts("dve", ab[:, 1], CPf[:], 1.0 / 16, -1.0, ALU.mult, ALU.add, ["CPf"], ["abf"])
            c.tt("dve", eq[:], ab[:, 1].unsqueeze(3).to_broadcast([128, 8, 16, 16]), iota_a[:], ALU.is_ge, ["abf", "iota_a"], ["eqg"])
            c.op("dve", lambda o: o.tensor_reduce(out=ab[:, 0], in_=eq[:], axis=AX.X, op=ALU.add), ["eqg"], ["abf"])
            c.op("dve", lambda o: o.scalar_tensor_tensor(out=ab[:, 1], in0=ab[:, 0], scalar=-16.0, in1=CPf[:], op0=ALU.mult, op1=ALU.add), ["abf", "CPf"], ["abf"])
            IJ4 = IJG[:].rearrange("p c (h r) -> p c h r", h=8)
            for which in range(2):
                c.tt("dve", eq[:], ab[:, which].unsqueeze(3).to_broadcast([128, 8, 16, 16]), iota_a[:], ALU.is_equal, ["abf", "iota_a"], ["eqg"])
                c.tt("dve", eq[:], eq[:], SI8[:, :, which, :].unsqueeze(2).to_broadcast([128, 8, 16, 16]), ALU.mult, ["eqg", "SIf"], ["eqg"])
                c.op("dve", lambda o: o.tensor_reduce(out=IJ4[:, which], in_=eq[:], axis=AX.X, op=ALU.add), ["eqg"], ["IJG"])
            c.tt("dve", ez[:], CV[:], CV[:, :, 0:1].to_broadcast([128, 8, 16]), ALU.subtract, ["CV"], ["ez"])
            c.act(ez[:].rearrange("p h a -> p (h a)"), ez[:].rearrange("p h a -> p (h a)"), AF.Exp, ["ez"], ["ez"])
            c.op("dve", lambda o: o.tensor_reduce(out=st8[:, 0:8], in_=ez[:], axis=AX.X, op=ALU.add), ["ez"], ["st8a"])
            c.op("dve", lambda o: o.reciprocal(out=st8[:, 8:16], in_=st8[:, 0:8]), ["st8a"], ["st8b"])
            c.tt("dve", IJ4[:, 2], ez[:], st8[:, 8:16].unsqueeze(2).to_broadcast([128, 8, 16]), ALU.mult, ["ez", "st8b"], ["IJG"])
            for w_ in range(3):
                c.tr(Bk[6][:, w_ * 128:(w_ + 1) * 128], IJG[:, w_, :], K.identf[:], ["IJG", "identf"], ["bank6"])
            T3, T3k = IJGT[tl], "IJGT%d" % tl
            c.cp("act", T3[:].rearrange("p c t -> p (c t)"), Bk[6][:, 0:384], ["bank6"], [T3k])
            for tk in range(128):
                k = ring.e % NB
                ring.e += 1
                pb_, pbk = Bk[6 + (tk // 4) % 2], "bank%d" % (6 + (tk // 4) % 2)
                c.ts("dve", Bt[k][:], iotaj[:], T3[:, 1, tk:tk + 1], None, ALU.is_equal, None, ["iotaj", T3k], ["Bt%d" % k])
                c.ts("dve", At[k][:], iotaj[:], T3[:, 0, tk:tk + 1], T3[:, 2, tk:tk + 1], ALU.is_equal, ALU.mult, ["iotaj", T3k], ["At%d" % k])
                c.mm(pb_[:, (tk % 4) * 128:(tk % 4 + 1) * 128], Bt[k][:], At[k][:], True, True, ["Bt%d" % k, "At%d" % k], [pbk])
                if tk % 4 == 3:
                    tb = tl * 128 + tk - 3
                    c.cp("act", GG[:, tb:tb + 4, :].rearrange("p t i -> p (t i)"), pb_[:, :], [pbk], ["GG"])

        def issue_pre(i):
            u_, uk = uTi[i % 3], "uTi%d" % (i % 3)
            v_, vk = vi[i % 3], "vi%d" % (i % 3)
            c.dma(u_[:].rearrange("p c j -> p (c j)"), K.UT[i], ["UT"], [uk], e="sp")
            c.dma(v_[:], K.VB[i], ["VB"], [vk], e="sp")
            pre, prek = Bk[4 + i % 2], "bank%d" % (4 + i % 2)
            for ch in range(8):
                c.mm(pre[:, 0:GT * 128], u_[:, ch, :], xTg[:, ch, :], ch == 0, ch == 7, [uk, "xTg"], [prek])

        def issue_gelu(i):
            pre, prek = Bk[4 + i % 2], "bank%d" % (4 + i % 2)
            c.act(gl[i % 2][:], pre[:, 0:GT * 128], AF.Gelu, [prek], ["gl%d" % (i % 2)])

        def issue_w(i):
            c.tt("dve", wT[i % 2][:], GG[:, :, i], gl[i % 2][:], ALU.mult, ["GG", "gl%d" % (i % 2)], ["wT%d" % (i % 2)])

        def issue_y(i):
            v_, vk = vi[i % 3], "vi%d" % (i % 3)
            for tl in range(GT):
                for nb in range(2):
                    c.mm(Bk[tl * 2 + nb][:, :], wT[i % 2][:, tl * 128:(tl + 1) * 128], v_[:, nb * 512:(nb + 1) * 512], i == 0, i == 127,
                         ["wT%d" % (i % 2), vk], ["bank%d" % (tl * 2 + nb)])

        issue_pre(0)
        issue_gelu(0)
        for i in range(128):
            if i + 1 < 128:
                issue_pre(i + 1)
            issue_w(i)
            if i + 1 < 128:
                issue_gelu(i + 1)
            issue_y(i)
        for tl, t in enumerate(tiles):
            m = 0 if t < NTL else 1
            emit_residual_banks(c, K, W, t, Bk[tl * 2], Bk[tl * 2 + 1], ["bank%d" % (tl * 2), "bank%d" % (tl * 2 + 1)], m)
    c.pop()


def emit_peer4(c, K, layer, ntiles):
    emit_peer_prep(c, K, layer)
    c.push()
    GT = 2
    wqb = c.sb("wqb", [128, 8, 2048], BF16)
    c.push()
    wst = [c.sb("wqst%d" % i, [128, 2048]) for i in range(2)]
    for ch in range(8):
        c.dma(wst[ch % 2][:], K.peer_w_q[layer, ch * 128:(ch + 1) * 128, :], [], ["wqst%d" % (ch % 2)], e="sp" if ch % 2 else "pool")
        c.cp("dve" if ch % 2 else "pool", wqb[:, ch, :], wst[ch % 2][:], ["wqst%d" % (ch % 2)], ["wqb"])
    c.pop()
    skf = c.sb("skf", [128, 2, 128])
    skb = c.sb("skb", [128, 2, 128], BF16)
    skT = c.sb("skT", [128, 2, 128], BF16)
    Bk = [c.ps("bank%d" % i, [128, 512]) for i in range(8)]
    Bkb = [b[:].bitcast(BF16) for b in Bk]
    for p in range(2):
        c.dma(skf[:, p, :], K.peer_sk[layer, p], [], ["skf"])
    c.cp("dve", skb[:], skf[:], ["skf"], ["skb"])
    for p in range(2):
        c.tr(Bkb[7][:, p * 128:(p + 1) * 128], skb[:, p, :], K.identb[:], ["skb", "identb"], ["bank7"])
    c.cp("dve", skT[:].rearrange("p a k -> p (a k)"), Bkb[7][:, 0:256], ["bank7"], ["skT"])
    iotaj = c.sb("iotaj", [128, 128])
    iota_a = c.sb("iota_a", [128, 8, 16, 16])
    c.dma(iotaj[:], K.iotaj_in, [], ["iotaj"])
    c.dma(iota_a[:].rearrange("p h r a -> p (h r a)"), K.iotaa_in, [], ["iota_a"])

    W = alloc_normwork(c, Bkb[7])
    W.ptrkey = "bank7"
    xTg = [c.sb("xTg%d" % i, [128, 8, GT * 128], BF16) for i in range(2)]
    qb16 = c.sb("qb16", [128, 512], BF16)
    qT = c.sb("qT", [128, 4, 128], BF16)
    S = c.sb("S", [128, 16, 128])
    SV = c.sb("SV", [128, 16, 16])
    SIu = c.sb("SIu", [128, 16, 16], U32)
    SIf = c.sb("SIf", [128, 16, 16])
    wk = c.sb("wk", [128, 256])
    cand = c.sb("cand", [128, 8, 256])
    eq = cand[:].rearrange("p h (a b) -> p h a b", a=16)
    CV = c.sb("CV", [128, 8, 16])
    CPu = c.sb("CPu", [128, 8, 16], U32)
    CPf = c.sb("CPf", [128, 8, 16])
    ab = c.sb("abf", [128, 2, 8, 16])
    IJG = c.sb("IJG", [128, 3, 128])
    IJGT = [[c.sb("IJGT%d%d" % (i, j), [128, 3, 128]) for j in range(GT)] for i in range(2)]
    ez = c.sb("ez", [128, 8, 16])
    st8 = c.sb("st8", [128, 16])
    NB = 24
    At = [c.sb("At%d" % i, [128, 64], BF16) for i in range(NB)]
    Bt = [c.sb("Bt%d" % i, [128, 128], BF16) for i in range(NB)]
    GG = [c.sb("GG%d" % i, [128, GT * 128, 64], BF16) for i in range(2)]
    NR = 3
    uTi = [c.sb("uTi%d" % i, [128, 8, 128], BF16) for i in range(NR)]
    vi = [c.sb("vi%d" % i, [128, 1024], BF16) for i in range(NR)]
    gl = [c.sb("gl%d" % i, [128, GT * 128], BF16) for i in range(2)]
    wT = [c.sb("wT%d" % i, [128, GT * 128], BF16) for i in range(2)]
    ring = Prog()
    ring.e = 0
    ring.pb = 0
    ngroups = ntiles // GT

    def P1(g, tl):
        t = g * GT + tl
        m = 0 if t < NTL else 1
        xg, xgk = xTg[g % 2], "xTg%d" % (g % 2)
        emit_norm_T_peer(c, K, W, hrows(K, t), "H%d" % t, m, xg[:, :, tl * 128:(tl + 1) * 128], xgk)
        yield
        for qq in range(4):
            for ch in range(8):
                c.mm(Bk[6][:, :], xg[:, ch, tl * 128:(tl + 1) * 128], wqb[:, ch, qq * 512:(qq + 1) * 512], ch == 0, ch == 7, [xgk, "wqb"], ["bank6"])
            c.cp("act", qb16[:, :], Bk[6][:, :], ["bank6"], ["qb16"])
            yield
            for b in range(4):
                c.tr(Bkb[7][:, b * 128:(b + 1) * 128], qb16[:, b * 128:(b + 1) * 128], K.identb[:], ["qb16", "identb"], ["bank7"])
            c.cp("act", qT[:].rearrange("p b t -> p (b t)"), Bkb[7][:, 0:512], ["bank7"], ["qT"])
            yield
            for b in range(4):
                c.mm(Bk[6][:, b * 128:(b + 1) * 128], qT[:, b, :], skT[:, b % 2, :], True, True, ["qT", "skT"], ["bank6"])
            c.cp("act", S[:, qq * 4:(qq + 1) * 4, :].rearrange("p b k -> p (b k)"), Bk[6][:, :], ["bank6"], ["S"])
            yield
        for hp in range(16):
            c.op("dve", lambda o: o.max(out=SV[:, hp, 0:8], in_=S[:, hp, :]), ["S"], ["SV"])
            c.op("dve", lambda o: o.max_index(out=SIu[:, hp, 0:8], in_max=SV[:, hp, 0:8], in_values=S[:, hp, :]), ["S", "SV"], ["SIu"])
            c.op("dve", lambda o: o.match_replace(out=wk[:, 0:128], in_to_replace=SV[:, hp, 0:8], in_values=S[:, hp, :], imm_value=-1e30),
                 ["S", "SV"], ["wk"])
            c.op("dve", lambda o: o.max(out=SV[:, hp, 8:16], in_=wk[:, 0:128]), ["wk"], ["SV"])
            c.op("dve", lambda o: o.max_index(out=SIu[:, hp, 8:16], in_max=SV[:, hp, 8:16], in_values=wk[:, 0:128]), ["wk", "SV"], ["SIu"])
            if hp % 2 == 1:
                yield
        c.cp("dve", SIf[:], SIu[:], ["SIu"], ["SIf"])
        SV8 = SV[:].rearrange("p (h two) a -> p h two a", h=8, two=2)
        SI8 = SIf[:].rearrange("p (h two) a -> p h two a", h=8, two=2)
        c.tt("dve", eq, SV8[:, :, 0, :].unsqueeze(3).to_broadcast([128, 8, 16, 16]),
             SV8[:, :, 1, :].unsqueeze(2).to_broadcast([128, 8, 16, 16]), ALU.add, ["SV"], ["cand"])
        yield
        for h in range(8):
            c.op("dve", lambda o: o.max(out=CV[:, h, 0:8], in_=cand[:, h, :]), ["cand"], ["CV"])
            c.op("dve", lambda o: o.max_index(out=CPu[:, h, 0:8], in_max=CV[:, h, 0:8], in_values=cand[:, h, :]), ["cand", "CV"], ["CPu"])
            c.op("dve", lambda o: o.match_replace(out=wk[:, :], in_to_replace=CV[:, h, 0:8], in_values=cand[:, h, :], imm_value=-1e30),
                 ["cand", "CV"], ["wk"])
            c.op("dve", lambda o: o.max(out=CV[:, h, 8:16], in_=wk[:, :]), ["wk"], ["CV"])
            c.op("dve", lambda o: o.max_index(out=CPu[:, h, 8:16], in_max=CV[:, h, 8:16], in_values=wk[:, :]), ["wk", "CV"], ["CPu"])
            if h % 2 == 1:
                yield
        c.cp("dve", CPf[:], CPu[:], ["CPu"], ["CPf"])
        c.ts("dve", ab[:, 1], CPf[:], 1.0 / 16, -1.0, ALU.mult, ALU.add, ["CPf"], ["abf"])
        c.tt("dve", eq, ab[:, 1].unsqueeze(3).to_broadcast([128, 8, 16, 16]), iota_a[:], ALU.is_ge, ["abf", "iota_a"], ["cand"])
        c.op("dve", lambda o: o.tensor_reduce(out=ab[:, 0], in_=eq, axis=AX.X, op=ALU.add), ["cand"], ["abf"])
        c.op("dve", lambda o: o.scalar_tensor_tensor(out=ab[:, 1], in0=ab[:, 0], scalar=-16.0, in1=CPf[:], op0=ALU.mult, op1=ALU.add), ["abf", "CPf"], ["abf"])
        yield
        IJ4 = IJG[:].rearrange("p c (h r) -> p c h r", h=8)
        for which in range(2):
            c.tt("dve", eq, ab[:, which].unsqueeze(3).to_broadcast([128, 8, 16, 16]), iota_a[:], ALU.is_equal, ["abf", "iota_a"], ["cand"])
            c.tt("dve", eq, eq, SI8[:, :, which, :].unsqueeze(2).to_broadcast([128, 8, 16, 16]), ALU.mult, ["cand", "SIf"], ["cand"])
            c.op("dve", lambda o: o.tensor_reduce(out=IJ4[:, which], in_=eq, axis=AX.X, op=ALU.add), ["cand"], ["IJG"])
            yield
        c.tt("dve", ez[:], CV[:], CV[:, :, 0:1].to_broadcast([128, 8, 16]), ALU.subtract, ["CV"], ["ez"])
        c.act(ez[:].rearrange("p h a -> p (h a)"), ez[:].rearrange("p h a -> p (h a)"), AF.Exp, ["ez"], ["ez"])
        c.op("dve", lambda o: o.tensor_reduce(out=st8[:, 0:8], in_=ez[:], axis=AX.X, op=ALU.add), ["ez"], ["st8a"])
        c.op("dve", lambda o: o.reciprocal(out=st8[:, 8:16], in_=st8[:, 0:8]), ["st8a"], ["st8b"])
        c.tt("dve", IJ4[:, 2], ez[:], st8[:, 8:16].unsqueeze(2).to_broadcast([128, 8, 16]), ALU.mult, ["ez", "st8b"], ["IJG"])
        yield
        for w_ in range(3):
            c.tr(Bk[6][:, w_ * 128:(w_ + 1) * 128], IJG[:, w_, :], K.identf[:], ["IJG", "identf"], ["bank6"])
        T3, T3k = IJGT[g % 2][tl], "IJGT%d%d" % (g % 2, tl)
        c.cp("act", T3[:].rearrange("p c t -> p (c t)"), Bk[6][:, 0:384], ["bank6"], [T3k])
        yield

    def expand(g, half, tl):
        T3, T3k = IJGT[g % 2][tl], "IJGT%d%d" % (g % 2, tl)
        G_, Gk = GG[half], "GG%d" % half
        io_h = iotaj[:, half * 64:(half + 1) * 64]
        for tk in range(128):
            k = ring.e % NB
            ring.e += 1
            if tk % 8 == 0:
                ring.pb += 1
            pb_, pbk = Bk[6 + ring.pb % 2], "bank%d" % (6 + ring.pb % 2)
            c.ts("dve", Bt[k][:], iotaj[:], T3[:, 1, tk:tk + 1], None, ALU.is_equal, None, ["iotaj", T3k], ["Bt%d" % k])
            c.ts("dve", At[k][:], io_h, T3[:, 0, tk:tk + 1], T3[:, 2, tk:tk + 1], ALU.is_equal, ALU.mult, ["iotaj", T3k], ["At%d" % k])
            c.mm(pb_[:, (tk % 8) * 64:(tk % 8 + 1) * 64], Bt[k][:], At[k][:], True, True, ["Bt%d" % k, "At%d" % k], [pbk])
            if tk % 8 == 7:
                tb = tl * 128 + tk - 7
                c.cp("act", G_[:, tb:tb + 8, :].rearrange("p t i -> p (t i)"), pb_[:, :], [pbk], [Gk])
                yield

    def dense(g, half):
        xg, xgk = xTg[g % 2], "xTg%d" % (g % 2)
        G_, Gk = GG[half], "GG%d" % half
        tiles = [g * GT + i for i in range(GT)]

        def issue_dma(i):
            u_, uk = uTi[i % NR], "uTi%d" % (i % NR)
            v_, vk = vi[i % NR], "vi%d" % (i % NR)
            c.dma(u_[:].rearrange("p c j -> p (c j)"), K.UT[i], ["UT"], [uk], e="sp")
            c.dma(v_[:], K.VB[i], ["VB"], [vk], e="sp")

        def issue_pre(i):
            u_, uk = uTi[i % NR], "uTi%d" % (i % NR)
            if i + 1 < 128:
                issue_dma(i + 1)
            pre = Bk[4 + i % 2][:, 0:256]
            for ch in range(8):
                c.mm(pre, u_[:, ch, :], xg[:, ch, :], ch == 0, ch == 7, [uk, xgk], ["bank%d" % (4 + i % 2)])

        def issue_gelu(i):
            pre = Bk[4 + i % 2][:, 0:256]
            c.act(gl[i % 2][:], pre, AF.Gelu, ["bank%d" % (4 + i % 2)], ["gl%d" % (i % 2)])

        if half == 0:
            issue_dma(0)
            issue_pre(0)
            issue_gelu(0)
        for i in range(half * 64, half * 64 + 64):
            if i + 1 < 128:
                issue_pre(i + 1)
            c.tt("dve", wT[i % 2][:], G_[:, :, i - half * 64], gl[i % 2][:], ALU.mult, [Gk, "gl%d" % (i % 2)], ["wT%d" % (i % 2)])
            if i + 1 < 128:
                issue_gelu(i + 1)
            v_, vk = vi[i % NR], "vi%d" % (i % NR)
            for tl in range(GT):
                for nb in range(2):
                    c.mm(Bk[tl * 2 + nb][:, :], wT[i % 2][:, tl * 128:(tl + 1) * 128], v_[:, nb * 512:(nb + 1) * 512], i == 0, i == 127,
                         ["wT%d" % (i % 2), vk], ["bank%d" % (tl * 2 + nb)])
            yield
        if half == 1:
            for tl, t in enumerate(tiles):
                m = 0 if t < NTL else 1
                emit_residual_banks(c, K, W, t, Bk[tl * 2], Bk[tl * 2 + 1], ["bank%d" % (tl * 2), "bank%d" % (tl * 2 + 1)], m)
            yield

    def chain(*gens):
        for g_ in gens:
            yield from g_

    _interleave([chain(P1(0, 0), P1(0, 1), expand(0, 0, 0), expand(0, 0, 1))])
    for g in range(ngroups):
        nxt = g + 1 < ngroups
        side = [expand(g, 1, 0), expand(g, 1, 1)]
        if nxt:
            side.append(P1(g + 1, 0))
        _interleave([dense(g, 0), chain(*side)])
        side = []
        if nxt:
            side = [P1(g + 1, 1), expand(g + 1, 0, 0), expand(g + 1, 0, 1)]
        _interleave([dense(g, 1), chain(*side)])
    c.pop()
```

```python
import numpy as np
from contextlib import ExitStack
import concourse.bass as bass
import concourse.mybir as mybir
from concourse.bass_utils import run_bass_kernel_spmd

F32 = mybir.dt.float32
BF16 = mybir.dt.bfloat16
AF = mybir.ActivationFunctionType
ALU = mybir.AluOpType
AX = mybir.AxisListType

EPOCH = 30000
NSLOT = 8
D = 1024
L = 4096
LC = 256
NT = 34
NTL = 32
EPS = 1e-6


class _Eng:
    def __init__(self, name, obj):
        self.name = name
        self.obj = obj
        self.sems = []
        self.count = 0
        self.waited = {}
        self.slots = []
        self.slot_i = 0
        self.ninstr = 0


class _Res:
    __slots__ = ("lw", "rd")

    def __init__(self):
        self.lw = None
        self.rd = {}


class Ctx:
    def __init__(self, nc):
        self.nc = nc
        self.es = ExitStack()
        self.semh = {}
        self.nsem = 0
        self.engs = {}
        for name, obj in (("pe", nc.tensor), ("dve", nc.vector), ("act", nc.scalar),
                          ("pool", nc.gpsimd), ("sp", nc.sync)):
            self.engs[name] = _Eng(name, obj)
        self.res = {}
        self.scopes = []

    def new_sem(self, name):
        self.nsem += 1
        h = self.es.enter_context(self.nc.semaphore("%s_%d" % (name, self.nsem)))
        key = "s%d_%s" % (self.nsem, name)
        self.semh[key] = h
        return key

    def push(self):
        self.scopes.append(ExitStack())

    def pop(self):
        self.barrier()
        self.scopes.pop().close()

    def _stk(self):
        return self.scopes[-1] if self.scopes else self.es

    def sb(self, name, shape, dt=F32):
        self.uid = getattr(self, "uid", 0) + 1
        return self._stk().enter_context(self.nc.sbuf_tensor("%s_s%d" % (name, self.uid), list(shape), dt))

    def ps(self, name, shape, dt=F32):
        self.uid = getattr(self, "uid", 0) + 1
        return self._stk().enter_context(self.nc.psum_tensor("%s_p%d" % (name, self.uid), list(shape), dt))

    def dram(self, name, shape, dt=F32):
        return self.nc.dram_tensor(name, list(shape), dt, kind="Internal").ap()

    def close(self):
        self.es.close()

    def _wait(self, E, tok):
        if tok is None:
            return
        sem, val = tok
        if E.waited.get(sem, 0) >= val:
            return
        E.obj.wait_ge(self.semh[sem], val)
        E.waited[sem] = val

    def _R(self, key):
        r = self.res.get(key)
        if r is None:
            r = self.res[key] = _Res()
        return r

    def op(self, e, fn, reads=(), writes=(), dma=False):
        E = self.engs[e]
        own = set(E.sems)
        need = []
        for k in reads:
            r = self._R(k)
            if r.lw is not None:
                need.append(r.lw)
        for k in writes:
            r = self._R(k)
            if r.lw is not None:
                need.append(r.lw)
            for s, v in r.rd.items():
                need.append((s, v))
        for tok in need:
            if e == "pe" and tok[0] in own:
                continue
            self._wait(E, tok)
        if dma:
            if not E.slots:
                for i in range(NSLOT):
                    E.slots.append([self.new_sem("%s_dma%d" % (e, i)), 0])
            sl = E.slots[E.slot_i % NSLOT]
            E.slot_i += 1
            if sl[1] > 0:
                self._wait(E, (sl[0], sl[1]))
            if sl[1] + 16 > EPOCH:
                sl[0] = self.new_sem("%s_dmaX" % e)
                sl[1] = 0
            ins = fn(E.obj)
            sl[1] += 16
            ins.then_inc(self.semh[sl[0]], 16)
            tok = (sl[0], sl[1])
        else:
            if not E.sems or E.count >= EPOCH:
                E.sems.append(self.new_sem(e))
                E.count = 0
            ins = fn(E.obj)
            E.count += 1
            ins.then_inc(self.semh[E.sems[-1]], 1)
            tok = (E.sems[-1], E.count)
        E.ninstr += 1
        for k in writes:
            r = self._R(k)
            r.lw = tok
            r.rd = {}
        for k in reads:
            if k in writes:
                continue
            r = self._R(k)
            r.rd[tok[0]] = max(r.rd.get(tok[0], 0), tok[1])
        return tok

    def barrier(self):
        toks = []
        for E in self.engs.values():
            if E.sems and E.count > 0:
                toks.append((E.sems[-1], E.count))
            for sl in E.slots:
                if sl[1] > 0:
                    toks.append((sl[0], sl[1]))
        for E in self.engs.values():
            own = set(E.sems)
            for t in toks:
                if t[0] in own:
                    continue
                self._wait(E, t)

    def finish(self, keys, e="sp"):
        E = self.engs[e]
        for k in keys:
            self._wait(E, self._R(k).lw)

    def dma(self, out, in_, reads, writes, e="sp", **kw):
        return self.op(e, lambda o: o.dma_start(out=out, in_=in_, **kw), reads, writes, dma=True)

    def mm(self, out, lhsT, rhs, start, stop, reads, writes):
        return self.op("pe", lambda o: o.matmul(out, lhsT, rhs, start=start, stop=stop), reads, writes)

    def tr(self, out, in_, ident, reads, writes):
        return self.op("pe", lambda o: o.transpose(out, in_, ident), reads, writes)

    def act(self, out, in_, func, reads, writes, **kw):
        return self.op("act", lambda o: o.activation(out=out, in_=in_, func=func, **kw), reads, writes)

    def tt(self, e, out, in0, in1, op, reads, writes):
        return self.op(e, lambda o: o.tensor_tensor(out=out, in0=in0, in1=in1, op=op), reads, writes)

    def ts(self, e, out, in0, s1, s2, op0, op1, reads, writes):
        if op1 is None:
            return self.op(e, lambda o: o.tensor_scalar(out=out, in0=in0, scalar1=s1, scalar2=None, op0=op0), reads, writes)
        return self.op(e, lambda o: o.tensor_scalar(out=out, in0=in0, scalar1=s1, scalar2=s2, op0=op0, op1=op1), reads, writes)

    def smul(self, e, out, in0, sc, reads, writes):
        if e == "act":
            return self.op(e, lambda o: o.activation(out=out, in_=in0, func=AF.Copy, scale=sc), reads, writes)
        return self.op(e, lambda o: o.tensor_scalar(out=out, in0=in0, scalar1=sc, scalar2=None, op0=ALU.mult), reads, writes)

    def cp(self, e, out, in_, reads, writes):
        if e == "act":
            return self.op(e, lambda o: o.copy(out=out, in_=in_), reads, writes)
        return self.op(e, lambda o: o.tensor_copy(out=out, in_=in_), reads, writes)


class Prog:
    pass


def _interleave(gens):
    alive = list(gens)
    while alive:
        for g in list(alive):
            try:
                next(g)
            except StopIteration:
                alive.remove(g)


def hrows(K, t):
    return K.H[t * 128:(t + 1) * 128, :]


def src_rows(K, layer, t):
    if layer == 0:
        if t < NTL:
            return K.x[t * 128:(t + 1) * 128, :], "x"
        return K.ctx[(t - NTL) * 128:(t - NTL + 1) * 128, :], "ctx"
    return hrows(K, t), "H%d" % t


def emit_modulation(c, K, layer):
    c.push()
    wbuf = [c.sb("modw%d" % i, [128, 3072]) for i in range(4)]
    pm = [c.ps("pm%d" % i, [128, 512]) for i in range(6)]
    pcol = c.ps("pcol", [128, 64])
    pg = c.ps("pgate", [128, 512])
    modb = c.sb("modb", [2, 6144])
    K.modrow = c.sb("modrow", [2, 6144])
    c.dma(modb[0:1, :], K.mod_b[layer:layer + 1, :], [], ["modb"])
    c.dma(modb[1:2, :], K.mod_b[layer:layer + 1, :], [], ["modb"])
    n = 0
    for half in range(2):
        for ch in range(8):
            wb = wbuf[n % 4]
            wk = "modw%d" % (n % 4)
            n += 1
            c.dma(wb[:], K.mod_w[layer, ch * 128:(ch + 1) * 128, half * 3072:(half + 1) * 3072], [], [wk],
                  e=("sp", "pool", "act")[n % 3])
            for j in range(6):
                c.mm(pm[j][0:2, :], K.scT[:, ch, :], wb[:, j * 512:(j + 1) * 512], ch == 0, ch == 7,
                     [wk, "scT"], ["pm%d" % j])
        for j in range(6):
            col = half * 3072 + j * 512
            c.tt("dve", K.modrow[0:2, col:col + 512], pm[j][0:2, :], modb[0:2, col:col + 512], ALU.add,
                 ["pm%d" % j, "modb"], ["modrow"])
    for si, seg in enumerate((0, 1, 3, 4)):
        for ch in range(8):
            idx = si * 8 + ch
            c.mm(pcol[:, idx * 2:idx * 2 + 2], K.modrow[0:2, seg * 1024 + ch * 128: seg * 1024 + (ch + 1) * 128],
                 K.identf[0:2, 0:2], True, True, ["modrow", "identf"], ["pcol"])
    colv = c.sb("colv", [128, 64])
    c.cp("dve", colv[:], pcol[:], ["pcol"], ["colv"])
    cv = colv[:].rearrange("p (s c m) -> p s c m", s=4, c=8, m=2)
    for m in range(2):
        for which, (ssc, ssh, g) in enumerate(((1, 0, K.g1col), (3, 2, K.g2col))):
            sc = K.scol[m][which]
            c.ts("dve", sc[:], cv[:, ssc, :, m], 1.0, None, ALU.add, None, ["colv"], ["scol%d%d" % (m, which)])
            c.tt("dve", sc[:], sc[:], g[:, layer, :], ALU.mult, ["scol%d%d" % (m, which), "gcol"], ["scol%d%d" % (m, which)])
            c.cp("dve", K.shcol[m][which][:], cv[:, ssh, :, m], ["colv"], ["shcol%d%d" % (m, which)])
    for m in range(2):
        for which, seg in enumerate((2, 5)):
            for half in range(2):
                c.mm(pg[:, :], K.sel[0:2, m, :], K.modrow[0:2, seg * 1024 + half * 512: seg * 1024 + (half + 1) * 512],
                     True, True, ["modrow", "sel"], ["pgate"])
                c.cp("act", K.gbc[m][which][:, half * 512:(half + 1) * 512], pg[:, :], ["pgate"], ["gbc%d%d" % (m, which)])
    c.pop()


def emit_norm_T(c, K, W, src, srckey, m, which, aT_out, aTkey, keep_h=None):
    W.n += 1
    i = W.n % 2
    hb, hk = W.hbuf[i], "hbuf%d" % i
    c.dma(hb[:], src, [srckey], [hk], e="sp" if W.n % 2 else "pool")
    c.tt("dve", W.junk[:], hb[:], hb[:], ALU.mult, [hk], ["junk"])
    c.op("dve", lambda o: o.tensor_reduce(out=W.ss[:, 0:1], in_=W.junk[:], axis=AX.X, op=ALU.add), ["junk"], ["ss"])
    c.ts("dve", W.ss[:, 1:2], W.ss[:, 0:1], 1.0 / D, EPS, ALU.mult, ALU.add, ["ss"], ["ss1"])
    c.act(W.ss[:, 3:4], W.ss[:, 1:2], AF.Sqrt, ["ss1"], ["ss3"])
    c.op("dve", lambda o: o.reciprocal(out=W.ss[:, 2:3], in_=W.ss[:, 3:4]), ["ss3"], ["ss2"])
    c.ts("dve", W.hn[:], hb[:], W.ss[:, 2:3], None, ALU.mult, None, [hk, "ss2"], ["hn"])
    for ch in range(8):
        c.tr(W.ptr[:, ch * 128:(ch + 1) * 128], W.hn[:, ch * 128:(ch + 1) * 128], K.identb[:], ["hn", "identb"], ["ptr"])
    pv = W.ptr[:, 0:1024].rearrange("p (c t) -> p c t", c=8)
    sc = K.scol[m][which][:].unsqueeze(2).to_broadcast([128, 8, 128])
    sh = K.shcol[m][which][:].unsqueeze(2).to_broadcast([128, 8, 128])
    c.tt("dve", W.tmpf[:].rearrange("p (c t) -> p c t", c=8), pv, sc, ALU.mult, ["ptr", "scol%d%d" % (m, which)], ["tmpf"])
    c.tt("dve", aT_out, W.tmpf[:].rearrange("p (c t) -> p c t", c=8), sh, ALU.add, ["tmpf", "shcol%d%d" % (m, which)], [aTkey])


def alloc_normwork(c, ptr):
    W = Prog()
    W.n = 0
    W.hbuf = [c.sb("hbuf%d" % i, [128, 1024]) for i in range(2)]
    W.junk = c.sb("junk", [128, 1024])
    W.tmpf = c.sb("tmpf", [128, 1024])
    W.ss = c.sb("ss", [128, 4])
    W.hn = c.sb("hn", [128, 1024], BF16)
    W.ptr = ptr
    return W


def emit_residual(c, K, W, layer_src, py, pykeys, m, which, t, first_layer_src=None):
    src, srckey = layer_src
    W.n += 1
    i = W.n % 2
    hb, hk = W.hbuf[i], "hbuf%d" % i
    c.dma(hb[:], src, [srckey], [hk], e="sp" if W.n % 2 else "pool")
    for hb_ in range(2):
        c.tt("dve", W.tmpf[:, hb_ * 512:(hb_ + 1) * 512], py[:, hb_ * 512:(hb_ + 1) * 512], K.gbc[m][which][:, hb_ * 512:(hb_ + 1) * 512],
             ALU.mult, list(pykeys) + ["gbc%d%d" % (m, which)], ["tmpf"])
    c.tt("dve", hb[:], hb[:], W.tmpf[:], ALU.add, [hk, "tmpf"], [hk])
    c.dma(hrows(K, t), hb[:], [hk], ["H%d" % t], e="sp")


def emit_attention(c, K):
    layer = 0
    c.push()
    kTa = [c.sb("kTa%d" % i, [128, NT * 128], BF16) for i in range(2)]
    kTb = [c.sb("kTb%d" % i, [128, NT * 128], BF16) for i in range(2)]
    Va = c.sb("Va", [128, NT, 2, 128], BF16)
    Vb = c.sb("Vb", [128, NT, 2, 128], BF16)
    for i in range(2):
        c.op("pool", lambda o: o.memset(kTa[i][:], 0.0), [], ["kTa"])
        c.op("pool", lambda o: o.memset(kTb[i][:], 0.0), [], ["kTb"])
    c.op("pool", lambda o: o.memset(Va[:], 0.0), [], ["Va"])
    c.op("pool", lambda o: o.memset(Vb[:], 0.0), [], ["Vb"])
    c.op("pool", lambda o: o.memset(Va[:, :, :, 64:65], 1.0), ["Va"], ["Va"])
    c.op("pool", lambda o: o.memset(Vb[:, :, :, 64:65], 1.0), ["Vb"], ["Vb"])
    K.qg = c.sb("qgs", [128, 640])
    K.sc10 = c.sb("sc10s", [128, 640])
    K.sink = c.sb("sinks", [128, 8])
    mk = c.sb("masksf", [128, 2, 128])
    mkb = c.sb("masksb", [128, 2, 128], BF16)
    K.maskLO = mkb[:, 0, :]
    K.maskUP = mkb[:, 1, :]
    c.dma(K.qg[:], K.qg_in, [], ["qg"])
    c.dma(K.sc10[:], K.sc10_in, [], ["sc10"])
    c.dma(K.sink[64:65, :], K.sink_in, [], ["sink"])
    c.dma(mk[:], K.masks_in, [], ["masksf"])
    c.cp("dve", mkb[:], mk[:], ["masksf"], ["masks"])
    c.push()
    winb = c.sb("winb", [128, 8, 1536], BF16)
    wst = [c.sb("wst%d" % i, [128, 1536]) for i in range(2)]
    for ch in range(8):
        c.dma(wst[ch % 2][:], K.att_w_in[ch * 128:(ch + 1) * 128, :], [], ["wst%d" % (ch % 2)], e="sp" if ch % 2 else "pool")
        c.cp("dve" if ch % 2 else "pool", winb[:, ch, :], wst[ch % 2][:], ["wst%d" % (ch % 2)], ["winb"])
    ptr = c.ps("ptr", [128, 1024], BF16)
    W = alloc_normwork(c, ptr)
    pp = [c.ps("pproj%d" % i, [128, 512]) for i in range(3)]
    ptq = c.ps("ptq", [128, 1280], BF16)
    aT = [c.sb("aT%d" % i, [128, 8, 128], BF16) for i in range(2)]
    pj = c.sb("pj", [128, 1536])
    G10 = c.sb("G10", [128, 640])
    c.tt("dve", G10[:], K.qg[:], K.sc10[:], ALU.mult, ["qg", "sc10"], ["G10"])
    sq = c.sb("sq", [128, 640])
    st10 = c.sb("st10", [128, 32])
    X1 = c.sb("X1", [128, 640])
    X2 = c.sb("X2", [128, 640])
    R1 = c.sb("R1", [128, 640])
    R2 = c.sb("R2", [128, 640])
    cosf = [c.sb("cosf%d" % i, [128, 64]) for i in range(2)]
    sinf = [c.sb("sinf%d" % i, [128, 64]) for i in range(2)]
    ST = c.sb("ST", [128, 1280], BF16)
    qst = [c.sb("qst%d" % i, [128, 10, 128], BF16) for i in range(2)]
    for t in range(NT):
        m = 0 if t < NTL else 1
        src, sk = src_rows(K, layer, t)
        a, ak = aT[t % 2], "aT%d" % (t % 2)
        emit_norm_T(c, K, W, src, sk, m, 0, a[:], ak)
        for nb in range(3):
            for ch in range(8):
                c.mm(pp[nb][:, :], a[:, ch, :], winb[:, ch, nb * 512:(nb + 1) * 512], ch == 0, ch == 7, [ak, "winb"], ["pproj%d" % nb])
        for nb in range(3):
            c.cp("act", pj[:, nb * 512:(nb + 1) * 512], pp[nb][:, :], ["pproj%d" % nb], ["pj"])
        c.cp("act", Va[:, t, :, 0:64], pj[:, 640:768].rearrange("p (k d) -> p k d", k=2), ["pj"], ["Va"])
        c.cp("act", Vb[:, t, :, 0:64], pj[:, 1408:1536].rearrange("p (k d) -> p k d", k=2), ["pj"], ["Vb"])
        c.tt("dve", sq[:], pj[:, 0:640], pj[:, 0:640], ALU.mult, ["pj"], ["sq"])
        c.op("dve", lambda o: o.tensor_reduce(out=st10[:, 0:10], in_=sq[:].rearrange("p (h d) -> p h d", h=10), axis=AX.X, op=ALU.add), ["sq"], ["st10a"])
        c.ts("dve", st10[:, 10:20], st10[:, 0:10], 1.0 / 64, EPS, ALU.mult, ALU.add, ["st10a"], ["st10b"])
        c.act(st10[:, 10:20], st10[:, 10:20], AF.Sqrt, ["st10b"], ["st10b"])
        c.op("dve", lambda o: o.reciprocal(out=st10[:, 20:30], in_=st10[:, 10:20]), ["st10b"], ["st10c"])
        c.tt("dve", X1[:].rearrange("p (h d) -> p h d", h=10), pj[:, 0:640].rearrange("p (h d) -> p h d", h=10),
             st10[:, 20:30].unsqueeze(2).to_broadcast([128, 10, 64]), ALU.mult, ["pj", "st10c"], ["X1"])
        c.tt("dve", X1[:], X1[:], G10[:], ALU.mult, ["X1", "G10"], ["X1"])
        c.tt("dve", X2[:], pj[:, 768:1408], K.sc10[:], ALU.mult, ["pj", "sc10"], ["X2"])
        if t < NTL:
            cf, sf = cosf[t % 2], sinf[t % 2]
            c.dma(cf[:], K.cosf[t * 128:(t + 1) * 128, :], [], ["cosf%d" % (t % 2)])
            c.dma(sf[:], K.sinf[t * 128:(t + 1) * 128, :], [], ["sinf%d" % (t % 2)])
        for bi, (X, xk) in enumerate(((X1, "X1"), (X2, "X2"))):
            if t < NTL:
                ck, sk2 = "cosf%d" % (t % 2), "sinf%d" % (t % 2)
                e1 = "dve"
                c.tt(e1, R1[:].rearrange("p (h d) -> p h d", h=10), X[:].rearrange("p (h d) -> p h d", h=10),
                     cf[:].unsqueeze(1).to_broadcast([128, 10, 64]), ALU.mult, [xk, ck], ["R1"])
                X5 = X[:].rearrange("p (h g s f) -> p h g s f", h=10, g=2, s=2, f=16)
                R5 = R2[:].rearrange("p (h g s f) -> p h g s f", h=10, g=2, s=2, f=16)
                S4 = sf[:].rearrange("p (g s f) -> p g s f", g=2, s=2, f=16)
                for s in range(2):
                    c.tt(e1, R5[:, :, :, s, :], X5[:, :, :, 1 - s, :], S4[:, :, s, :].unsqueeze(1).to_broadcast([128, 10, 2, 16]),
                         ALU.mult, [xk, sk2], ["R2"])
                srcq0, srcq1, rk = R1, R2, ["R1", "R2"]
            else:
                srcq0, srcq1, rk = X, None, [xk]
            b0 = bi * 5 * 128
            qout = ST[:, b0:b0 + 512].rearrange("p (pp half d) -> p half pp d", pp=4, half=2, d=64)
            kout = ST[:, b0 + 512:b0 + 640]
            qi0 = srcq0[:, 0:512].rearrange("p (half pp d) -> p half pp d", half=2, pp=4, d=64)
            if srcq1 is not None:
                qi1 = srcq1[:, 0:512].rearrange("p (half pp d) -> p half pp d", half=2, pp=4, d=64)
                c.tt("dve", qout, qi0, qi1, ALU.add, rk, ["ST"])
                c.tt("dve", kout, srcq0[:, 512:640], srcq1[:, 512:640], ALU.add, rk, ["ST"])
            else:
                c.cp("dve", qout, qi0, rk, ["ST"])
                c.cp("act", kout, srcq0[:, 512:640], rk, ["ST"])
        for blk in range(10):
            c.tr(ptq[:, blk * 128:(blk + 1) * 128], ST[:, blk * 128:(blk + 1) * 128], K.identb[:], ["ST", "identb"], ["ptq"])
        qs, qk_ = qst[t % 2], "qst%d" % (t % 2)
        c.cp("act", qs[:].rearrange("p b t -> p (b t)"), ptq[:, :], ["ptq"], [qk_])
        for hf in range(2):
            c.cp("act", kTa[hf][hf * 64:(hf + 1) * 64, t * 128:(t + 1) * 128], qs[hf * 64:(hf + 1) * 64, 4, :], [qk_], ["kTa"])
            c.cp("act", kTb[hf][hf * 64:(hf + 1) * 64, t * 128:(t + 1) * 128], qs[hf * 64:(hf + 1) * 64, 9, :], [qk_], ["kTb"])
        c.dma(K.QTA[:, :, t * 128:(t + 1) * 128], qs[:, 0:4, :], [qk_], ["QTA"], e="sp")
        c.dma(K.QTB[:, :, t * 128:(t + 1) * 128], qs[:, 5:9, :], [qk_], ["QTB"], e="sp")
    c.pop()
    c.push()
    woutb = c.sb("woutb", [64, 16, 1024], BF16)
    c.push()
    wst2 = [c.sb("wst2_%d" % i, [64, 4, 1024]) for i in range(2)]
    wo = K.att_w_out.rearrange("(hh d) n -> d hh n", d=64)
    for g in range(4):
        c.dma(wst2[g % 2][:], wo[:, g * 4:(g + 1) * 4, :], [], ["wst2_%d" % (g % 2)], e="sp" if g % 2 else "pool")
        c.cp("dve" if g % 2 else "pool", woutb[:, g * 4:(g + 1) * 4, :], wst2[g % 2][:], ["wst2_%d" % (g % 2)], ["woutb"])
    c.pop()
    pS = [c.ps("pS%d" % i, [128, 512]) for i in range(2)]
    pO = [c.ps("pO%d" % i, [128, 512]) for i in range(2)]
    pB = c.ps("pB", [128, 512])
    pY = c.ps("pY", [128, 1024])
    PT = [c.sb("PT%d" % i, [128, 512], BF16) for i in range(3)]
    OTs = c.sb("OTs", [128, 512])
    rr = c.sb("rr", [128, 512])
    mixT = c.sb("mixT", [64, 16, 512], BF16)
    qa_t = [c.sb("qa_t%d" % i, [128, 4, 512], BF16) for i in range(2)]
    qb_t = [c.sb("qb_t%d" % i, [128, 4, 512], BF16) for i in range(2)]
    esink = c.sb("esink", [128, 8])
    c.act(esink[64:65, :], K.sink[64:65, :], AF.Exp, ["sink"], ["esink"])
    W = Prog()
    W.n = 0
    W.hbuf = [c.sb("hbuf%d" % i, [128, 1024]) for i in range(2)]
    W.tmpf = c.sb("tmpf", [128, 1024])
    cnt = Prog()
    cnt.s = 0
    cnt.o = 0

    def finalize(po, pok, NQ, mix_out, sink_ap=None):
        if sink_ap is not None:
            c.tt("dve", rr[64:65, 0:NQ].rearrange("p (h q) -> p h q", h=4), po[64:65, 0:NQ].rearrange("p (h q) -> p h q", h=4),
                 sink_ap, ALU.add, [pok, "esink"], ["rr"])
            c.op("dve", lambda o: o.reciprocal(out=rr[64:65, 0:NQ], in_=rr[64:65, 0:NQ]), ["rr"], ["rr"])
        else:
            c.op("dve", lambda o: o.reciprocal(out=rr[64:65, 0:NQ], in_=po[64:65, 0:NQ]), [pok], ["rr"])
        c.mm(pB[0:64, 0:NQ], K.onesf[64:65, 0:64], rr[64:65, 0:NQ], True, True, ["rr", "onesf"], ["pB"])
        c.cp("act", OTs[0:64, 0:NQ], po[0:64, 0:NQ], [pok], ["OTs"])
        c.tt("dve", mix_out, OTs[0:64, 0:NQ] if len(mix_out.shape) == 2 else OTs[0:64, 0:NQ].rearrange("p (h q) -> p h q", h=4),
             pB[0:64, 0:NQ] if len(mix_out.shape) == 2 else pB[0:64, 0:NQ].rearrange("p (h q) -> p h q", h=4),
             ALU.mult, ["OTs", "pB"], ["mixT"])

    qtiles = [(i * 512, 512) for i in range(8)] + [(L, 256)]
    for qi, (q0, NQ) in enumerate(qtiles):
        is_ctx = q0 >= L
        qa, qak = qa_t[qi % 2], "qa_t%d" % (qi % 2)
        qb, qbk = qb_t[qi % 2], "qb_t%d" % (qi % 2)
        c.dma(qa[:, :, 0:NQ], K.QTA[:, :, q0:q0 + NQ], ["QTA"], [qak], e="sp")
        c.dma(qb[:, :, 0:NQ], K.QTB[:, :, q0:q0 + NQ], ["QTB"], [qbk], e="pool")
        ktiles = [32, 33] if is_ctx else list(range(NT))
        for half in range(2):
            ps_ = slice(half * 64, (half + 1) * 64)
            for pq in range(4):
                h = half * 4 + pq
                po, pok = pO[cnt.o % 2], "pO%d" % (cnt.o % 2)
                cnt.o += 1
                def S_a(ki):
                    kt = ktiles[ki]
                    n_ = cnt.s + ki
                    c.mm(pS[n_ % 2][:, 0:NQ], kTa[half][:, kt * 128:(kt + 1) * 128], qa[:, pq, 0:NQ], True, True, ["kTa", qak], ["pS%d" % (n_ % 2)])

                S_a(0)
                for ki, kt in enumerate(ktiles):
                    n_ = cnt.s + ki
                    s_, sk_ = pS[n_ % 2], "pS%d" % (n_ % 2)
                    p_, pk_ = PT[n_ % 3], "PT%d" % (n_ % 3)
                    if ki + 1 < len(ktiles):
                        S_a(ki + 1)
                    c.act(p_[:, 0:NQ], s_[:, 0:NQ], AF.Exp, [sk_], [pk_])
                    c.mm(po[:, 0:NQ], Va[:, kt, half, :], p_[:, 0:NQ], ki == 0, ki == len(ktiles) - 1, ["Va", pk_], [pok])
                cnt.s += len(ktiles)
                finalize(po, pok, NQ, mixT[:, h, 0:NQ])
        for jb in range(NQ // 128):
            j = q0 // 128 + jb
            if is_ctx:
                kl = [(32, None), (33, None)]
            else:
                kl = [(32, None), (33, None)]
                if j - 1 >= 0:
                    kl.append((j - 1, "LO"))
                kl.append((j, None))
                if j + 1 < NTL:
                    kl.append((j + 1, "UP"))
            for half in range(2):
                ps_ = slice(half * 64, (half + 1) * 64)
                po, pok = pO[cnt.o % 2], "pO%d" % (cnt.o % 2)
                cnt.o += 1
                def S_b(ki):
                    kt = kl[ki][0]
                    n_ = cnt.s + ki
                    c.mm(pS[n_ % 2][:, :].rearrange("p (h q) -> p h q", h=4), kTb[half][:, kt * 128:(kt + 1) * 128], qb[:, :, jb * 128:(jb + 1) * 128],
                         True, True, ["kTb", qbk], ["pS%d" % (n_ % 2)])

                S_b(0)
                for ki, (kt, msk) in enumerate(kl):
                    n_ = cnt.s + ki
                    s_, sk_ = pS[n_ % 2], "pS%d" % (n_ % 2)
                    p_, pk_ = PT[n_ % 3], "PT%d" % (n_ % 3)
                    if ki + 1 < len(kl):
                        S_b(ki + 1)
                    c.act(p_[:, :], s_[:, :], AF.Exp, [sk_], [pk_])
                    if msk is not None:
                        mt = K.maskLO if msk == "LO" else K.maskUP
                        c.tt("pool", p_[:, :].rearrange("p (h q) -> p h q", h=4), p_[:, :].rearrange("p (h q) -> p h q", h=4),
                             mt[:].unsqueeze(1).to_broadcast([128, 4, 128]), ALU.mult, [pk_, "masks"], [pk_])
                    c.mm(po[:, :], Vb[:, kt, half, :], p_[:, :], ki == 0, ki == len(kl) - 1, ["Vb", pk_], [pok])
                cnt.s += len(kl)
                finalize(po, pok, 512, mixT[:, 8 + half * 4: 8 + half * 4 + 4, jb * 128:(jb + 1) * 128],
                         sink_ap=esink[64:65, half * 4:half * 4 + 4].unsqueeze(2).to_broadcast([1, 4, 128]))
        for tt_ in range(NQ // 128):
            t = q0 // 128 + tt_
            m = 1 if is_ctx else 0
            for nb in range(2):
                for hh in range(16):
                    c.mm(pY[:, nb * 512:(nb + 1) * 512], mixT[:, hh, tt_ * 128:(tt_ + 1) * 128], woutb[:, hh, nb * 512:(nb + 1) * 512],
                         hh == 0, hh == 15, ["mixT", "woutb"], ["pY"])
            emit_residual(c, K, W, src_rows(K, layer, t), pY[:, :], ["pY"], m, 0, t)
    c.pop()
    c.pop()


def emit_peer_prep(c, K, layer):
    c.push()
    ust = [c.sb("ust%d" % i, [128, 1024]) for i in range(2)]
    vst = [c.sb("vst%d" % i, [128, 1024]) for i in range(2)]
    ub = [c.sb("ub%d" % i, [128, 1024], BF16) for i in range(2)]
    vb = [c.sb("vb%d" % i, [128, 1024], BF16) for i in range(2)]
    uT = [c.sb("uTs%d" % i, [128, 1024], BF16) for i in range(2)]
    pu = [c.ps("pu%d" % i, [128, 1024], BF16) for i in range(2)]
    for i in range(128):
        k = i % 2
        c.dma(ust[k][:], K.peer_u[layer, i * 128:(i + 1) * 128, :], [], ["ust%d" % k], e="sp")
        c.dma(vst[k][:], K.peer_v[layer, i * 128:(i + 1) * 128, :], [], ["vst%d" % k], e="pool")
        c.cp("dve", ub[k][:], ust[k][:], ["ust%d" % k], ["ub%d" % k])
        c.cp("pool", vb[k][:], vst[k][:], ["vst%d" % k], ["vb%d" % k])
        for ch in range(8):
            c.tr(pu[k][:, ch * 128:(ch + 1) * 128], ub[k][:, ch * 128:(ch + 1) * 128], K.identb[:], ["ub%d" % k, "identb"], ["pu%d" % k])
        c.cp("act", uT[k][:], pu[k][:], ["pu%d" % k], ["uTs%d" % k])
        c.dma(K.UT[i], uT[k][:], ["uTs%d" % k], ["UT"], e="sp")
        c.dma(K.VB[i], vb[k][:], ["vb%d" % k], ["VB"], e="pool")
    c.pop()


def emit_peer(c, K, layer, ntiles):
    emit_peer_prep(c, K, layer)
    c.push()
    GT = 2
    wqb = c.sb("wqb", [128, 8, 2048], BF16)
    wst = [c.sb("wqst%d" % i, [128, 2048]) for i in range(2)]
    for ch in range(8):
        c.dma(wst[ch % 2][:], K.peer_w_q[layer, ch * 128:(ch + 1) * 128, :], [], ["wqst%d" % (ch % 2)], e="sp" if ch % 2 else "pool")
        c.cp("dve" if ch % 2 else "pool", wqb[:, ch, :], wst[ch % 2][:], ["wqst%d" % (ch % 2)], ["wqb"])
    skf = c.sb("skf", [128, 2, 128])
    skb = c.sb("skb", [128, 2, 128], BF16)
    skT = c.sb("skT", [128, 2, 128], BF16)
    Bk = [c.ps("bank%d" % i, [128, 512]) for i in range(8)]
    Bkb = [b[:].bitcast(BF16) for b in Bk]
    for p in range(2):
        c.dma(skf[:, p, :], K.peer_sk[layer, p], [], ["skf"])
    c.cp("dve", skb[:], skf[:], ["skf"], ["skb"])
    for p in range(2):
        c.tr(Bkb[7][:, p * 128:(p + 1) * 128], skb[:, p, :], K.identb[:], ["skb", "identb"], ["bank7"])
    c.cp("dve", skT[:].rearrange("p a k -> p (a k)"), Bkb[7][:, 0:256], ["bank7"], ["skT"])

    W = alloc_normwork(c, Bkb[5])
    W.ptrkey = "bank5"
    xTg = c.sb("xTg", [128, 8, GT * 128], BF16)
    qb16 = c.sb("qb16", [128, 1024], BF16)
    qT = c.sb("qT", [128, 8, 128], BF16)
    S = [c.sb("S%d" % i, [128, 16, 128]) for i in range(GT)]
    Bb = [c.sb("Bb%d" % i, [128, 8, 128]) for i in range(GT)]
    TH = [c.sb("TH%d" % i, [128, 8, 128]) for i in range(GT)]
    SV = c.sb("SV", [128, 16, 16])
    wk = c.sb("wk", [128, 256])
    cand = c.sb("cand", [128, 8, 256])
    CV = c.sb("CV", [128, 8, 16])
    ez = c.sb("ez", [128, 8, 16])
    st8 = c.sb("st8", [128, 40])
    NB = 3
    tmpr = [c.sb("tmpr%d" % i, [128, 8, 128], BF16) for i in range(NB)]
    msk = [c.sb("msk%d" % i, [128, 8, 128], BF16) for i in range(NB)]
    tm2 = [c.sb("tm2_%d" % i, [128, 8, 128], BF16) for i in range(NB)]
    E1 = [c.sb("E1_%d" % i, [128, 8, 128], BF16) for i in range(GT)]
    E2Z = [c.sb("E2Z_%d" % i, [128, 8, 128], BF16) for i in range(GT)]
    uTi = [c.sb("uTi%d" % i, [128, 8, 128], BF16) for i in range(3)]
    vi = [c.sb("vi%d" % i, [128, 1024], BF16) for i in range(3)]
    gl = [c.sb("gl%d" % i, [128, GT * 128], BF16) for i in range(2)]
    wT = [c.sb("wT%d" % i, [128, GT * 128], BF16) for i in range(2)]
    ring = Prog()
    ring.n = 0
    ring.u = 0
    ngroups = ntiles // GT
    for g in range(ngroups):
        tiles = [g * GT + i for i in range(GT)]
        for tl, t in enumerate(tiles):
            m = 0 if t < NTL else 1
            emit_norm_T_peer(c, K, W, hrows(K, t), "H%d" % t, m, xTg[:, :, tl * 128:(tl + 1) * 128], "xTg")
            for hh in range(2):
                for nb in range(2):
                    for ch in range(8):
                        c.mm(Bk[nb][:, :], xTg[:, ch, tl * 128:(tl + 1) * 128], wqb[:, ch, hh * 1024 + nb * 512: hh * 1024 + (nb + 1) * 512],
                             ch == 0, ch == 7, ["xTg", "wqb"], ["bank%d" % nb])
                for nb in range(2):
                    c.cp("act", qb16[:, nb * 512:(nb + 1) * 512], Bk[nb][:, :], ["bank%d" % nb], ["qb16"])
                for b in range(8):
                    c.tr(Bkb[2][:, b * 128:(b + 1) * 128], qb16[:, b * 128:(b + 1) * 128], K.identb[:], ["qb16", "identb"], ["bank2"])
                c.cp("dve", qT[:].rearrange("p b t -> p (b t)"), Bkb[2][:, :], ["bank2"], ["qT"])
                for b in range(8):
                    bank = 3 + b // 4
                    c.mm(Bk[bank][:, (b % 4) * 128:(b % 4 + 1) * 128], qT[:, b, :], skT[:, b % 2, :], True, True, ["qT", "skT"], ["bank%d" % bank])
                for bb in range(2):
                    c.cp("act", S[tl][:, hh * 8 + bb * 4: hh * 8 + (bb + 1) * 4, :].rearrange("p b k -> p (b k)"), Bk[3 + bb][:, :],
                         ["bank%d" % (3 + bb)], ["S%d" % tl])
            Sk = "S%d" % tl
            for hp in range(16):
                c.op("dve", lambda o: o.max(out=SV[:, hp, 0:8], in_=S[tl][:, hp, :]), [Sk], ["SV"])
                c.op("dve", lambda o: o.match_replace(out=wk[:, 0:128], in_to_replace=SV[:, hp, 0:8], in_values=S[tl][:, hp, :], imm_value=-1e30),
                     [Sk, "SV"], ["wk"])
                c.op("dve", lambda o: o.max(out=SV[:, hp, 8:16], in_=wk[:, 0:128]), ["wk"], ["SV"])
            SV8 = SV[:].rearrange("p (h two) a -> p h two a", h=8, two=2)
            c.tt("dve", cand[:].rearrange("p h (a b) -> p h a b", a=16), SV8[:, :, 0, :].unsqueeze(3).to_broadcast([128, 8, 16, 16]),
                 SV8[:, :, 1, :].unsqueeze(2).to_broadcast([128, 8, 16, 16]), ALU.add, ["SV"], ["cand"])
            for h in range(8):
                c.op("dve", lambda o: o.max(out=CV[:, h, 0:8], in_=cand[:, h, :]), ["cand"], ["CV"])
                c.op("dve", lambda o: o.match_replace(out=wk[:, :], in_to_replace=CV[:, h, 0:8], in_values=cand[:, h, :], imm_value=-1e30),
                     ["cand", "CV"], ["wk"])
                c.op("dve", lambda o: o.max(out=CV[:, h, 8:16], in_=wk[:, :]), ["wk"], ["CV"])
            c.tt("dve", ez[:], CV[:], CV[:, :, 0:1].to_broadcast([128, 8, 16]), ALU.subtract, ["CV"], ["ez"])
            c.act(ez[:].rearrange("p h a -> p (h a)"), ez[:].rearrange("p h a -> p (h a)"), AF.Exp, ["ez"], ["ez"])
            c.op("dve", lambda o: o.tensor_reduce(out=st8[:, 0:8], in_=ez[:], axis=AX.X, op=ALU.add), ["ez"], ["st8a"])
            c.act(st8[:, 8:16], st8[:, 0:8], AF.Ln, ["st8a"], ["st8b"])
            c.tt("dve", st8[:, 16:24], st8[:, 8:16], SV8[:, :, 1, 0], ALU.add, ["st8b", "SV"], ["st8c"])
            c.ts("dve", st8[:, 24:32], CV[:, :, 15], -1e-5, None, ALU.add, None, ["CV"], ["st8d"])
            S8 = S[tl][:].rearrange("p (h two) k -> p h two k", h=8, two=2)
            Bt, Btk = Bb[tl], "Bb%d" % tl
            c.tt("dve", Bt[:], S8[:, :, 0, :], SV8[:, :, 0, 0:1].to_broadcast([128, 8, 128]), ALU.subtract, [Sk, "SV"], [Btk])
            c.act(E1[tl][:].rearrange("p h k -> p (h k)"), Bt[:].rearrange("p h k -> p (h k)"), AF.Exp, [Btk], ["E1_%d" % tl])
            c.tt("dve", Bt[:], S8[:, :, 1, :], st8[:, 16:24].unsqueeze(2).to_broadcast([128, 8, 128]), ALU.subtract, [Sk, "st8c"], [Btk])
            c.act(E2Z[tl][:].rearrange("p h k -> p (h k)"), Bt[:].rearrange("p h k -> p (h k)"), AF.Exp, [Btk], ["E2Z_%d" % tl])
            c.tt("dve", TH[tl][:], st8[:, 24:32].unsqueeze(2).to_broadcast([128, 8, 128]), S8[:, :, 0, :], ALU.subtract, [Sk, "st8d"], ["TH%d" % tl])
        st_ = {}

        def issue_pre(i):
            u_, uk = uTi[i % 3], "uTi%d" % (i % 3)
            v_, vk = vi[i % 3], "vi%d" % (i % 3)
            c.dma(u_[:].rearrange("p c j -> p (c j)"), K.UT[i], ["UT"], [uk], e="sp")
            c.dma(v_[:], K.VB[i], ["VB"], [vk], e="sp")
            pre, prek = Bk[4 + i % 2], "bank%d" % (4 + i % 2)
            for ch in range(8):
                c.mm(pre[:, 0:GT * 128], u_[:, ch, :], xTg[:, ch, :], ch == 0, ch == 7, [uk, "xTg"], [prek])

        def issue_gelu(i):
            pre, prek = Bk[4 + i % 2], "bank%d" % (4 + i % 2)
            c.act(gl[i % 2][:], pre[:, 0:GT * 128], AF.Gelu, [prek], ["gl%d" % (i % 2)])

        def issue_w(i):
            gtb, gtk = Bk[6 + i % 2], "bank%d" % (6 + i % 2)
            c.tt("dve", wT[i % 2][:], gtb[:, 0:GT * 128], gl[i % 2][:], ALU.mult, [gtk, "gl%d" % (i % 2)], ["wT%d" % (i % 2)])

        def issue_y(i):
            v_, vk = vi[i % 3], "vi%d" % (i % 3)
            for tl in range(GT):
                for nb in range(2):
                    c.mm(Bk[tl * 2 + nb][:, :], wT[i % 2][:, tl * 128:(tl + 1) * 128], v_[:, nb * 512:(nb + 1) * 512], i == 0, i == 127,
                         ["wT%d" % (i % 2), vk], ["bank%d" % (tl * 2 + nb)])

        issue_pre(0)
        issue_gelu(0)
        for i in range(128):
            if i + 1 < 128:
                issue_pre(i + 1)
            gtb, gtk = Bk[6 + i % 2], "bank%d" % (6 + i % 2)
            for tl in range(GT):
                S8 = S[tl][:].rearrange("p (h two) k -> p h two k", h=8, two=2)
                k = ring.n % NB
                ring.n += 1
                c.tt("dve", tmpr[k][:], S8[:, :, 1, :], TH[tl][:, :, i:i + 1].to_broadcast([128, 8, 128]), ALU.is_ge,
                     ["S%d" % tl, "TH%d" % tl], ["tmpr%d" % k])
                c.tt("dve", tm2[k][:].rearrange("p h k -> p (h k)"), tmpr[k][:].rearrange("p h k -> p (h k)"), E2Z[tl][:].rearrange("p h k -> p (h k)"),
                     ALU.mult, ["tmpr%d" % k, "E2Z_%d" % tl], ["tm2_%d" % k])
                c.tt("pool", msk[k][:], tm2[k][:], E1[tl][:, :, i:i + 1].to_broadcast([128, 8, 128]), ALU.mult, ["tm2_%d" % k, "E1_%d" % tl], ["msk%d" % k])
                for h in range(8):
                    c.mm(gtb[:, tl * 128:(tl + 1) * 128], msk[k][:, h, :], K.identb[:], h == 0, h == 7, ["msk%d" % k, "identb"], [gtk])
            if i >= 1:
                issue_w(i - 1)
                issue_y(i - 1)
            if i + 1 < 128:
                issue_gelu(i + 1)
        issue_w(127)
        issue_y(127)
        for tl, t in enumerate(tiles):
            m = 0 if t < NTL else 1
            emit_residual_banks(c, K, W, t, Bk[tl * 2], Bk[tl * 2 + 1], ["bank%d" % (tl * 2), "bank%d" % (tl * 2 + 1)], m)
    c.pop()


def emit_norm_T_peer(c, K, W, src, srckey, m, aT_out, aTkey):
    W.n += 1
    i = W.n % 2
    hb, hk = W.hbuf[i], "hbuf%d" % i
    c.dma(hb[:], src, [srckey], [hk], e="sp" if W.n % 2 else "pool")
    c.tt("pool", W.junk[:], hb[:], hb[:], ALU.mult, [hk], ["junk"])
    c.op("dve", lambda o: o.tensor_reduce(out=W.ss[:, 0:1], in_=W.junk[:], axis=AX.X, op=ALU.add), ["junk"], ["ss"])
    c.ts("dve", W.ss[:, 1:2], W.ss[:, 0:1], 1.0 / D, EPS, ALU.mult, ALU.add, ["ss"], ["ss1"])
    c.act(W.ss[:, 3:4], W.ss[:, 1:2], AF.Sqrt, ["ss1"], ["ss3"])
    c.op("dve", lambda o: o.reciprocal(out=W.ss[:, 2:3], in_=W.ss[:, 3:4]), ["ss3"], ["ss2"])
    c.ts("dve", W.hn[:], hb[:], W.ss[:, 2:3], None, ALU.mult, None, [hk, "ss2"], ["hn"])
    for ch in range(8):
        c.tr(W.ptr[:, ch * 128:(ch + 1) * 128], W.hn[:, ch * 128:(ch + 1) * 128], K.identb[:], ["hn", "identb"], [W.ptrkey])
    pv = W.ptr[:, 0:1024].rearrange("p (c t) -> p c t", c=8)
    sc = K.scol[m][1][:].unsqueeze(2).to_broadcast([128, 8, 128])
    sh = K.shcol[m][1][:].unsqueeze(2).to_broadcast([128, 8, 128])
    c.tt("dve", W.tmpf[:].rearrange("p (c t) -> p c t", c=8), pv, sc, ALU.mult, [W.ptrkey, "scol%d1" % m], ["tmpf"])
    c.tt("pool", aT_out, W.tmpf[:].rearrange("p (c t) -> p c t", c=8), sh, ALU.add, ["tmpf", "shcol%d1" % m], [aTkey])


def emit_residual_banks(c, K, W, t, b0, b1, keys, m):
    W.n += 1
    i = W.n % 2
    hb, hk = W.hbuf[i], "hbuf%d" % i
    c.dma(hb[:], hrows(K, t), ["H%d" % t], [hk], e="sp" if W.n % 2 else "pool")
    g = K.gbc[m][1]
    c.tt("dve", W.tmpf[:, 0:512], b0[:, :], g[:, 0:512], ALU.mult, [keys[0], "gbc%d1" % m], ["tmpf"])
    c.tt("dve", W.tmpf[:, 512:1024], b1[:, :], g[:, 512:1024], ALU.mult, [keys[1], "gbc%d1" % m], ["tmpf"])
    c.tt("pool", hb[:], hb[:], W.tmpf[:], ALU.add, [hk, "tmpf"], [hk])
    c.dma(hrows(K, t), hb[:], [hk], ["H%d" % t], e="sp")


def emit_final(c, K):
    c.push()
    hb = [c.sb("fh%d" % i, [128, 1024]) for i in range(2)]
    junk = c.sb("fjunk", [128, 1024])
    ss = c.sb("fss", [128, 4])
    K.fg = c.sb("fgs", [128, 1024])
    c.dma(K.fg[:], K.fg_in, [], ["fg"])
    for t in range(NTL):
        h, hk = hb[t % 2], "fh%d" % (t % 2)
        c.dma(h[:], hrows(K, t), ["H%d" % t], [hk], e="sp" if t % 2 else "pool")
        c.tt("pool", junk[:], h[:], h[:], ALU.mult, [hk], ["fjunk"])
        c.op("dve", lambda o: o.tensor_reduce(out=ss[:, 0:1], in_=junk[:], axis=AX.X, op=ALU.add), ["fjunk"], ["fss"])
        c.ts("dve", ss[:, 1:2], ss[:, 0:1], 1.0 / D, EPS, ALU.mult, ALU.add, ["fss"], ["fss1"])
        c.act(ss[:, 3:4], ss[:, 1:2], AF.Sqrt, ["fss1"], ["fss3"])
        c.op("dve", lambda o: o.reciprocal(out=ss[:, 2:3], in_=ss[:, 3:4]), ["fss3"], ["fss2"])
        c.op("dve", lambda o: o.scalar_tensor_tensor(out=h[:], in0=h[:], scalar=ss[:, 2:3], in1=K.fg[:], op0=ALU.mult, op1=ALU.mult),
             [hk, "fss2", "fg"], [hk])
        c.dma(K.out[t * 128:(t + 1) * 128, :], h[:], [hk], ["out"], e="sp")
    c.pop()


def build_program(stages=("mod0", "att", "peer0", "mod1", "rec", "peer1", "final"), dump_h=False):
    nc = bass.Bass("TRN2", target_bir_lowering=False)
    K = Prog()

    def IN(name, shape, dt=F32):
        return nc.dram_tensor(name, list(shape), dt, kind="ExternalInput").ap()

    K.x = IN("x", [L, D])
    K.ctx = IN("ctx", [LC, D])
    K.ccT = IN("ccT", [128, 8, 2])
    K.mod_w = IN("mod_w", [2, D, 6 * D])
    K.mod_b = IN("mod_b", [2, 6 * D])
    K.gcols = IN("gcols", [128, 2, 2, 8])
    K.att_w_in = IN("att_w_in", [D, 1536])
    K.att_w_out = IN("att_w_out", [D, D])
    K.qg_in = IN("qg", [128, 640])
    K.sc10_in = IN("sc10", [128, 640])
    K.sink_in = IN("sink", [1, 8])
    K.cosf = IN("cosf", [L, 64])
    K.sinf = IN("sinf", [L, 64])
    K.masks_in = IN("masks", [128, 2, 128])
    K.ident_in = IN("ident", [128, 128])
    K.sel_in = IN("sel", [2, 2, 128])
    K.peer_w_q = IN("peer_w_q", [2, D, 2048])
    K.peer_sk = IN("peer_sk", [2, 2, 128, 128])
    K.peer_u = IN("peer_u", [2, 16384, D])
    K.peer_v = IN("peer_v", [2, 16384, D])
    K.fg_in = IN("fg", [128, D])
    K.iotaj_in = IN("iotaj", [128, 128])
    K.iotaa_in = IN("iotaa", [128, 2048])
    K.rec_w_in = IN("rec_w_in", [D, 4624])
    K.rec_w_out = IN("rec_w_out", [D, D])
    K.recc_in = IN("recc", [128, 12, 128])
    K.dmt_in = IN("dmt", [128, 2, 4, 128])
    K.decs_in = IN("decs", [128, 2, 2, 4])
    K.outg_in = IN("outg", [128, 512])
    K.gng_in = IN("gng", [128, 512])
    K.convw_in = IN("convw", [128, 12, 5])
    K.alog_in = IN("alog", [128, 8])
    K.dtb_in = IN("dtb", [128, 8])
    K.cos2 = IN("cos2", [L, 128])
    K.sin2 = IN("sin2", [L, 128])
    K.out = nc.dram_tensor("out", [L, D], F32, kind="ExternalOutput").ap()

    c = Ctx(nc)
    K.H = c.dram("H", [NT * 128, D]) if not dump_h else nc.dram_tensor("H", [NT * 128, D], F32, kind="ExternalOutput").ap()
    K.QTA = c.dram("QTA", [128, 4, NT * 128], BF16)
    K.QTB = c.dram("QTB", [128, 4, NT * 128], BF16)
    K.UT = c.dram("UT", [128, 128, 1024], BF16)
    K.VB = c.dram("VB", [128, 128, 1024], BF16)
    K.AT = c.dram("AT", [128, 8, NT * 128], BF16)
    K.TK = c.dram("TK", [NT, 128, 3088])
    K.FT2 = c.dram("FT2", [NT, 128, 8, 128])
    K.FT = c.dram("FT", [12, 128, NT * 128])
    K.TM = c.dram("TM", [12, NT, 128, 128])
    for nm in ("PU", "PNW", "PQG", "PQK", "PKE", "RQK", "RQQ", "RKD", "OG", "OR"):
        setattr(K, nm, c.dram(nm, [2, NT, 128, 512]))
    K.PEG = c.dram("PEG", [2, NT, 128, 4])

    K.identf = c.sb("identf", [128, 128])
    K.identb = c.sb("identb", [128, 128], BF16)
    K.onesf = c.sb("onesf", [128, 64])
    K.sel = c.sb("sel", [2, 2, 128])
    K.scT = c.sb("scT", [128, 8, 2])
    gc = c.sb("gcol", [128, 2, 2, 8])
    K.g1col = gc[:, 0]
    K.g2col = gc[:, 1]
    K.scol = [[c.sb("scol%d%d" % (m, w), [128, 8]) for w in range(2)] for m in range(2)]
    K.shcol = [[c.sb("shcol%d%d" % (m, w), [128, 8]) for w in range(2)] for m in range(2)]
    K.gbc = [[c.sb("gbc%d%d" % (m, w), [128, 1024]) for w in range(2)] for m in range(2)]

    c.dma(K.identf[:], K.ident_in, [], ["identf"])
    c.cp("dve", K.identb[:], K.identf[:], ["identf"], ["identb"])
    c.op("pool", lambda o: o.memset(K.onesf[:], 1.0), [], ["onesf"])
    c.dma(K.sel[:], K.sel_in, [], ["sel"])
    c.dma(K.scT[:], K.ccT, [], ["scT"])
    c.act(K.scT[:].rearrange("p c m -> p (c m)"), K.scT[:].rearrange("p c m -> p (c m)"), AF.Silu, ["scT"], ["scT"])
    c.dma(gc[:], K.gcols, [], ["gcol"])

    if "loadH" in stages:
        Hin = IN("Hin", [NT * 128, D])
        for t in range(NT):
            c.dma(hrows(K, t), Hin[t * 128:(t + 1) * 128, :], [], ["H%d" % t], e="sp" if t % 2 else "pool")
    if "mod0" in stages:
        emit_modulation(c, K, 0)
    if "att" in stages:
        emit_attention(c, K)
    if "peer0" in stages:
        emit_peer4(c, K, 0, NT)
    if "mod1" in stages:
        emit_modulation(c, K, 1)
    if "rec" in stages:
        emit_rec(c, K)
    if "peer1" in stages:
        emit_peer4(c, K, 1, NTL)
    if "final" in stages:
        emit_final(c, K)
    c.barrier()
    keys = ["out"] + ["H%d" % t for t in range(NT)]
    for e in ("sp", "pool", "act"):
        c.finish(keys, e=e)
    c.close()
    K.ctxobj = c
    return nc, K


def rope_tables():
    quarter = 16
    freqs = (10000.0 ** (-np.arange(quarter, dtype=np.float32) / quarter)).astype(np.float32)
    t = np.arange(L)
    row = (t // 64).astype(np.float32)
    col = (t % 64).astype(np.float32)
    ar = row[:, None] * freqs[None, :]
    ac = col[:, None] * freqs[None, :]
    cosf = np.concatenate([np.cos(ar), np.cos(ar), np.cos(ac), np.cos(ac)], axis=1).astype(np.float32)
    sinf = np.concatenate([-np.sin(ar), np.sin(ar), -np.sin(ac), np.sin(ac)], axis=1).astype(np.float32)
    return cosf, sinf


def make_in_maps(inp, cores):
    f = lambda a: np.ascontiguousarray(np.asarray(a, dtype=np.float32))
    cosf, sinf = rope_tables()
    kq = np.arange(128)
    masks = np.stack([(kq[:, None] >= kq[None, :]), (kq[:, None] <= kq[None, :])], axis=1).astype(np.float32)
    sel = np.zeros((2, 2, 128), np.float32)
    sel[0, 0, :] = 1.0
    sel[1, 1, :] = 1.0
    sc10 = np.ones((128, 640), np.float32)
    sc10[:, :512] = 0.125
    g1 = f(inp["norm1_g"]).reshape(2, 8, 128)
    g2 = f(inp["norm2_g"]).reshape(2, 8, 128)
    gcols = np.ascontiguousarray(np.stack([g1, g2], axis=0).transpose(3, 0, 1, 2))
    qg = np.concatenate([np.tile(f(inp["att_q_norm"])[0], 8), np.tile(f(inp["att_k_norm"])[0], 2)])
    qg = np.ascontiguousarray(np.broadcast_to(qg[None, :], (128, 640)))
    fg = np.ascontiguousarray(np.broadcast_to(f(inp["final_norm_g"])[None, :], (128, D)))
    shared = {
        "mod_w": f(inp["mod_w"]), "mod_b": f(inp["mod_b"]), "gcols": gcols,
        "att_w_in": f(inp["att_w_in"])[0], "att_w_out": f(inp["att_w_out"])[0],
        "qg": qg, "sc10": sc10, "sink": f(inp["att_sink"]).reshape(1, 8),
        "cosf": cosf, "sinf": sinf, "masks": masks, "ident": np.eye(128, dtype=np.float32), "sel": sel,
        "peer_w_q": f(inp["peer_w_q"]), "peer_sk": f(inp["peer_sub_keys"]),
        "peer_u": f(inp["peer_u"]), "peer_v": f(inp["peer_v"]), "fg": fg,
    }
    recc, dmt, decs = rec_consts()
    cos2, sin2 = rope_tables2()
    rep = lambda v, n: np.ascontiguousarray(np.broadcast_to(np.asarray(v, np.float32).reshape(1, -1), (128, n)))
    shared.update({
        "rec_w_in": f(inp["rec_w_in"])[0], "rec_w_out": f(inp["rec_w_out"])[0],
        "recc": recc, "dmt": dmt, "decs": decs,
        "outg": rep(np.tile(f(inp["rec_out_norm"])[0], 4), 512), "gng": rep(f(inp["rec_gn_g"])[0], 512),
        "convw": np.ascontiguousarray(f(inp["rec_conv_w"])[0].reshape(5, 12, 128).transpose(2, 1, 0)),
        "alog": rep(f(inp["rec_a_log"])[0].reshape(8), 8), "dtb": rep(f(inp["rec_dt_bias"])[0].reshape(8), 8),
        "cos2": cos2, "sin2": sin2,
        "iotaj": np.ascontiguousarray(np.broadcast_to(np.arange(128, dtype=np.float32)[None, :], (128, 128))),
        "iotaa": np.ascontiguousarray(np.broadcast_to(np.tile(np.arange(16, dtype=np.float32), 128)[None, :], (128, 2048))),
    })
    maps = []
    for b in cores:
        cc = np.stack([f(inp["c"])[b], f(inp["c_ctx"])], axis=1)
        ccT = np.ascontiguousarray(cc.reshape(8, 128, 2).transpose(1, 0, 2))
        d = dict(shared)
        d["x"] = f(inp["x"])[b]
        d["ctx"] = f(inp["ctx"])[b]
        d["ccT"] = ccT
        maps.append(d)
    return maps


def kernel(**inputs):
    nc, K = build_program()
    maps = make_in_maps(inputs, list(range(8)))
    res = run_bass_kernel_spmd(nc, maps, core_ids=list(range(8)))
    out = np.stack([np.asarray(r["out"], dtype=np.float32) for r in res.results], axis=0)
    return out


RC_U, RC_NU, RC_NEG, RC_NEGT, RC_MD, RC_MO, RC_ONES, RC_NONES = 0, 2, 4, 6, 8, 9, 10, 11
GAMMAS = [1.0 - 2.0 ** (-(5.0 + h)) for h in range(4)]


def rec_consts():
    idx = np.arange(128)
    out = np.zeros((128, 12, 128), np.float32)
    for d in range(2):
        after = (idx[:, None] > idx[None, :]) if d == 0 else (idx[:, None] < idx[None, :])
        U = (after.T | np.eye(128, dtype=bool)).astype(np.float32)
        out[:, RC_U + d] = U
        out[:, RC_NU + d] = -U
        NEG = np.where(after, 0.0, -30000.0).astype(np.float32)
        out[:, RC_NEG + d] = NEG
        out[:, RC_NEGT + d] = NEG.T
    blk = (idx[:, None] // 64) == (idx[None, :] // 64)
    out[:, RC_MD] = blk.astype(np.float32)
    out[:, RC_MO] = (~blk).astype(np.float32)
    out[:, RC_ONES] = 1.0
    out[:, RC_NONES] = -1.0
    dmt = np.zeros((128, 2, 4, 128), np.float32)
    decs = np.zeros((128, 2, 2, 4), np.float32)
    for d in range(2):
        after = (idx[:, None] > idx[None, :]) if d == 0 else (idx[:, None] < idx[None, :])
        incl = after | np.eye(128, dtype=bool)
        dist = np.abs(idx[:, None] - idx[None, :]).astype(np.float64)
        pos = idx if d == 0 else 127 - idx
        for h in range(4):
            g = GAMMAS[h]
            m_is = np.where(incl, g ** dist, 0.0)
            dmt[:, d, h, :] = m_is.T
            decs[:, d, 0, h] = g ** (pos + 1.0)
            decs[:, d, 1, h] = g ** (127.0 - pos)
    return out, dmt, decs


def rope_tables2():
    quarter = 32
    freqs = (10000.0 ** (-np.arange(quarter, dtype=np.float32) / quarter)).astype(np.float32)
    t = np.arange(L)
    row = (t // 64).astype(np.float32)
    col = (t % 64).astype(np.float32)
    ar = row[:, None] * freqs[None, :]
    ac = col[:, None] * freqs[None, :]
    cos2 = np.concatenate([np.cos(ar), np.cos(ar), np.cos(ac), np.cos(ac)], axis=1).astype(np.float32)
    sin2 = np.concatenate([-np.sin(ar), np.sin(ar), -np.sin(ac), np.sin(ac)], axis=1).astype(np.float32)
    return cos2, sin2


def emit_rec_features(c, K):
    layer = 1
    c.push()
    c.push()
    wrb = c.sb("wrb", [128, 8, 3088], BF16)
    wst = [c.sb("wrst%d" % i, [128, 1544]) for i in range(2)]
    for ch in range(8):
        for hf in range(2):
            c.dma(wst[hf][:], K.rec_w_in[ch * 128:(ch + 1) * 128, 1536 + hf * 1544:1536 + (hf + 1) * 1544], [], ["wrst%d" % hf], e="sp" if hf else "pool")
            c.cp("dve" if hf else "pool", wrb[:, ch, hf * 1544:(hf + 1) * 1544], wst[hf][:], ["wrst%d" % hf], ["wrb"])
    aTt = [c.sb("aTt%d" % i, [128, 8, 128], BF16) for i in range(2)]
    bk = [c.ps("rb%d" % i, [128, 512]) for i in range(7)]
    ptr = c.ps("ptr", [128, 1024], BF16)
    W = alloc_normwork(c, ptr)
    TKb = [c.sb("TKb%d" % i, [128, 3088]) for i in range(2)]
    Xb = c.sb("Xb", [128, 1024])
    r1 = c.sb("rr1", [128, 1024])
    r2 = c.sb("rr2", [128, 1024])
    cos2 = [c.sb("cos2_%d" % i, [128, 128]) for i in range(2)]
    sin2 = [c.sb("sin2_%d" % i, [128, 128]) for i in range(2)]
    gv = c.sb("gv", [128, 32])
    negA = c.sb("negA", [128, 8])
    ftb = [c.sb("ftb%d" % i, [128, 8, 128]) for i in range(2)]
    c.act(negA[:], K.alog[:], AF.Exp, ["alog"], ["negA"])
    c.ts("dve", negA[:], negA[:], -1.0, None, ALU.mult, None, ["negA"], ["negA"])
    segs = [(0, 512, 0), (512, 16, 1), (528, 512, 2), (1040, 512, 3), (1552, 512, 4), (2064, 512, 5), (2576, 512, 6)]
    for t in range(NT):
        m = 0 if t < NTL else 1
        aT_, atk = aTt[t % 2], "aTt%d" % (t % 2)
        emit_norm_T(c, K, W, hrows(K, t), "H%d" % t, m, 0, aT_[:], atk)
        c.dma(K.AT[:, :, t * 128:(t + 1) * 128], aT_[:], [atk], ["AT"], e="pool")
        for (c0, n, b) in segs:
            for ch in range(8):
                c.mm(bk[b][:, 0:n], aT_[:, ch, :], wrb[:, ch, c0:c0 + n], ch == 0, ch == 7, [atk, "wrb"], ["rb%d" % b])
        T_, tk = TKb[t % 2], "TKb%d" % (t % 2)
        c.act(T_[:, 0:512], bk[0][:, :], AF.Silu, ["rb0"], [tk])
        c.act(T_[:, 512:1024], bk[5][:, :], AF.Silu, ["rb5"], [tk])
        c.act(T_[:, 1024:1536], bk[6][:, :], AF.Silu, ["rb6"], [tk])
        c.cp("dve", T_[:, 2560:3072], bk[4][:, :], ["rb4"], [tk])
        c.tt("dve", gv[:, 0:8], bk[1][:, 0:8], K.dtb[:], ALU.add, ["rb1", "dtb"], ["gv0"])
        c.act(gv[:, 8:16], gv[:, 0:8], AF.Exp, ["gv0"], ["gv1"])
        c.act(gv[:, 16:24], gv[:, 8:16], AF.Ln, ["gv1", "cst"], ["gv2"], bias=K.cst[:, 0:1], scale=1.0)
        c.tt("dve", T_[:, 3072:3080], gv[:, 16:24], negA[:], ALU.mult, ["gv2", "negA"], [tk])
        c.act(T_[:, 3080:3088], bk[1][:, 8:16], AF.Sigmoid, ["rb1"], [tk])
        dst = Xb if t < NTL else T_
        dk_ = "Xb" if t < NTL else tk
        off = 0 if t < NTL else 1536
        c.cp("act", dst[:, off:off + 512], bk[2][:, :], ["rb2"], [dk_])
        c.op("act", lambda o: o.mul(out=dst[:, off + 512:off + 1024], in_=bk[3][:, :], mul=128.0 ** -0.5), ["rb3"], [dk_])
        if t < NTL:
            cf, sf = cos2[t % 2], sin2[t % 2]
            c.dma(cf[:], K.cos2[t * 128:(t + 1) * 128, :], [], ["cos2_%d" % (t % 2)])
            c.dma(sf[:], K.sin2[t * 128:(t + 1) * 128, :], [], ["sin2_%d" % (t % 2)])
            c.tt("dve", r1[:].rearrange("p (h d) -> p h d", h=8), Xb[:].rearrange("p (h d) -> p h d", h=8),
                 cf[:].unsqueeze(1).to_broadcast([128, 8, 128]), ALU.mult, ["Xb", "cos2_%d" % (t % 2)], ["rr1"])
            X5 = Xb[:].rearrange("p (h g s f) -> p h g s f", h=8, g=2, s=2, f=32)
            R5 = r2[:].rearrange("p (h g s f) -> p h g s f", h=8, g=2, s=2, f=32)
            S4 = sf[:].rearrange("p (g s f) -> p g s f", g=2, s=2, f=32)
            for s in range(2):
                c.tt("dve", R5[:, :, :, s, :], X5[:, :, :, 1 - s, :], S4[:, :, s, :].unsqueeze(1).to_broadcast([128, 8, 2, 32]),
                     ALU.mult, ["Xb", "sin2_%d" % (t % 2)], ["rr2"])
            c.tt("dve", T_[:, 1536:2560], r1[:], r2[:], ALU.add, ["rr1", "rr2"], [tk])
        for j in range(8):
            c.tr(bk[2 + j // 4][:, (j % 4) * 128:(j % 4 + 1) * 128], T_[:, 1536 + j * 128:1536 + (j + 1) * 128], K.identf[:],
                 [tk, "identf"], ["rb%d" % (2 + j // 4)])
        fb, fk = ftb[t % 2], "ftb%d" % (t % 2)
        c.cp("act", fb[:, 0:4, :].rearrange("p a t -> p (a t)"), bk[2][:, :], ["rb2"], [fk])
        c.cp("act", fb[:, 4:8, :].rearrange("p a t -> p (a t)"), bk[3][:, :], ["rb3"], [fk])
        c.dma(K.FT2[t], fb[:], [fk], ["FT2_%d" % t], e="sp")
        c.dma(K.TK[t], T_[:], [tk], ["TK%d" % t], e="sp")
    c.pop()
    c.push()
    preL = c.sb("preL", [128, L + 4])
    preC = c.sb("preC", [128, LC + 4])
    xa = c.sb("xa", [128, NT * 128])
    sq = c.sb("sqr", [128, NT * 128])
    tmc = c.sb("tmc", [128, NT, 128])
    wch = [c.sb("wch%d" % i, [128, 8, 128], BF16) for i in range(2)]
    wcs = [c.sb("wcs%d" % i, [128, 8, 128]) for i in range(2)]
    pb = [c.ps("cpb%d" % i, [128, 512]) for i in range(2)]
    pn = [c.ps("cpn%d" % i, [128, 512]) for i in range(2)]
    pT = [c.ps("cpt%d" % i, [128, 512]) for i in range(2)]
    rs = [c.sb("rsb%d" % i, [128, 512]) for i in range(2)]
    aTg = [c.sb("aTg%d" % i, [128, 8, 512], BF16) for i in range(2)]
    c.op("pool", lambda o: o.memset(preL[:], 0.0), [], ["preL"])
    c.op("pool", lambda o: o.memset(preC[:], 0.0), [], ["preC"])
    groups = [(i * 512, 512) for i in range(8)] + [(L, 256)]
    wsrc = K.rec_w_in.rearrange("(c p) n -> p c n", p=128)
    for cc in range(12):
        wc_, wck = wch[cc % 2], "wch%d" % (cc % 2)
        c.dma(wcs[cc % 2][:], wsrc[:, :, cc * 128:(cc + 1) * 128], [], ["wcs%d" % (cc % 2)], e="pool")
        c.cp("act", wc_[:], wcs[cc % 2][:], ["wcs%d" % (cc % 2)], [wck])
        for gi, (t0, N) in enumerate(groups):
            p_, pk = pb[gi % 2], "cpb%d" % (gi % 2)
            ag, agk = aTg[gi % 2], "aTg%d" % (gi % 2)
            c.dma(ag[:, :, 0:N], K.AT[:, :, t0:t0 + N], ["AT"], [agk], e="sp")
            for ch in range(8):
                c.mm(p_[:, 0:N], wc_[:, ch, :], ag[:, ch, 0:N], ch == 0, ch == 7, [wck, agk], [pk])
            if t0 < L:
                c.cp("act", preL[:, 2 + t0:2 + t0 + N], p_[:, 0:N], [pk], ["preL"])
            else:
                c.cp("act", preC[:, 2:2 + N], p_[:, 0:N], [pk], ["preC"])
        for (pre, prk, o0, n) in ((preL, "preL", 0, L), (preC, "preC", L, LC)):
            e1 = "dve" if o0 == 0 else "pool"
            c.ts(e1, xa[:, o0:o0 + n], pre[:, 0:n], K.convw[:, cc, 0:1], None, ALU.mult, None, [prk, "convw"], ["xa"])
            for k in range(1, 5):
                c.op("dve", lambda o: o.scalar_tensor_tensor(out=xa[:, o0:o0 + n], in0=pre[:, k:k + n], scalar=K.convw[:, cc, k:k + 1],
                                                         in1=xa[:, o0:o0 + n], op0=ALU.mult, op1=ALU.add), [prk, "convw", "xa"], ["xa"])
        c.act(xa[:], xa[:], AF.Silu, ["xa"], ["xa"])
        if cc < 8:
            c.tt("dve", sq[:], xa[:], xa[:], ALU.mult, ["xa"], ["sqr"])
            scale = (128.0 ** -0.5) if cc < 4 else 1.0
            for gi, (t0, N) in enumerate(groups):
                p_, pk = pn[gi % 2], "cpn%d" % (gi % 2)
                r_, rk = rs[gi % 2], "rsb%d" % (gi % 2)
                c.mm(p_[:, 0:N], K.recc[:, RC_ONES, :], sq[:, t0:t0 + N], True, True, ["sqr", "recc"], [pk])
                c.act(r_[:, 0:N], p_[:, 0:N], AF.Sqrt, [pk, "cst"], [rk], bias=K.cst[:, 1:2], scale=1.0)
                c.op("dve", lambda o: o.reciprocal(out=r_[:, 0:N], in_=r_[:, 0:N]), [rk], [rk])
                c.op("dve", lambda o: o.scalar_tensor_tensor(out=xa[:, t0:t0 + N], in0=xa[:, t0:t0 + N], scalar=scale, in1=r_[:, 0:N],
                                                           op0=ALU.mult, op1=ALU.mult), ["xa", rk], ["xa"])
        c.dma(K.FT[cc], xa[:], ["xa"], ["FT"], e="sp")
        for t in range(NT):
            p_, pk = pT[(t // 4) % 2], "cpt%d" % ((t // 4) % 2)
            c.tr(p_[:, (t % 4) * 128:(t % 4 + 1) * 128], xa[:, t * 128:(t + 1) * 128], K.identf[:], ["xa", "identf"], [pk])
            if t % 4 == 3 or t == NT - 1:
                tb = (t // 4) * 4
                nn = t - tb + 1
                c.cp("act" if (t // 4) % 2 else "dve", tmc[:, tb:tb + nn, :].rearrange("p a ch -> p (a ch)"), p_[:, 0:nn * 128], [pk], ["tmc"])
        c.dma(K.TM[cc].rearrange("a t ch -> t a ch"), tmc[:], ["tmc"], ["TM"], e="sp")
    c.pop()
    c.pop()


def emit_rec_pre(c, K):
    c.push()
    RC = K.recc
    qkv = c.sb("qkv", [128, 12, 128])
    gts = c.sb("gts", [128, 16])
    qT = c.sb("qTf", [128, 4, 128])
    kT = c.sb("kTf", [128, 4, 128])
    rt = c.sb("rtk", [128, 12, 128])
    ft2 = c.sb("ft2", [128, 8, 128])
    Mdb = RC[:, RC_MD, :].unsqueeze(1).to_broadcast([128, 4, 128])
    Mob = RC[:, RC_MO, :].unsqueeze(1).to_broadcast([128, 4, 128])
    Ib = K.identf[:].unsqueeze(1).to_broadcast([128, 4, 128])
    f2 = lambda ap: ap.rearrange("p a b -> p (a b)")
    Bs = []
    for d in range(2):
        B = Prog()
        B.sx = "_%d" % d
        B.gb = [c.ps("gbk%d_%d" % (i, d), [128, 512]) for i in range(2)]
        B.xb = [c.ps("xbk%d_%d" % (i, d), [128, 512]) for i in range(2)]
        B.g = 0
        B.sv = c.sb("sv%d" % d, [128, 32])
        for nm in ("kb", "kbT", "B0", "B1", "DT", "Dm", "DTI", "LT", "LoT", "Lm", "qkd", "nwk", "qg", "qgT", "kend", "qkm", "qdq", "qdqT", "kdd"):
            setattr(B, nm, c.sb("%s%d" % (nm, d), [128, 4, 128]))
        for nm in ("xbuf", "x1", "ybuf"):
            setattr(B, nm, c.sb("%s%d" % (nm, d), [128, 4, 256]))
        B.Tn = [c.sb("Tn%d_%d" % (i, d), [128, 4, 128]) for i in range(6)]
        B.Pn = [c.sb("Pn%d_%d" % (i, d), [128, 4, 128]) for i in range(2)]
        Bs.append(B)

    def nextbank(B):
        b = B.g % 2
        B.g += 1
        return B.gb[b], "gbk%d%s" % (b, B.sx)

    def neumann(buf, bk_):
        B = neumann.B
        sx = B.sx
        for n in range(5, -1, -1):
            for h in range(4):
                c.mm(B.xb[h // 2][:, (h % 2) * 256:(h % 2 + 1) * 256], B.Tn[n][:, h, :], buf[:, h, :], True, True, ["Tn%d" % n + sx, bk_], ["xbk%d" % (h // 2) + sx])
            for hb in range(2):
                c.tt("dve", f2(buf[:, hb * 2:hb * 2 + 2, :]), f2(buf[:, hb * 2:hb * 2 + 2, :]), B.xb[hb][:, :],
                     ALU.add if n > 0 else ALU.subtract, [bk_, "xbk%d" % hb + sx], [bk_])
            yield

    def unit(t, d, B):
        sx = B.sx
        neumann.B = B
        a = gts[:, d * 4:(d + 1) * 4]
        beta = gts[:, 8 + d * 4:8 + (d + 1) * 4]
        U = RC[:, RC_U + d, :]
        yield
        p_, pk = nextbank(B)
        c.mm(p_[:, 0:4], U, a, True, True, ["recc", "gts"], [pk])
        c.mm(p_[:, 4:8], RC[:, RC_ONES, :], a, True, True, ["recc", "gts"], [pk])
        c.cp("dve", B.sv[:, 0:8], p_[:, 0:8], [pk], [("sv" + sx)])
        c.act(B.sv[:, 8:16], B.sv[:, 0:8], AF.Exp, [("sv" + sx)], [("sv_e" + sx)])
        c.tt("dve", B.sv[:, 16:20], B.sv[:, 4:8], B.sv[:, 0:4], ALU.subtract, [("sv" + sx)], [("sv_d" + sx)])
        c.act(B.sv[:, 20:24], B.sv[:, 16:20], AF.Exp, [("sv_d" + sx)], [("sv_k" + sx)])
        c.tt("dve", B.sv[:, 24:28], beta, B.sv[:, 8:12], ALU.mult, ["gts", ("sv_e" + sx)], [("sv_b" + sx)])
        c.dma(K.PEG[d, t], B.sv[:, 12:16], [("sv_e" + sx)], ["PEG%d_%d" % (d, t)], e="pool")
        for h in range(4):
            c.smul("dve" if h % 2 else "act", B.kb[:, h, :], qkv[:, 4 + h, :], beta[:, h:h + 1], ["qkv", "gts"], [("kbt" + sx)])
        yield
        p_, pk = nextbank(B)
        for h in range(4):
            c.tr(p_[:, h * 128:(h + 1) * 128], B.kb[:, h, :], K.identf[:], [("kbt" + sx), "identf"], [pk])
        c.cp("act", f2(B.kbT[:]), p_[:, :], [pk], [("kbT" + sx)])
        for h in range(4):
            c.smul("act", B.B0[:, h, :], RC[:, RC_ONES, :], a[:, h:h + 1], ["recc", "gts"], [("B0" + sx)])
            c.smul("act" if h % 2 else "dve", B.B1[:, h, :], U, a[:, h:h + 1], ["recc", "gts"], [("B1" + sx)])
        yield
        p_, pk = nextbank(B)
        for h in range(4):
            o_ = p_[:, h * 128:(h + 1) * 128]
            c.mm(o_, B.B0[:, h, :], U, True, False, [("B0" + sx), "recc"], [pk])
            c.mm(o_, B.B1[:, h, :], RC[:, RC_NONES, :], False, False, [("B1" + sx), "recc"], [pk])
            c.mm(o_, K.identf[:], RC[:, RC_NEGT + d, :], False, True, ["identf", "recc"], [pk])
        c.act(f2(B.DT[:]), p_[:, :], AF.Exp, [pk], [("DT" + sx)])
        yield
        p_, pk = nextbank(B)
        for h in range(4):
            o_ = p_[:, h * 128:(h + 1) * 128]
            c.mm(o_, B.B1[:, h, :], RC[:, RC_ONES, :], True, False, [("B1" + sx), "recc"], [pk])
            c.mm(o_, B.B0[:, h, :], RC[:, RC_NU + d, :], False, False, [("B0" + sx), "recc"], [pk])
            c.mm(o_, K.identf[:], RC[:, RC_NEG + d, :], False, True, ["identf", "recc"], [pk])
        c.act(f2(B.Dm[:]), p_[:, :], AF.Exp, [pk], [("Dm" + sx)])
        yield
        p_, pk = nextbank(B)
        for h in range(4):
            c.mm(p_[:, h * 128:(h + 1) * 128], kT[:, h, :], B.kbT[:, h, :], True, True, ["kTf", ("kbT" + sx)], [pk])
        c.tt("dve", f2(B.LT[:]), p_[:, :], f2(B.DT[:]), ALU.mult, [pk, ("DT" + sx)], [("LT" + sx)])
        c.tt("dve", B.Tn[0][:], B.LT[:], Mdb, ALU.mult, [("LT" + sx), "recc"], ["Tn0" + sx])
        c.tt("dve", B.LoT[:], B.LT[:], Mob, ALU.mult, [("LT" + sx), "recc"], [("LoT" + sx)])
        yield
        p_, pk = nextbank(B)
        for h in range(4):
            c.mm(p_[:, h * 128:(h + 1) * 128], B.kbT[:, h, :], kT[:, h, :], True, True, ["kTf", ("kbT" + sx)], [pk])
        c.tt("dve", f2(B.Lm[:]), p_[:, :], f2(B.Dm[:]), ALU.mult, [pk, ("Dm" + sx)], [("Lm" + sx)])
        c.tt("dve", B.Pn[0][:], B.Lm[:], Mdb, ALU.mult, [("Lm" + sx), "recc"], ["Pn0" + sx])
        yield
        p_, pk = nextbank(B)
        for h in range(4):
            c.mm(p_[:, h * 128:(h + 1) * 128], kT[:, h, :], qT[:, h, :], True, True, ["kTf", "qTf"], [pk])
        c.tt("dve", B.DTI[:], B.DT[:], Ib, ALU.add, [("DT" + sx), "identf"], [("DTI" + sx)])
        c.tt("dve", f2(B.qkd[:]), p_[:, :], f2(B.DTI[:]), ALU.mult, [pk, ("DTI" + sx)], [("qkd" + sx)])
        c.dma(K.PQK[d, t], f2(B.qkd[:]), [("qkd" + sx)], ["PQK%d_%d" % (d, t)], e="sp")
        for n in range(5):
            if n < 4:
                p_, pk = nextbank(B)
                for h in range(4):
                    c.mm(p_[:, h * 128:(h + 1) * 128], B.Tn[n][:, h, :], B.Pn[n % 2][:, h, :], True, True, [("Tn%d" % n + sx), ("Pn%d" % (n % 2) + sx)], [pk])
                c.cp("act", f2(B.Pn[(n + 1) % 2][:]), p_[:, :], [pk], [("Pn%d" % ((n + 1) % 2) + sx)])
            p_, pk = nextbank(B)
            for h in range(4):
                c.mm(p_[:, h * 128:(h + 1) * 128], B.Pn[n % 2][:, h, :], B.Tn[n][:, h, :], True, True, [("Tn%d" % n + sx), ("Pn%d" % (n % 2) + sx)], [pk])
            c.cp("dve", f2(B.Tn[n + 1][:]), p_[:, :], [pk], [("Tn%d" % (n + 1) + sx)])
            yield
        for h in range(4):
            c.smul("act", B.xbuf[:, h, 0:128], qkv[:, 8 + h, :], beta[:, h:h + 1], ["qkv", "gts"], [("xbuf" + sx)])
            c.ts("dve", B.xbuf[:, h, 128:256], qkv[:, 4 + h, :], B.sv[:, 24 + h:25 + h], None, ALU.mult, None, ["qkv", ("sv_b" + sx)], [("xbuf" + sx)])
        neumann.B = B
        yield from neumann(B.xbuf, "xbuf" + sx)
        c.cp("act", B.x1[:], B.xbuf[:], [("xbuf" + sx)], [("x1" + sx)])
        for h in range(4):
            c.mm(B.xb[h // 2][:, (h % 2) * 256:(h % 2 + 1) * 256], B.LoT[:, h, :], B.x1[:, h, :], True, True, [("LoT" + sx), ("x1" + sx)], [("xbk%d" % (h // 2) + sx)])
        for hb in range(2):
            c.cp("act", f2(B.ybuf[:, hb * 2:hb * 2 + 2, :]), B.xb[hb][:, :], [("xbk%d" % hb + sx)], [("ybuf" + sx)])
        neumann.B = B
        yield from neumann(B.ybuf, "ybuf" + sx)
        c.tt("dve", B.x1[:], B.x1[:], B.ybuf[:], ALU.subtract, [("x1" + sx), ("ybuf" + sx)], [("x1" + sx)])
        c.dma(K.PU[d, t].rearrange("p (h v) -> p h v", h=4), B.x1[:, :, 0:128], [("x1" + sx)], ["PU%d_%d" % (d, t)], e="sp")
        yield
        p_, pk = nextbank(B)
        for h in range(4):
            c.tr(p_[:, h * 128:(h + 1) * 128], B.x1[:, h, 128:256], K.identf[:], [("x1" + sx), "identf"], [pk])
        c.op("act", lambda o: o.mul(out=f2(B.nwk[:]), in_=p_[:, :], mul=-1.0), [pk], [("nwk" + sx)])
        c.dma(K.PNW[d, t], f2(B.nwk[:]), [("nwk" + sx)], ["PNW%d_%d" % (d, t)], e="pool")
        for h in range(4):
            c.smul("dve" if h % 2 else "act", B.qg[:, h, :], qkv[:, h, :], B.sv[:, 8 + h:9 + h], ["qkv", ("sv_e" + sx)], [("qgt" + sx)])
            c.smul("act" if h % 2 else "dve", B.kend[:, h, :], qkv[:, 4 + h, :], B.sv[:, 20 + h:21 + h], ["qkv", ("sv_k" + sx)], [("kend" + sx)])
        yield
        p_, pk = nextbank(B)
        for h in range(4):
            c.tr(p_[:, h * 128:(h + 1) * 128], B.qg[:, h, :], K.identf[:], [("qgt" + sx), "identf"], [pk])
        c.cp("act", f2(B.qgT[:]), p_[:, :], [pk], [("qgT" + sx)])
        c.dma(K.PQG[d, t], f2(B.qgT[:]), [("qgT" + sx)], ["PQG%d_%d" % (d, t)], e="sp")
        c.dma(K.PKE[d, t], f2(B.kend[:]), [("kend" + sx)], ["PKE%d_%d" % (d, t)], e="pool")
        yield
        p_, pk = nextbank(B)
        for h in range(4):
            c.mm(p_[:, h * 128:(h + 1) * 128], ft2[:, 4 + h, :], ft2[:, h, :], True, True, ["ft2"], [pk])
        c.tt("dve", f2(B.qkm[:]), p_[:, :], f2(K.dmt[:, d]), ALU.mult, [pk, "dmt"], [("qkm" + sx)])
        c.dma(K.RQK[d, t], f2(B.qkm[:]), [("qkm" + sx)], ["RQK%d_%d" % (d, t)], e="sp")
        for h in range(4):
            c.smul("act", B.qdq[:, h, :], rt[:, h, :], K.decs[:, d, 0, h:h + 1], ["rtk", "decs"], [("qdq" + sx)])
            c.ts("dve", B.kdd[:, h, :], rt[:, 4 + h, :], K.decs[:, d, 1, h:h + 1], None, ALU.mult, None, ["rtk", "decs"], [("kdd" + sx)])
        yield
        p_, pk = nextbank(B)
        for h in range(4):
            c.tr(p_[:, h * 128:(h + 1) * 128], B.qdq[:, h, :], K.identf[:], [("qdq" + sx), "identf"], [pk])
        c.cp("act", f2(B.qdqT[:]), p_[:, :], [pk], [("qdqT" + sx)])
        c.dma(K.RQQ[d, t], f2(B.qdqT[:]), [("qdqT" + sx)], ["RQQ%d_%d" % (d, t)], e="pool")
        c.dma(K.RKD[d, t], f2(B.kdd[:]), [("kdd" + sx)], ["RKD%d_%d" % (d, t)], e="sp")


    for t in range(NT):
        c.dma(qkv[:], K.TM[:, t].rearrange("a p ch -> p a ch"), ["TM"], ["qkv"], e="sp")
        c.dma(gts[:], K.TK[t][:, 3072:3088], ["TK%d" % t], ["gts"], e="pool")
        c.dma(qT[:], K.FT[0:4, :, t * 128:(t + 1) * 128].rearrange("a p t -> p a t"), ["FT"], ["qTf"], e="sp")
        c.dma(kT[:], K.FT[4:8, :, t * 128:(t + 1) * 128].rearrange("a p t -> p a t"), ["FT"], ["kTf"], e="pool")
        c.dma(rt[:], K.TK[t][:, 1536:3072].rearrange("p (a ch) -> p a ch", a=12), ["TK%d" % t], ["rtk"], e="sp")
        c.dma(ft2[:], K.FT2[t], ["FT2_%d" % t], ["ft2"], e="pool")

        _interleave([unit(t, 0, Bs[0]), unit(t, 1, Bs[1])])
    c.pop()


def emit_rec_scan(c, K):
    c.push()
    Sg = [[c.sb("Sg%d%d" % (d, h), [128, 128]) for h in range(4)] for d in range(2)]
    Sr = [[c.sb("Sr%d%d" % (d, h), [128, 128]) for h in range(4)] for d in range(2)]
    for d in range(2):
        for h in range(4):
            c.op("pool", lambda o: o.memset(Sg[d][h][:], 0.0), [], ["Sg%d%d" % (d, h)])
            c.op("pool", lambda o: o.memset(Sr[d][h][:], 0.0), [], ["Sr%d%d" % (d, h)])
    names = ("u", "nw", "qg", "qk", "ke", "rqk", "rqq", "rkd", "vd")
    bufs = [[{n: c.sb("sc_%s%d%d" % (n, d, i), [128, 4, 128]) for n in names} for i in range(2)] for d in range(2)]
    egl = [[c.sb("sc_egl%d%d" % (d, i), [128, 4]) for i in range(2)] for d in range(2)]
    wb = [c.sb("sc_w%d" % d, [128, 4, 128]) for d in range(2)]
    og = [c.sb("sc_og%d" % d, [128, 4, 128]) for d in range(2)]
    orr = [c.sb("sc_or%d" % d, [128, 4, 128]) for d in range(2)]
    pbk = [[c.ps("scp%d%d" % (d, i), [128, 512]) for i in range(4)] for d in range(2)]
    f2 = lambda ap: ap.rearrange("p a b -> p (a b)")
    order = [[32, 33] + list(range(32)), [33, 32] + list(range(31, -1, -1))]
    cdec = [g ** 128.0 for g in GAMMAS]
    for step in range(NT):
        for d in range(2):
            t = order[d][step]
            i = step % 2
            B = bufs[d][i]
            kk = lambda n: "sc_%s%d%d" % (n, d, i)
            srcs = {"u": (K.PU, "PU"), "nw": (K.PNW, "PNW"), "qg": (K.PQG, "PQG"), "qk": (K.PQK, "PQK"), "ke": (K.PKE, "PKE"),
                    "rqk": (K.RQK, "RQK"), "rqq": (K.RQQ, "RQQ"), "rkd": (K.RKD, "RKD")}
            for j, (n, (src, sn)) in enumerate(srcs.items()):
                c.dma(f2(B[n][:]), src[d, t], ["%s%d_%d" % (sn, d, t)], [kk(n)], e="sp" if j % 2 else "pool")
            c.dma(f2(B["vd"][:]), K.TK[t][:, 2560:3072], ["TK%d" % t], [kk("vd")], e="sp")
            c.dma(egl[d][i][:], K.PEG[d, t], ["PEG%d_%d" % (d, t)], ["sc_egl%d%d" % (d, i)], e="pool")
            pw, po, pS, pr = pbk[d]
            pk = ["scp%d%d" % (d, j) for j in range(4)]
            latent = t < NTL
            for h in range(4):
                sgk = "Sg%d%d" % (d, h)
                c.mm(pw[:, h * 128:(h + 1) * 128], B["nw"][:, h, :], Sg[d][h][:], True, True, [kk("nw"), sgk], [pk[0]])
            c.tt("dve", f2(wb[d][:]), f2(B["u"][:]), pw[:, :], ALU.add, [kk("u"), pk[0]], ["sc_w%d" % d])
            for h in range(4):
                sgk = "Sg%d%d" % (d, h)
                if latent:
                    c.mm(po[:, h * 128:(h + 1) * 128], B["qg"][:, h, :], Sg[d][h][:], True, False, [kk("qg"), sgk], [pk[1]])
                    c.mm(po[:, h * 128:(h + 1) * 128], B["qk"][:, h, :], wb[d][:, h, :], False, True, [kk("qk"), "sc_w%d" % d], [pk[1]])
                c.mm(pS[:, h * 128:(h + 1) * 128], B["ke"][:, h, :], wb[d][:, h, :], True, True, [kk("ke"), "sc_w%d" % d], [pk[2]])
            for h in range(4):
                sgk = "Sg%d%d" % (d, h)
                c.op("dve", lambda o: o.scalar_tensor_tensor(out=Sg[d][h][:], in0=Sg[d][h][:], scalar=egl[d][i][:, h:h + 1], in1=pS[:, h * 128:(h + 1) * 128],
                                                           op0=ALU.mult, op1=ALU.add), [sgk, "sc_egl%d%d" % (d, i), pk[2]], [sgk])
            if latent:
                c.cp("act", f2(og[d][:]), po[:, :], [pk[1]], ["sc_og%d" % d])
                c.dma(K.OG[d, t], f2(og[d][:]), ["sc_og%d" % d], ["OG%d_%d" % (d, t)], e="sp")
            for h in range(4):
                srk = "Sr%d%d" % (d, h)
                if latent:
                    c.mm(pr[:, h * 128:(h + 1) * 128], B["rqq"][:, h, :], Sr[d][h][:], True, False, [kk("rqq"), srk], [pk[3]])
                    c.mm(pr[:, h * 128:(h + 1) * 128], B["rqk"][:, h, :], B["vd"][:, h, :], False, True, [kk("rqk"), kk("vd")], [pk[3]])
                c.mm(pw[:, h * 128:(h + 1) * 128], B["rkd"][:, h, :], B["vd"][:, h, :], True, True, [kk("rkd"), kk("vd")], [pk[0]])
            for h in range(4):
                srk = "Sr%d%d" % (d, h)
                c.op("pool" if False else "dve", lambda o: o.scalar_tensor_tensor(out=Sr[d][h][:], in0=Sr[d][h][:], scalar=float(cdec[h]), in1=pw[:, h * 128:(h + 1) * 128],
                                                           op0=ALU.mult, op1=ALU.add), [srk, pk[0]], [srk])
            if latent:
                c.cp("act", f2(orr[d][:]), pr[:, :], [pk[3]], ["sc_or%d" % d])
                c.dma(K.OR[d, t], f2(orr[d][:]), ["sc_or%d" % d], ["OR%d_%d" % (d, t)], e="pool")
    c.pop()


def emit_rec_merge(c, K):
    layer = 1
    c.push()
    woutb = c.sb("rwoutb", [128, 8, 1024], BF16)
    wst = [c.sb("rwst%d" % i, [128, 1024]) for i in range(2)]
    for ch in range(8):
        c.dma(wst[ch % 2][:], K.rec_w_out[ch * 128:(ch + 1) * 128, :], [], ["rwst%d" % (ch % 2)], e="sp" if ch % 2 else "pool")
        c.cp("dve" if ch % 2 else "pool", woutb[:, ch, :], wst[ch % 2][:], ["rwst%d" % (ch % 2)], ["rwoutb"])
    ogb = [[c.sb("m_og%d%d" % (d, i), [128, 4, 128]) for i in range(2)] for d in range(2)]
    orb = [[c.sb("m_or%d%d" % (d, i), [128, 4, 128]) for i in range(2)] for d in range(2)]
    zg = [c.sb("m_zg%d" % i, [128, 1536]) for i in range(2)]
    ds = c.sb("m_ds", [128, 4, 128])
    sq = c.sb("m_sq", [128, 4, 128])
    xc = c.sb("m_xc", [128, 4, 128])
    st = c.sb("m_st", [128, 32])
    mixf = c.sb("m_mixf", [128, 1024])
    mixb = c.sb("m_mixb", [128, 1024], BF16)
    mixT = c.sb("m_mixT", [128, 8, 128], BF16)
    ptr = c.ps("m_ptr", [128, 1024], BF16)
    pY = c.ps("m_pY", [128, 1024])
    W = Prog()
    W.n = 0
    W.hbuf = [c.sb("hbuf%d" % i, [128, 1024]) for i in range(2)]
    W.tmpf = c.sb("tmpf", [128, 1024])
    f2 = lambda ap: ap.rearrange("p a b -> p (a b)")
    bc4 = lambda ap: ap.unsqueeze(2).to_broadcast([128, 4, 128])
    for t in range(NTL):
        i = t % 2
        for d in range(2):
            c.dma(f2(ogb[d][i][:]), K.OG[d, t], ["OG%d_%d" % (d, t)], ["m_og%d%d" % (d, i)], e="sp")
            c.dma(f2(orb[d][i][:]), K.OR[d, t], ["OR%d_%d" % (d, t)], ["m_or%d%d" % (d, i)], e="pool")
        c.dma(zg[i][:], K.TK[t][:, 0:1536], ["TK%d" % t], ["m_zg%d" % i], e="sp")
        c.tt("dve", ds[:], ogb[0][i][:], ogb[1][i][:], ALU.add, ["m_og0%d" % i, "m_og1%d" % i], ["m_ds"])
        c.tt("dve", sq[:], ds[:], ds[:], ALU.mult, ["m_ds"], ["m_sq"])
        c.op("dve", lambda o: o.tensor_reduce(out=st[:, 0:4], in_=sq[:], axis=AX.X, op=ALU.add), ["m_sq"], ["m_st0"])
        c.ts("dve", st[:, 4:8], st[:, 0:4], 1.0 / 128, EPS, ALU.mult, ALU.add, ["m_st0"], ["m_st1"])
        c.act(st[:, 4:8], st[:, 4:8], AF.Sqrt, ["m_st1"], ["m_st1"])
        c.op("dve", lambda o: o.reciprocal(out=st[:, 8:12], in_=st[:, 4:8]), ["m_st1"], ["m_st2"])
        c.tt("dve", ds[:], ds[:], bc4(st[:, 8:12]), ALU.mult, ["m_ds", "m_st2"], ["m_ds"])
        c.tt("dve", f2(ds[:]), f2(ds[:]), K.outg[:], ALU.mult, ["m_ds", "outg"], ["m_ds"])
        c.tt("dve", mixf[:, 0:512], f2(ds[:]), zg[i][:, 0:512], ALU.mult, ["m_ds", "m_zg%d" % i], ["m_mixf"])
        for d in range(2):
            x = orb[d][i]
            xk = "m_or%d%d" % (d, i)
            c.op("dve", lambda o: o.tensor_reduce(out=st[:, 12:16], in_=x[:], axis=AX.X, op=ALU.add), [xk], ["m_st3"])
            c.ts("dve", st[:, 16:20], st[:, 12:16], 1.0 / 128, None, ALU.mult, None, ["m_st3"], ["m_st4"])
            c.tt("dve", xc[:], x[:], bc4(st[:, 16:20]), ALU.subtract, [xk, "m_st4"], ["m_xc"])
            c.tt("dve", sq[:], xc[:], xc[:], ALU.mult, ["m_xc"], ["m_sq"])
            c.op("dve", lambda o: o.tensor_reduce(out=st[:, 20:24], in_=sq[:], axis=AX.X, op=ALU.add), ["m_sq"], ["m_st5"])
            c.ts("dve", st[:, 24:28], st[:, 20:24], 1.0 / 128, EPS, ALU.mult, ALU.add, ["m_st5"], ["m_st6"])
            c.act(st[:, 24:28], st[:, 24:28], AF.Sqrt, ["m_st6"], ["m_st6"])
            c.op("dve", lambda o: o.reciprocal(out=st[:, 28:32], in_=st[:, 24:28]), ["m_st6"], ["m_st7"])
            c.tt("dve", xc[:], xc[:], bc4(st[:, 28:32]), ALU.mult, ["m_xc", "m_st7"], ["m_xc"])
            c.tt("dve", f2(xc[:]), f2(xc[:]), K.gng[:], ALU.mult, ["m_xc", "gng"], ["m_xc"])
            if d == 0:
                c.tt("dve", mixf[:, 512:1024], f2(xc[:]), zg[i][:, 512:1024], ALU.mult, ["m_xc", "m_zg%d" % i], ["m_mixf"])
            else:
                c.tt("dve", f2(xc[:]), f2(xc[:]), zg[i][:, 1024:1536], ALU.mult, ["m_xc", "m_zg%d" % i], ["m_xc"])
                c.tt("dve", mixf[:, 512:1024], mixf[:, 512:1024], f2(xc[:]), ALU.add, ["m_xc", "m_mixf"], ["m_mixf"])
        c.cp("act", mixb[:], mixf[:], ["m_mixf"], ["m_mixb"])
        for ch in range(8):
            c.tr(ptr[:, ch * 128:(ch + 1) * 128], mixb[:, ch * 128:(ch + 1) * 128], K.identb[:], ["m_mixb", "identb"], ["m_ptr"])
        c.cp("act", f2(mixT[:]), ptr[:, :], ["m_ptr"], ["m_mixT"])
        for nb in range(2):
            for ch in range(8):
                c.mm(pY[:, nb * 512:(nb + 1) * 512], mixT[:, ch, :], woutb[:, ch, nb * 512:(nb + 1) * 512], ch == 0, ch == 7, ["m_mixT", "rwoutb"], ["m_pY"])
        emit_residual(c, K, W, (hrows(K, t), "H%d" % t), pY[:, :], ["m_pY"], 0, 0, t)
    c.pop()


def emit_rec(c, K):
    c.push()
    K.recc = c.sb("recc", [128, 12, 128])
    K.dmt = c.sb("dmt", [128, 2, 4, 128])
    K.decs = c.sb("decs", [128, 2, 2, 4])
    K.outg = c.sb("outg", [128, 512])
    K.gng = c.sb("gng", [128, 512])
    K.convw = c.sb("convw", [128, 12, 5])
    K.alog = c.sb("alog", [128, 8])
    K.dtb = c.sb("dtb", [128, 8])
    K.cst = c.sb("cst", [128, 2])
    c.dma(K.recc[:], K.recc_in, [], ["recc"])
    c.dma(K.dmt[:], K.dmt_in, [], ["dmt"])
    c.dma(K.decs[:], K.decs_in, [], ["decs"])
    c.dma(K.outg[:], K.outg_in, [], ["outg"])
    c.dma(K.gng[:], K.gng_in, [], ["gng"])
    c.dma(K.convw[:], K.convw_in, [], ["convw"])
    c.dma(K.alog[:], K.alog_in, [], ["alog"])
    c.dma(K.dtb[:], K.dtb_in, [], ["dtb"])
    c.op("pool", lambda o: o.memset(K.cst[:, 0:1], 1.0), [], ["cst"])
    c.op("pool", lambda o: o.memset(K.cst[:, 1:2], EPS), [], ["cst"])
    emit_rec_features(c, K)
    emit_rec_pre(c, K)
    emit_rec_scan(c, K)
    emit_rec_merge(c, K)
    c.pop()


U32 = mybir.dt.uint32


def emit_peer2(c, K, layer, ntiles):
    emit_peer_prep(c, K, layer)
    c.push()
    GT = 2
    wqb = c.sb("wqb", [128, 8, 2048], BF16)
    c.push()
    wst = [c.sb("wqst%d" % i, [128, 2048]) for i in range(2)]
    for ch in range(8):
        c.dma(wst[ch % 2][:], K.peer_w_q[layer, ch * 128:(ch + 1) * 128, :], [], ["wqst%d" % (ch % 2)], e="sp" if ch % 2 else "pool")
        c.cp("dve" if ch % 2 else "pool", wqb[:, ch, :], wst[ch % 2][:], ["wqst%d" % (ch % 2)], ["wqb"])
    c.pop()
    skf = c.sb("skf", [128, 2, 128])
    skb = c.sb("skb", [128, 2, 128], BF16)
    skT = c.sb("skT", [128, 2, 128], BF16)
    Bk = [c.ps("bank%d" % i, [128, 512]) for i in range(8)]
    Bkb = [b[:].bitcast(BF16) for b in Bk]
    for p in range(2):
        c.dma(skf[:, p, :], K.peer_sk[layer, p], [], ["skf"])
    c.cp("dve", skb[:], skf[:], ["skf"], ["skb"])
    for p in range(2):
        c.tr(Bkb[7][:, p * 128:(p + 1) * 128], skb[:, p, :], K.identb[:], ["skb", "identb"], ["bank7"])
    c.cp("dve", skT[:].rearrange("p a k -> p (a k)"), Bkb[7][:, 0:256], ["bank7"], ["skT"])
    iotaj = c.sb("iotaj", [128, 128])
    iota_a = c.sb("iota_a", [128, 8, 16, 16])
    c.dma(iotaj[:], K.iotaj_in, [], ["iotaj"])
    c.dma(iota_a[:].rearrange("p h r a -> p (h r a)"), K.iotaa_in, [], ["iota_a"])

    W = alloc_normwork(c, Bkb[5])
    W.ptrkey = "bank5"
    xTg = c.sb("xTg", [128, 8, GT * 128], BF16)
    qb16 = c.sb("qb16", [128, 1024], BF16)
    qT = c.sb("qT", [128, 8, 128], BF16)
    S = c.sb("S", [128, 16, 128])
    SV = c.sb("SV", [128, 16, 16])
    SIu = c.sb("SIu", [128, 16, 16], U32)
    SIf = c.sb("SIf", [128, 16, 16])
    wk = c.sb("wk", [128, 256])
    cand = c.sb("cand", [128, 8, 256])
    CV = c.sb("CV", [128, 8, 16])
    CPu = c.sb("CPu", [128, 8, 16], U32)
    CPf = c.sb("CPf", [128, 8, 16])
    ab = c.sb("abf", [128, 2, 8, 16])
    eq = c.sb("eqg", [128, 8, 16, 16])
    IJG = c.sb("IJG", [128, 3, 128])
    IJGT = [c.sb("IJGT%d" % i, [128, 3, 128]) for i in range(2)]
    ez = c.sb("ez", [128, 8, 16])
    st8 = c.sb("st8", [128, 16])
    NB = 4
    At = [c.sb("At%d" % i, [128, 128], BF16) for i in range(NB)]
    Bt = [c.sb("Bt%d" % i, [128, 128], BF16) for i in range(NB)]
    GG = c.sb("GG", [128, GT * 128, 128], BF16)
    uTi = [c.sb("uTi%d" % i, [128, 8, 128], BF16) for i in range(3)]
    vi = [c.sb("vi%d" % i, [128, 1024], BF16) for i in range(3)]
    gl = [c.sb("gl%d" % i, [128, GT * 128], BF16) for i in range(2)]
    wT = [c.sb("wT%d" % i, [128, GT * 128], BF16) for i in range(2)]
    ring = Prog()
    ring.n = 0
    ring.e = 0
    ngroups = ntiles // GT
    for g in range(ngroups):
        tiles = [g * GT + i for i in range(GT)]
        for tl, t in enumerate(tiles):
            m = 0 if t < NTL else 1
            emit_norm_T_peer(c, K, W, hrows(K, t), "H%d" % t, m, xTg[:, :, tl * 128:(tl + 1) * 128], "xTg")
            for hh in range(2):
                for nb in range(2):
                    for ch in range(8):
                        c.mm(Bk[nb][:, :], xTg[:, ch, tl * 128:(tl + 1) * 128], wqb[:, ch, hh * 1024 + nb * 512: hh * 1024 + (nb + 1) * 512],
                             ch == 0, ch == 7, ["xTg", "wqb"], ["bank%d" % nb])
                for nb in range(2):
                    c.cp("act", qb16[:, nb * 512:(nb + 1) * 512], Bk[nb][:, :], ["bank%d" % nb], ["qb16"])
                for b in range(8):
                    c.tr(Bkb[2][:, b * 128:(b + 1) * 128], qb16[:, b * 128:(b + 1) * 128], K.identb[:], ["qb16", "identb"], ["bank2"])
                c.cp("act", qT[:].rearrange("p b t -> p (b t)"), Bkb[2][:, :], ["bank2"], ["qT"])
                for b in range(8):
                    bank = 3 + b // 4
                    c.mm(Bk[bank][:, (b % 4) * 128:(b % 4 + 1) * 128], qT[:, b, :], skT[:, b % 2, :], True, True, ["qT", "skT"], ["bank%d" % bank])
                for bb in range(2):
                    c.cp("act", S[:, hh * 8 + bb * 4: hh * 8 + (bb + 1) * 4, :].rearrange("p b k -> p (b k)"), Bk[3 + bb][:, :],
                         ["bank%d" % (3 + bb)], ["S"])
            for hp in range(16):
                c.op("dve", lambda o: o.max(out=SV[:, hp, 0:8], in_=S[:, hp, :]), ["S"], ["SV"])
                c.op("dve", lambda o: o.max_index(out=SIu[:, hp, 0:8], in_max=SV[:, hp, 0:8], in_values=S[:, hp, :]), ["S", "SV"], ["SIu"])
                c.op("dve", lambda o: o.match_replace(out=wk[:, 0:128], in_to_replace=SV[:, hp, 0:8], in_values=S[:, hp, :], imm_value=-1e30),
                     ["S", "SV"], ["wk"])
                c.op("dve", lambda o: o.max(out=SV[:, hp, 8:16], in_=wk[:, 0:128]), ["wk"], ["SV"])
                c.op("dve", lambda o: o.max_index(out=SIu[:, hp, 8:16], in_max=SV[:, hp, 8:16], in_values=wk[:, 0:128]), ["wk", "SV"], ["SIu"])
            c.cp("dve", SIf[:], SIu[:], ["SIu"], ["SIf"])
            SV8 = SV[:].rearrange("p (h two) a -> p h two a", h=8, two=2)
            SI8 = SIf[:].rearrange("p (h two) a -> p h two a", h=8, two=2)
            c.tt("dve", cand[:].rearrange("p h (a b) -> p h a b", a=16), SV8[:, :, 0, :].unsqueeze(3).to_broadcast([128, 8, 16, 16]),
                 SV8[:, :, 1, :].unsqueeze(2).to_broadcast([128, 8, 16, 16]), ALU.add, ["SV"], ["cand"])
            for h in range(8):
                c.op("dve", lambda o: o.max(out=CV[:, h, 0:8], in_=cand[:, h, :]), ["cand"], ["CV"])
                c.op("dve", lambda o: o.max_index(out=CPu[:, h, 0:8], in_max=CV[:, h, 0:8], in_values=cand[:, h, :]), ["cand", "CV"], ["CPu"])
                c.op("dve", lambda o: o.match_replace(out=wk[:, :], in_to_replace=CV[:, h, 0:8], in_values=cand[:, h, :], imm_value=-1e30),
                     ["cand", "CV"], ["wk"])
                c.op("dve", lambda o: o.max(out=CV[:, h, 8:16], in_=wk[:, :]), ["wk"], ["CV"])
                c.op("dve", lambda o: o.max_index(out=CPu[:, h, 8:16], in_max=CV[:, h, 8:16], in_values=wk[:, :]), ["wk", "CV"], ["CPu"])
            c.cp("dve", CPf[:], CPu[:], ["CPu"], ["CPf"])
            c.ts("dve", ab[:, 1], CPf[:], 1.0 / 16, -1.0, ALU.mult, ALU.add, ["CPf"], ["abf"])
            c.tt("dve", eq[:], ab[:, 1].unsqueeze(3).to_broadcast([128, 8, 16, 16]), iota_a[:], ALU.is_ge, ["abf", "iota_a"], ["eqg"])
            c.op("dve", lambda o: o.tensor_reduce(out=ab[:, 0], in_=eq[:], axis=AX.X, op=ALU.add), ["eqg"], ["abf"])
            c.op("dve", lambda o: o.scalar_tensor_tensor(out=ab[:, 1], in0=ab[:, 0], scalar=-16.0, in1=CPf[:], op0=ALU.mult, op1=ALU.add), ["abf", "CPf"], ["abf"])
            IJ4 = IJG[:].rearrange("p c (h r) -> p c h r", h=8)
            for which in range(2):
                c.tt("dve", eq[:], ab[:, which].unsqueeze(3).to_broadcast([128, 8, 16, 16]), iota_a[:], ALU.is_equal, ["abf", "iota_a"], ["eqg"])
                c.tt("dve", eq[:], eq[:], SI8[:, :, which, :].unsqueeze(2).to_broadcast([128, 8, 16, 16]), ALU.mult, ["eqg", "SIf"], ["eqg"])
                c.op("dve", lambda o: o.tensor_reduce(out=IJ4[:, which], in_=eq[:], axis=AX.X, op=ALU.add), ["eqg"], ["IJG"])
            c.tt("dve", ez[:], CV[:], CV[:, :, 0:1].to_broadcast([128, 8, 16]), ALU.subtract, ["CV"], ["ez"])
            c.act(ez[:].rearrange("p h a -> p (h a)"), ez[:].rearrange("p h a -> p (h a)"), AF.Exp, ["ez"], ["ez"])
            c.op("dve", lambda o: o.tensor_reduce(out=st8[:, 0:8], in_=ez[:], axis=AX.X, op=ALU.add), ["ez"], ["st8a"])
            c.op("dve", lambda o: o.reciprocal(out=st8[:, 8:16], in_=st8[:, 0:8]), ["st8a"], ["st8b"])
            c.tt("dve", IJ4[:, 2], ez[:], st8[:, 8:16].unsqueeze(2).to_broadcast([128, 8, 16]), ALU.mult, ["ez", "st8b"], ["IJG"])
            for w_ in range(3):
                c.tr(Bk[6][:, w_ * 128:(w_ + 1) * 128], IJG[:, w_, :], K.identf[:], ["IJG", "identf"], ["bank6"])
            T3, T3k = IJGT[tl], "IJGT%d" % tl
            c.cp("act", T3[:].rearrange("p c t -> p (c t)"), Bk[6][:, 0:384], ["bank6"], [T3k])
            for tk in range(128):
                k = ring.e % NB
                ring.e += 1
                pb_, pbk = Bk[6 + (tk // 4) % 2], "bank%d" % (6 + (tk // 4) % 2)
                c.ts("dve", Bt[k][:], iotaj[:], T3[:, 1, tk:tk + 1], None, ALU.is_equal, None, ["iotaj", T3k], ["Bt%d" % k])
                c.ts("dve", At[k][:], iotaj[:], T3[:, 0, tk:tk + 1], T3[:, 2, tk:tk + 1], ALU.is_equal, ALU.mult, ["iotaj", T3k], ["At%d" % k])
                c.mm(pb_[:, (tk % 4) * 128:(tk % 4 + 1) * 128], Bt[k][:], At[k][:], True, True, ["Bt%d" % k, "At%d" % k], [pbk])
                if tk % 4 == 3:
                    tb = tl * 128 + tk - 3
                    c.cp("act", GG[:, tb:tb + 4, :].rearrange("p t i -> p (t i)"), pb_[:, :], [pbk], ["GG"])

        def issue_pre(i):
            u_, uk = uTi[i % 3], "uTi%d" % (i % 3)
            v_, vk = vi[i % 3], "vi%d" % (i % 3)
            c.dma(u_[:].rearrange("p c j -> p (c j)"), K.UT[i], ["UT"], [uk], e="sp")
            c.dma(v_[:], K.VB[i], ["VB"], [vk], e="sp")
            pre, prek = Bk[4 + i % 2], "bank%d" % (4 + i % 2)
            for ch in range(8):
                c.mm(pre[:, 0:GT * 128], u_[:, ch, :], xTg[:, ch, :], ch == 0, ch == 7, [uk, "xTg"], [prek])

        def issue_gelu(i):
            pre, prek = Bk[4 + i % 2], "bank%d" % (4 + i % 2)
            c.act(gl[i % 2][:], pre[:, 0:GT * 128], AF.Gelu, [prek], ["gl%d" % (i % 2)])

        def issue_w(i):
            c.tt("dve", wT[i % 2][:], GG[:, :, i], gl[i % 2][:], ALU.mult, ["GG", "gl%d" % (i % 2)], ["wT%d" % (i % 2)])

        def issue_y(i):
            v_, vk = vi[i % 3], "vi%d" % (i % 3)
            for tl in range(GT):
                for nb in range(2):
                    c.mm(Bk[tl * 2 + nb][:, :], wT[i % 2][:, tl * 128:(tl + 1) * 128], v_[:, nb * 512:(nb + 1) * 512], i == 0, i == 127,
                         ["wT%d" % (i % 2), vk], ["bank%d" % (tl * 2 + nb)])

        issue_pre(0)
        issue_gelu(0)
        for i in range(128):
            if i + 1 < 128:
                issue_pre(i + 1)
            issue_w(i)
            if i + 1 < 128:
                issue_gelu(i + 1)
            issue_y(i)
        for tl, t in enumerate(tiles):
            m = 0 if t < NTL else 1
            emit_residual_banks(c, K, W, t, Bk[tl * 2], Bk[tl * 2 + 1], ["bank%d" % (tl * 2), "bank%d" % (tl * 2 + 1)], m)
    c.pop()


def emit_peer4(c, K, layer, ntiles):
    emit_peer_prep(c, K, layer)
    c.push()
    GT = 2
    wqb = c.sb("wqb", [128, 8, 2048], BF16)
    c.push()
    wst = [c.sb("wqst%d" % i, [128, 2048]) for i in range(2)]
    for ch in range(8):
        c.dma(wst[ch % 2][:], K.peer_w_q[layer, ch * 128:(ch + 1) * 128, :], [], ["wqst%d" % (ch % 2)], e="sp" if ch % 2 else "pool")
        c.cp("dve" if ch % 2 else "pool", wqb[:, ch, :], wst[ch % 2][:], ["wqst%d" % (ch % 2)], ["wqb"])
    c.pop()
    skf = c.sb("skf", [128, 2, 128])
    skb = c.sb("skb", [128, 2, 128], BF16)
    skT = c.sb("skT", [128, 2, 128], BF16)
    Bk = [c.ps("bank%d" % i, [128, 512]) for i in range(8)]
    Bkb = [b[:].bitcast(BF16) for b in Bk]
    for p in range(2):
        c.dma(skf[:, p, :], K.peer_sk[layer, p], [], ["skf"])
    c.cp("dve", skb[:], skf[:], ["skf"], ["skb"])
    for p in range(2):
        c.tr(Bkb[7][:, p * 128:(p + 1) * 128], skb[:, p, :], K.identb[:], ["skb", "identb"], ["bank7"])
    c.cp("dve", skT[:].rearrange("p a k -> p (a k)"), Bkb[7][:, 0:256], ["bank7"], ["skT"])
    iotaj = c.sb("iotaj", [128, 128])
    iota_a = c.sb("iota_a", [128, 8, 16, 16])
    c.dma(iotaj[:], K.iotaj_in, [], ["iotaj"])
    c.dma(iota_a[:].rearrange("p h r a -> p (h r a)"), K.iotaa_in, [], ["iota_a"])

    W = alloc_normwork(c, Bkb[7])
    W.ptrkey = "bank7"
    xTg = [c.sb("xTg%d" % i, [128, 8, GT * 128], BF16) for i in range(2)]
    qb16 = c.sb("qb16", [128, 512], BF16)
    qT = c.sb("qT", [128, 4, 128], BF16)
    S = c.sb("S", [128, 16, 128])
    SV = c.sb("SV", [128, 16, 16])
    SIu = c.sb("SIu", [128, 16, 16], U32)
    SIf = c.sb("SIf", [128, 16, 16])
    wk = c.sb("wk", [128, 256])
    cand = c.sb("cand", [128, 8, 256])
    eq = cand[:].rearrange("p h (a b) -> p h a b", a=16)
    CV = c.sb("CV", [128, 8, 16])
    CPu = c.sb("CPu", [128, 8, 16], U32)
    CPf = c.sb("CPf", [128, 8, 16])
    ab = c.sb("abf", [128, 2, 8, 16])
    IJG = c.sb("IJG", [128, 3, 128])
    IJGT = [[c.sb("IJGT%d%d" % (i, j), [128, 3, 128]) for j in range(GT)] for i in range(2)]
    ez = c.sb("ez", [128, 8, 16])
    st8 = c.sb("st8", [128, 16])
    NB = 24
    At = [c.sb("At%d" % i, [128, 64], BF16) for i in range(NB)]
    Bt = [c.sb("Bt%d" % i, [128, 128], BF16) for i in range(NB)]
    GG = [c.sb("GG%d" % i, [128, GT * 128, 64], BF16) for i in range(2)]
    NR = 3
    uTi = [c.sb("uTi%d" % i, [128, 8, 128], BF16) for i in range(NR)]
    vi = [c.sb("vi%d" % i, [128, 1024], BF16) for i in range(NR)]
    gl = [c.sb("gl%d" % i, [128, GT * 128], BF16) for i in range(3)]
    wT = [c.sb("wT%d" % i, [128, GT * 128], BF16) for i in range(3)]
    ring = Prog()
    ring.e = 0
    ring.pb = 0
    ngroups = ntiles // GT

    def P1(g, tl):
        t = g * GT + tl
        m = 0 if t < NTL else 1
        xg, xgk = xTg[g % 2], "xTg%d" % (g % 2)
        emit_norm_T_peer(c, K, W, hrows(K, t), "H%d" % t, m, xg[:, :, tl * 128:(tl + 1) * 128], xgk)
        yield
        for qq in range(4):
            for ch in range(8):
                c.mm(Bk[6][:, :], xg[:, ch, tl * 128:(tl + 1) * 128], wqb[:, ch, qq * 512:(qq + 1) * 512], ch == 0, ch == 7, [xgk, "wqb"], ["bank6"])
            c.cp("act", qb16[:, :], Bk[6][:, :], ["bank6"], ["qb16"])
            yield
            for b in range(4):
                c.tr(Bkb[7][:, b * 128:(b + 1) * 128], qb16[:, b * 128:(b + 1) * 128], K.identb[:], ["qb16", "identb"], ["bank7"])
            c.cp("act", qT[:].rearrange("p b t -> p (b t)"), Bkb[7][:, 0:512], ["bank7"], ["qT"])
            yield
            for b in range(4):
                c.mm(Bk[6][:, b * 128:(b + 1) * 128], qT[:, b, :], skT[:, b % 2, :], True, True, ["qT", "skT"], ["bank6"])
            c.cp("act", S[:, qq * 4:(qq + 1) * 4, :].rearrange("p b k -> p (b k)"), Bk[6][:, :], ["bank6"], ["S"])
            yield
        for hp in range(16):
            c.op("dve", lambda o: o.max(out=SV[:, hp, 0:8], in_=S[:, hp, :]), ["S"], ["SV"])
            c.op("dve", lambda o: o.max_index(out=SIu[:, hp, 0:8], in_max=SV[:, hp, 0:8], in_values=S[:, hp, :]), ["S", "SV"], ["SIu"])
            c.op("dve", lambda o: o.match_replace(out=wk[:, 0:128], in_to_replace=SV[:, hp, 0:8], in_values=S[:, hp, :], imm_value=-1e30),
                 ["S", "SV"], ["wk"])
            c.op("dve", lambda o: o.max(out=SV[:, hp, 8:16], in_=wk[:, 0:128]), ["wk"], ["SV"])
            c.op("dve", lambda o: o.max_index(out=SIu[:, hp, 8:16], in_max=SV[:, hp, 8:16], in_values=wk[:, 0:128]), ["wk", "SV"], ["SIu"])
            if hp % 2 == 1:
                yield
        c.cp("dve", SIf[:], SIu[:], ["SIu"], ["SIf"])
        SV8 = SV[:].rearrange("p (h two) a -> p h two a", h=8, two=2)
        SI8 = SIf[:].rearrange("p (h two) a -> p h two a", h=8, two=2)
        c.tt("dve", eq, SV8[:, :, 0, :].unsqueeze(3).to_broadcast([128, 8, 16, 16]),
             SV8[:, :, 1, :].unsqueeze(2).to_broadcast([128, 8, 16, 16]), ALU.add, ["SV"], ["cand"])
        yield
        for h in range(8):
            c.op("dve", lambda o: o.max(out=CV[:, h, 0:8], in_=cand[:, h, :]), ["cand"], ["CV"])
            c.op("dve", lambda o: o.max_index(out=CPu[:, h, 0:8], in_max=CV[:, h, 0:8], in_values=cand[:, h, :]), ["cand", "CV"], ["CPu"])
            c.op("dve", lambda o: o.match_replace(out=wk[:, :], in_to_replace=CV[:, h, 0:8], in_values=cand[:, h, :], imm_value=-1e30),
                 ["cand", "CV"], ["wk"])
            c.op("dve", lambda o: o.max(out=CV[:, h, 8:16], in_=wk[:, :]), ["wk"], ["CV"])
            c.op("dve", lambda o: o.max_index(out=CPu[:, h, 8:16], in_max=CV[:, h, 8:16], in_values=wk[:, :]), ["wk", "CV"], ["CPu"])
            if h % 2 == 1:
                yield
        c.cp("dve", CPf[:], CPu[:], ["CPu"], ["CPf"])
        c.ts("dve", ab[:, 1], CPf[:], 1.0 / 16, -1.0, ALU.mult, ALU.add, ["CPf"], ["abf"])
        c.tt("dve", eq, ab[:, 1].unsqueeze(3).to_broadcast([128, 8, 16, 16]), iota_a[:], ALU.is_ge, ["abf", "iota_a"], ["cand"])
        c.op("dve", lambda o: o.tensor_reduce(out=ab[:, 0], in_=eq, axis=AX.X, op=ALU.add), ["cand"], ["abf"])
        c.op("dve", lambda o: o.scalar_tensor_tensor(out=ab[:, 1], in0=ab[:, 0], scalar=-16.0, in1=CPf[:], op0=ALU.mult, op1=ALU.add), ["abf", "CPf"], ["abf"])
        yield
        IJ4 = IJG[:].rearrange("p c (h r) -> p c h r", h=8)
        for which in range(2):
            c.tt("dve", eq, ab[:, which].unsqueeze(3).to_broadcast([128, 8, 16, 16]), iota_a[:], ALU.is_equal, ["abf", "iota_a"], ["cand"])
            c.tt("dve", eq, eq, SI8[:, :, which, :].unsqueeze(2).to_broadcast([128, 8, 16, 16]), ALU.mult, ["cand", "SIf"], ["cand"])
            c.op("dve", lambda o: o.tensor_reduce(out=IJ4[:, which], in_=eq, axis=AX.X, op=ALU.add), ["cand"], ["IJG"])
            yield
        c.tt("dve", ez[:], CV[:], CV[:, :, 0:1].to_broadcast([128, 8, 16]), ALU.subtract, ["CV"], ["ez"])
        c.act(ez[:].rearrange("p h a -> p (h a)"), ez[:].rearrange("p h a -> p (h a)"), AF.Exp, ["ez"], ["ez"])
        c.op("dve", lambda o: o.tensor_reduce(out=st8[:, 0:8], in_=ez[:], axis=AX.X, op=ALU.add), ["ez"], ["st8a"])
        c.op("dve", lambda o: o.reciprocal(out=st8[:, 8:16], in_=st8[:, 0:8]), ["st8a"], ["st8b"])
        c.tt("dve", IJ4[:, 2], ez[:], st8[:, 8:16].unsqueeze(2).to_broadcast([128, 8, 16]), ALU.mult, ["ez", "st8b"], ["IJG"])
        yield
        for w_ in range(3):
            c.tr(Bk[6][:, w_ * 128:(w_ + 1) * 128], IJG[:, w_, :], K.identf[:], ["IJG", "identf"], ["bank6"])
        T3, T3k = IJGT[g % 2][tl], "IJGT%d%d" % (g % 2, tl)
        c.cp("act", T3[:].rearrange("p c t -> p (c t)"), Bk[6][:, 0:384], ["bank6"], [T3k])
        yield

    def expand(g, half, tl):
        T3, T3k = IJGT[g % 2][tl], "IJGT%d%d" % (g % 2, tl)
        G_, Gk = GG[half], "GG%d" % half
        io_h = iotaj[:, half * 64:(half + 1) * 64]
        for tk in range(128):
            k = ring.e % NB
            ring.e += 1
            if tk % 8 == 0:
                ring.pb += 1
            pb_, pbk = Bk[6 + ring.pb % 2], "bank%d" % (6 + ring.pb % 2)
            c.ts("dve", Bt[k][:], iotaj[:], T3[:, 1, tk:tk + 1], None, ALU.is_equal, None, ["iotaj", T3k], ["Bt%d" % k])
            c.ts("dve", At[k][:], io_h, T3[:, 0, tk:tk + 1], T3[:, 2, tk:tk + 1], ALU.is_equal, ALU.mult, ["iotaj", T3k], ["At%d" % k])
            c.mm(pb_[:, (tk % 8) * 64:(tk % 8 + 1) * 64], Bt[k][:], At[k][:], True, True, ["Bt%d" % k, "At%d" % k], [pbk])
            if tk % 8 == 7:
                tb = tl * 128 + tk - 7
                c.cp("act", G_[:, tb:tb + 8, :].rearrange("p t i -> p (t i)"), pb_[:, :], [pbk], [Gk])
                yield

    def dense(g, half):
        xg, xgk = xTg[g % 2], "xTg%d" % (g % 2)
        G_, Gk = GG[half], "GG%d" % half
        tiles = [g * GT + i for i in range(GT)]

        def issue_dma(i):
            u_, uk = uTi[i % NR], "uTi%d" % (i % NR)
            v_, vk = vi[i % NR], "vi%d" % (i % NR)
            c.dma(u_[:].rearrange("p c j -> p (c j)"), K.UT[i], ["UT"], [uk], e="sp")
            c.dma(v_[:], K.VB[i], ["VB"], [vk], e="sp")

        def issue_pre(i):
            u_, uk = uTi[i % NR], "uTi%d" % (i % NR)
            if i + 1 < 128:
                issue_dma(i + 1)
            pre = Bk[4 + i % 2][:, 0:256]
            for ch in range(8):
                c.mm(pre, u_[:, ch, :], xg[:, ch, :], ch == 0, ch == 7, [uk, xgk], ["bank%d" % (4 + i % 2)])

        def issue_gelu(i):
            pre = Bk[4 + i % 2][:, 0:256]
            c.act(gl[i % 3][:], pre, AF.Gelu, ["bank%d" % (4 + i % 2)], ["gl%d" % (i % 3)])

        if half == 0:
            issue_dma(0)
            issue_pre(0)
            issue_gelu(0)
        for i in range(half * 64, half * 64 + 64):
            if i + 1 < 128:
                issue_pre(i + 1)
            c.tt("dve", wT[i % 3][:], G_[:, :, i - half * 64], gl[i % 3][:], ALU.mult, [Gk, "gl%d" % (i % 3)], ["wT%d" % (i % 3)])
            if i + 1 < 128:
                issue_gelu(i + 1)
            v_, vk = vi[i % NR], "vi%d" % (i % NR)
            for tl in range(GT):
                for nb in range(2):
                    c.mm(Bk[tl * 2 + nb][:, :], wT[i % 3][:, tl * 128:(tl + 1) * 128], v_[:, nb * 512:(nb + 1) * 512], i == 0, i == 127,
                         ["wT%d" % (i % 3), vk], ["bank%d" % (tl * 2 + nb)])
            yield
        if half == 1:
            for tl, t in enumerate(tiles):
                m = 0 if t < NTL else 1
                emit_residual_banks(c, K, W, t, Bk[tl * 2], Bk[tl * 2 + 1], ["bank%d" % (tl * 2), "bank%d" % (tl * 2 + 1)], m)
            yield

    def chain(*gens):
        for g_ in gens:
            yield from g_

    _interleave([chain(P1(0, 0), P1(0, 1), expand(0, 0, 0), expand(0, 0, 1))])
    for g in range(ngroups):
        nxt = g + 1 < ngroups
        side = [expand(g, 1, 0), expand(g, 1, 1)]
        if nxt:
            side.append(P1(g + 1, 0))
        _interleave([dense(g, 0), chain(*side)])
        side = []
        if nxt:
            side = [P1(g + 1, 1), expand(g + 1, 0, 0), expand(g + 1, 0, 1)]
        _interleave([dense(g, 1), chain(*side)])
    c.pop()
```
